# Optimizing a Trainium2 kernel written in Bass

```python
import math
import jax, jax.numpy as jnp
from jax import lax
import numpy as np

D_MODEL = 2048
BATCH = 4
SEQ = 4096
DEPTH = 2

GRID_W = 64
CTX_LEN = 256
HEAD_DIM = 128
NA_HEADS = 8
NA_ROWS = 8
NA_COLS = 16
GQA_Q_HEADS = 8
GQA_KV_HEADS = 2
Q_BLOCK = 128
ROPE_THETA = 10000.0
GDN_HEADS = 16
GDN_HEAD_DIM = 128
GDN_CHUNK = 64
CONV_K = 4
FFN_DIM = 5632
N_EXPERTS = 8
MOE_TOP_K = 2
MOE_BLOCK = 512
EPS = 1e-6

NA_W = NA_HEADS * HEAD_DIM
GQA_Q_W = GQA_Q_HEADS * HEAD_DIM
GQA_KV_W = GQA_KV_HEADS * HEAD_DIM
EVEN_IN_W = 3 * NA_W + GQA_Q_W + 2 * GQA_KV_W
EVEN_MIX_W = NA_W + GQA_Q_W
GDN_W = GDN_HEADS * GDN_HEAD_DIM
GDN_QKV_W = 3 * GDN_W
ODD_IN_W = GDN_QKV_W + GDN_W + 4 * GDN_HEADS

kernel_name = 'hybrid_na_gqa_gdn_moe_dit'


def _rms(x, w):
    xf = x.astype(jnp.float32)
    y = xf * lax.rsqrt(jnp.mean(xf * xf, axis=-1, keepdims=True) + EPS)
    return (y * w.astype(jnp.float32)).astype(x.dtype)


def _l2norm(x):
    xf = x.astype(jnp.float32)
    return (xf * lax.rsqrt(jnp.sum(xf * xf, axis=-1, keepdims=True) + EPS)).astype(x.dtype)


def _modulate(h, shift, scale):
    return h * (1 + scale) + shift


def _adaln(c, c_ctx, w, b):
    m = (jax.nn.silu(c) @ w + b)[:, None, :]
    mc = (jax.nn.silu(c_ctx) @ w + b)[None, None, :]
    return jnp.split(m, 6, axis=-1), jnp.split(mc, 6, axis=-1)


def _swiglu(h, wg, wu, wd):
    return (jax.nn.silu(h @ wg) * (h @ wu)) @ wd


def _axial_rope(x, row, col):
    d = x.shape[-1]
    half = d // 2
    quarter = half // 2
    inv = ROPE_THETA ** (-jnp.arange(quarter, dtype=jnp.float32) / quarter)
    xf = x.astype(jnp.float32)

    def rot(xh, pos):
        ang = pos[:, None] * inv[None, :]
        cos = jnp.cos(ang)[None, :, None, :]
        sin = jnp.sin(ang)[None, :, None, :]
        x1, x2 = xh[..., :quarter], xh[..., quarter:]
        return jnp.concatenate([x1 * cos - x2 * sin, x2 * cos + x1 * sin], axis=-1)

    return jnp.concatenate([rot(xf[..., :half], row), rot(xf[..., half:], col)], axis=-1).astype(x.dtype)


def _attend_blocks(q, k, v):
    B, Lq, Hk, G, d = q.shape
    nb = Lq // Q_BLOCK
    qb = jnp.moveaxis(q.reshape(B, nb, Q_BLOCK, Hk, G, d), 1, 0)
    scale = d ** -0.5

    def one(qi):
        s = jnp.einsum('bqhgd,bkhd->bhgqk', qi, k, preferred_element_type=jnp.float32) * scale
        p = jax.nn.softmax(s, axis=-1).astype(v.dtype)
        return jnp.einsum('bhgqk,bkhd->bqhgd', p, v)

    o = lax.map(one, qb)
    return jnp.moveaxis(o, 0, 1).reshape(B, Lq, Hk * G * d)


def _neighbourhood_attention(q, k, v, k_ctx, v_ctx, rpb):
    B, S, H, d = q.shape
    rows = S // GRID_W
    kh = min(NA_ROWS, rows)
    kw = NA_COLS
    scale = d ** -0.5
    qg = jnp.moveaxis(q.reshape(B, rows, GRID_W, H, d), 1, 0)
    kg = k.reshape(B, rows, GRID_W, H, d)
    vg = v.reshape(B, rows, GRID_W, H, d)
    col = np.arange(GRID_W)
    col_idx = np.clip(col - kw // 2, 0, GRID_W - kw)[:, None] + np.arange(kw)[None, :]
    col_off = col_idx - col[:, None] + (NA_COLS - 1)
    rpb_c = rpb.astype(jnp.float32)[:, :, col_off]
    n_loc = kh * kw

    def one_row(args):
        r, q_row = args
        rs = jnp.clip(r - kh // 2, 0, rows - kh)
        k_win = lax.dynamic_slice_in_dim(kg, rs, kh, axis=1)[:, :, col_idx]
        v_win = lax.dynamic_slice_in_dim(vg, rs, kh, axis=1)[:, :, col_idx]
        row_off = rs + jnp.arange(kh) - r + (NA_ROWS - 1)
        bias = jnp.transpose(jnp.take(rpb_c, row_off, axis=1), (0, 2, 1, 3))
        s_loc = jnp.einsum('bchd,bicjhd->bhcij', q_row, k_win, preferred_element_type=jnp.float32) * scale + bias[None]
        s_ctx = jnp.einsum('bchd,bkhd->bhck', q_row, k_ctx, preferred_element_type=jnp.float32) * scale
        s = jnp.concatenate([s_loc.reshape(B, H, GRID_W, n_loc), s_ctx], axis=-1)
        p = jax.nn.softmax(s, axis=-1).astype(v.dtype)
        o = (jnp.einsum('bhcij,bicjhd->bchd', p[..., :n_loc].reshape(B, H, GRID_W, kh, kw), v_win)
             + jnp.einsum('bhck,bkhd->bchd', p[..., n_loc:], v_ctx))
        return o

    o = lax.map(one_row, (jnp.arange(rows), qg))
    return jnp.moveaxis(o, 0, 1).reshape(B, S, H * d)


def _short_conv(x, w):
    K = w.shape[0]
    return lax.conv_general_dilated(
        x, w[:, None, :].astype(x.dtype), window_strides=(1,), padding=[(K // 2, K - 1 - K // 2)],
        dimension_numbers=('NWC', 'WIO', 'NWC'), feature_group_count=x.shape[-1])


def _gated_delta(q, k, v, g, beta, state):
    B, T, H, dk = q.shape
    dv = v.shape[-1]
    out_dtype = v.dtype
    C = GDN_CHUNK
    n = T // C

    def f(t):
        return jnp.moveaxis(t.astype(jnp.float32).reshape((B, n, C, H) + t.shape[3:]), 3, 1)

    q = f(q) * dk ** -0.5
    k = f(k)
    v = f(v)
    beta = f(beta)
    g = jnp.cumsum(f(g), axis=-1)
    incl = jnp.tril(jnp.ones((C, C), bool))
    strict = jnp.tril(jnp.ones((C, C), bool), -1)
    gd = g[..., :, None] - g[..., None, :]
    decay = jnp.where(incl, jnp.exp(jnp.where(incl, gd, 0.0)), 0.0)
    kb = k * beta[..., None]
    lower = jnp.where(strict, jnp.einsum('bhncd,bhnsd->bhncs', kb, k) * decay, 0.0)
    a = lower + jnp.eye(C, dtype=jnp.float32)
    rhs = jnp.concatenate([v * beta[..., None], kb * jnp.exp(g)[..., None]], axis=-1)
    sol = lax.linalg.triangular_solve(a, rhs, left_side=True, lower=True, unit_diagonal=True)
    u, w = sol[..., :dv], sol[..., dv:]
    a_intra = jnp.where(incl, jnp.einsum('bhncd,bhnsd->bhncs', q, k) * decay, 0.0)
    xs = tuple(jnp.moveaxis(t, 2, 0) for t in (q, k, u, w, g, a_intra))

    def step(S, inp):
        qi, ki, ui, wi, gi, ai = inp
        v_new = ui - jnp.einsum('bhck,bhkv->bhcv', wi, S)
        o = (jnp.einsum('bhck,bhkv->bhcv', qi * jnp.exp(gi)[..., None], S)
             + jnp.einsum('bhcs,bhsv->bhcv', ai, v_new))
        g_last = gi[..., -1:]
        S = S * jnp.exp(g_last)[..., None] + jnp.einsum(
            'bhck,bhcv->bhkv', ki * jnp.exp(g_last - gi)[..., None], v_new)
        return S, o

    state, o = lax.scan(step, state.astype(jnp.float32), xs)
    o = o.transpose(1, 0, 3, 2, 4).reshape(B, T, H, dv)
    return o.astype(out_dtype), state


def _gdn_project(p, conv_w, a_log_f, a_log_b, dt_f, dt_b):
    B, L, _ = p.shape
    qkv = jax.nn.silu(_short_conv(p[..., :GDN_QKV_W], conv_w))
    q, k, v = [t.reshape(B, L, GDN_HEADS, GDN_HEAD_DIM) for t in jnp.split(qkv, 3, axis=-1)]
    z = p[..., GDN_QKV_W:GDN_QKV_W + GDN_W].reshape(B, L, GDN_HEADS, GDN_HEAD_DIM)
    b_f, b_b, a_f, a_b = jnp.split(p[..., GDN_QKV_W + GDN_W:].astype(jnp.float32), 4, axis=-1)
    g_f = -jnp.exp(a_log_f.astype(jnp.float32)) * jax.nn.softplus(a_f + dt_f.astype(jnp.float32))
    g_b = -jnp.exp(a_log_b.astype(jnp.float32)) * jax.nn.softplus(a_b + dt_b.astype(jnp.float32))
    return _l2norm(q), _l2norm(k), v, z, (g_f, jax.nn.sigmoid(b_f)), (g_b, jax.nn.sigmoid(b_b))


def _gated_out(o, z, w):
    B, L, H, d = o.shape
    return (_rms(o, w) * jax.nn.silu(z)).reshape(B, L, H * d)


def _moe_swiglu(h, router_w, wg, wu, wd):
    B, S, D = h.shape
    T = B * S
    hf = h.reshape(T, D)
    logits = jnp.dot(hf, router_w, preferred_element_type=jnp.float32)
    top_logit, top_e = lax.top_k(logits, MOE_TOP_K)
    gate = jax.nn.softmax(top_logit, axis=-1)
    n_assign = T * MOE_TOP_K
    e_flat = top_e.reshape(n_assign)
    tok_flat = jnp.arange(n_assign, dtype=jnp.int32) // MOE_TOP_K
    order = jnp.argsort(e_flat)
    e_sorted = e_flat[order]
    counts = jnp.bincount(e_flat, length=N_EXPERTS)
    padded = (counts + MOE_BLOCK - 1) // MOE_BLOCK * MOE_BLOCK
    pad_end = jnp.cumsum(padded)
    pad_start = pad_end - padded
    grp_start = jnp.cumsum(counts) - counts
    slot = pad_start[e_sorted] + jnp.arange(n_assign, dtype=jnp.int32) - grp_start[e_sorted]
    n_blocks = -(-n_assign // MOE_BLOCK) + N_EXPERTS
    n_slots = n_blocks * MOE_BLOCK
    slot_tok = jnp.zeros((n_slots,), jnp.int32).at[slot].set(tok_flat[order])
    slot_gate = jnp.zeros((n_slots,), jnp.float32).at[slot].set(gate.reshape(n_assign)[order])
    blk_expert = jnp.minimum(
        jnp.searchsorted(pad_end, jnp.arange(n_blocks, dtype=jnp.int32) * MOE_BLOCK, side='right'), N_EXPERTS - 1)
    xb = hf[slot_tok].reshape(n_blocks, MOE_BLOCK, D)

    def expert_block(args):
        xi, e = args
        return (jax.nn.silu(xi @ wg[e]) * (xi @ wu[e])) @ wd[e]

    yb = lax.map(expert_block, (xb, blk_expert)).reshape(n_slots, D)
    y = jnp.zeros((T, D), h.dtype).at[slot_tok].add(yb * slot_gate[:, None].astype(yb.dtype))
    return y.reshape(B, S, D)


def _split_even(p):
    B, L, _ = p.shape
    sizes = (NA_W, NA_W, NA_W, GQA_Q_W, GQA_KV_W, GQA_KV_W)
    heads = (NA_HEADS, NA_HEADS, NA_HEADS, GQA_Q_HEADS, GQA_KV_HEADS, GQA_KV_HEADS)
    parts = jnp.split(p, [int(i) for i in np.cumsum(sizes)[:-1]], axis=-1)
    return [t.reshape(B, L, nh, HEAD_DIM) for t, nh in zip(parts, heads)]


def _even_layer(x, cx, c, c_ctx, ada_w, ada_b, norm_mix, norm_ffn, w_in, w_out, na_rpb,
                q_norm_w, k_norm_w, w_gate, w_up, w_down, last):
    B, S, _ = x.shape
    C = cx.shape[1]
    grp = GQA_Q_HEADS // GQA_KV_HEADS
    m, mc = _adaln(c, c_ctx, ada_w, ada_b)
    qa, ka, va, qb, kb, vb = _split_even(_modulate(_rms(x, norm_mix), m[0], m[1]) @ w_in)
    qac, kac, vac, qbc, kbc, vbc = _split_even(_modulate(_rms(cx, norm_mix), mc[0], mc[1]) @ w_in)
    oa = _neighbourhood_attention(qa, ka, va, kac, vac, na_rpb)
    t = jnp.arange(S)
    row = (t // GRID_W).astype(jnp.float32)
    col = (t % GRID_W).astype(jnp.float32)
    qb = _axial_rope(_rms(qb, q_norm_w), row, col)
    kb = _axial_rope(_rms(kb, k_norm_w), row, col)
    qbc = _rms(qbc, q_norm_w)
    kbc = _rms(kbc, k_norm_w)
    ob = _attend_blocks(qb.reshape(B, S, GQA_KV_HEADS, grp, HEAD_DIM),
                        jnp.concatenate([kbc, kb], axis=1), jnp.concatenate([vbc, vb], axis=1))
    x = x + m[2] * (jnp.concatenate([oa, ob], axis=-1) @ w_out)
    x = x + m[5] * _swiglu(_modulate(_rms(x, norm_ffn), m[3], m[4]), w_gate, w_up, w_down)
    if not last:
        oac = _attend_blocks(qac[:, :, :, None, :], kac, vac)
        obc = _attend_blocks(qbc.reshape(B, C, GQA_KV_HEADS, grp, HEAD_DIM), kbc, vbc)
        cx = cx + mc[2] * (jnp.concatenate([oac, obc], axis=-1) @ w_out)
        cx = cx + mc[5] * _swiglu(_modulate(_rms(cx, norm_ffn), mc[3], mc[4]), w_gate, w_up, w_down)
    return x, cx


def _odd_layer(x, cx, c, c_ctx, ada_w, ada_b, norm_mix, norm_ffn, w_in, conv_w, a_log_fwd, a_log_bwd,
               dt_bias_fwd, dt_bias_bwd, gdn_norm_w, w_out, router_w, moe_w_gate, moe_w_up, moe_w_down, last):
    B = x.shape[0]
    m, mc = _adaln(c, c_ctx, ada_w, ada_b)
    gp = (a_log_fwd, a_log_bwd, dt_bias_fwd, dt_bias_bwd)
    q, k, v, z, (g_f, b_f), (g_b, b_b) = _gdn_project(
        _modulate(_rms(x, norm_mix), m[0], m[1]) @ w_in, conv_w, *gp)
    qc, kc, vc, zc, (gc_f, bc_f), (gc_b, bc_b) = _gdn_project(
        _modulate(_rms(cx, norm_mix), mc[0], mc[1]) @ w_in, conv_w, *gp)

    def flip(t):
        return jnp.flip(t, axis=1)

    s0 = jnp.zeros((B, GDN_HEADS, GDN_HEAD_DIM, GDN_HEAD_DIM), jnp.float32)
    oc_f, s_f = _gated_delta(qc, kc, vc, gc_f, bc_f, s0)
    oc_b, s_b = _gated_delta(flip(qc), flip(kc), flip(vc), flip(gc_b), flip(bc_b), s0)
    o_f, _ = _gated_delta(q, k, v, g_f, b_f, s_f)
    o_b, _ = _gated_delta(flip(q), flip(k), flip(v), flip(g_b), flip(b_b), s_b)
    o = o_f + flip(o_b)
    x = x + m[2] * (_gated_out(o, z, gdn_norm_w) @ w_out)
    x = x + m[5] * _moe_swiglu(_modulate(_rms(x, norm_ffn), m[3], m[4]), router_w, moe_w_gate, moe_w_up, moe_w_down)
    if not last:
        oc = oc_f + flip(oc_b)
        cx = cx + mc[2] * (_gated_out(oc, zc, gdn_norm_w) @ w_out)
        cx = cx + mc[5] * _moe_swiglu(_modulate(_rms(cx, norm_ffn), mc[3], mc[4]),
                                      router_w, moe_w_gate, moe_w_up, moe_w_down)
    return x, cx


def setup_inputs(seed: int = 0) -> dict:
    key = jax.random.key(seed)
    it = iter(jax.random.split(key, 40))
    D = D_MODEL
    inv_d = D ** -0.5

    def nrm(shape, std):
        return jax.random.normal(next(it), shape, jnp.float32) * std

    def gain(n):
        return 1.0 + nrm((n,), 0.02)

    def a_log():
        return jnp.log(jax.random.uniform(next(it), (GDN_HEADS,), jnp.float32, 1.0, 16.0))

    def dt_bias():
        u = jax.random.uniform(next(it), (GDN_HEADS,), jnp.float32)
        dt = jnp.exp(u * (math.log(0.1) - math.log(1e-3)) + math.log(1e-3))
        return dt + jnp.log(-jnp.expm1(-dt))

    return {
        'x': nrm((BATCH, SEQ, D), 1.0),
        'c': nrm((BATCH, D), 1.0),
        'ctx': nrm((BATCH, CTX_LEN, D), 1.0),
        'c_ctx': nrm((D,), 1.0),
        'ada_w0': nrm((D, 6 * D), 0.5 * inv_d),
        'ada_b0': nrm((6 * D,), 0.01),
        'norm_mix0': gain(D),
        'norm_ffn0': gain(D),
        'w_in0': nrm((D, EVEN_IN_W), inv_d),
        'w_out0': nrm((EVEN_MIX_W, D), EVEN_MIX_W ** -0.5),
        'na_rpb': nrm((NA_HEADS, 2 * NA_ROWS - 1, 2 * NA_COLS - 1), 0.05),
        'q_norm_w': gain(HEAD_DIM),
        'k_norm_w': gain(HEAD_DIM),
        'ffn_w_gate': nrm((D, FFN_DIM), inv_d),
        'ffn_w_up': nrm((D, FFN_DIM), inv_d),
        'ffn_w_down': nrm((FFN_DIM, D), FFN_DIM ** -0.5),
        'ada_w1': nrm((D, 6 * D), 0.5 * inv_d),
        'ada_b1': nrm((6 * D,), 0.01),
        'norm_mix1': gain(D),
        'norm_ffn1': gain(D),
        'w_in1': nrm((D, ODD_IN_W), inv_d),
        'conv_w1': nrm((CONV_K, GDN_QKV_W), CONV_K ** -0.5),
        'a_log_fwd': a_log(),
        'a_log_bwd': a_log(),
        'dt_bias_fwd': dt_bias(),
        'dt_bias_bwd': dt_bias(),
        'gdn_norm_w': gain(GDN_HEAD_DIM),
        'w_out1': nrm((GDN_W, D), GDN_W ** -0.5),
        'router_w': nrm((D, N_EXPERTS), inv_d),
        'moe_w_gate': nrm((N_EXPERTS, D, FFN_DIM), inv_d),
        'moe_w_up': nrm((N_EXPERTS, D, FFN_DIM), inv_d),
        'moe_w_down': nrm((N_EXPERTS, FFN_DIM, D), FFN_DIM ** -0.5),
        'final_norm_w': gain(D),
    }


def reference(x, c, ctx, c_ctx, ada_w0, ada_b0, norm_mix0, norm_ffn0, w_in0, w_out0, na_rpb, q_norm_w, k_norm_w,
              ffn_w_gate, ffn_w_up, ffn_w_down, ada_w1, ada_b1, norm_mix1, norm_ffn1, w_in1, conv_w1,
              a_log_fwd, a_log_bwd, dt_bias_fwd, dt_bias_bwd, gdn_norm_w, w_out1, router_w,
              moe_w_gate, moe_w_up, moe_w_down, final_norm_w):
    layers = (
        (ada_w0, ada_b0, norm_mix0, norm_ffn0, w_in0, w_out0, na_rpb, q_norm_w, k_norm_w,
         ffn_w_gate, ffn_w_up, ffn_w_down),
        (ada_w1, ada_b1, norm_mix1, norm_ffn1, w_in1, conv_w1, a_log_fwd, a_log_bwd, dt_bias_fwd, dt_bias_bwd,
         gdn_norm_w, w_out1, router_w, moe_w_gate, moe_w_up, moe_w_down),
    )
    cx = ctx
    for i in range(DEPTH):
        last = i == DEPTH - 1
        if i % 2 == 0:
            x, cx = _even_layer(x, cx, c, c_ctx, *layers[i], last=last)
        else:
            x, cx = _odd_layer(x, cx, c, c_ctx, *layers[i], last=last)
    return _rms(x, final_norm_w)
```

```python
import numpy as np
import concourse.bass as bass
import concourse.mybir as mybir
from concourse.bass_utils import run_bass_kernel_spmd
from contextlib import ExitStack

F32 = mybir.dt.float32
BF16 = mybir.dt.bfloat16
ALU = mybir.AluOpType
AF = mybir.ActivationFunctionType
AX = mybir.AxisListType

SEM_CAP = 4000
DMA_RING = 6


class Op:
    __slots__ = ("eng", "fn", "deps", "dma", "signal", "sem", "semval", "idx", "ring", "ringprev")

    def __init__(self, eng, fn, dma):
        self.eng = eng
        self.fn = fn
        self.deps = []
        self.dma = dma
        self.signal = dma
        self.sem = None
        self.semval = 0
        self.ring = None
        self.ringprev = 0


class Sched:
    ENGS = ("pe", "act", "dve", "pool", "sp")

    def __init__(self, nc, strict_same_engine=True):
        self.nc = nc
        self.ops = {e: [] for e in self.ENGS}
        self.last_w = {}
        self.readers = {}
        self.strict = strict_same_engine
        self.sb_off = 16512
        self.sb_mark = []
        self.scr = {}
        self.n_alloc = 0

    def sb(self, name, shape, dtype):
        esz = 4 if dtype == F32 else 2
        n = 1
        for s in shape[1:]:
            n *= s
        nbytes = (n * esz + 63) // 64 * 64
        off = self.sb_off
        self.sb_off += nbytes
        assert self.sb_off <= 229344, f"SBUF overflow at {name}: {self.sb_off}"
        self.n_alloc += 1
        return self.nc.alloc_sbuf_tensor_at(f"{name}_{self.n_alloc}", list(shape), dtype, offset=off)

    def push(self):
        self.sb_mark.append((self.sb_off, dict(self.scr)))

    def pop(self, barrier=True):
        if barrier:
            self.barrier()
        self.sb_off, self.scr = self.sb_mark.pop()

    def scratch(self, key, shape, dtype):
        k = (key, tuple(shape), str(dtype))
        if k not in self.scr:
            self.scr[k] = self.sb(str(key), shape, dtype)
        return self.scr[k], ("scr",) + k

    mute = False

    def add(self, eng, fn, reads=(), writes=(), dma=False):
        if self.mute:
            return None
        op = Op(eng, fn, dma)
        deps = []
        for t in reads:
            w = self.last_w.get(t)
            if w is not None:
                deps.append((w, True))
        for t in writes:
            w = self.last_w.get(t)
            if w is not None:
                deps.append((w, False))
            rd = self.readers.get(t)
            if rd:
                for r in rd[0].values():
                    deps.append((r, False))
                for r in rd[1]:
                    deps.append((r, False))
        seen = set()
        for d, raw in deps:
            if d is op or id(d) in seen:
                continue
            if d.eng == eng and not d.dma:
                if eng == "pe" or not self.strict or not raw:
                    continue
            seen.add(id(d))
            op.deps.append(d)
            d.signal = True
        for t in reads:
            rd = self.readers.get(t)
            if rd is None:
                rd = self.readers[t] = ({}, [])
            if dma:
                rd[1].append(op)
            else:
                rd[0][eng] = op
        for t in writes:
            self.last_w[t] = op
            self.readers[t] = ({}, [])
        self.ops[eng].append(op)
        return op

    def pe(self, fn, reads=(), writes=()):
        return self.add("pe", fn, reads, writes)

    def act(self, fn, reads=(), writes=()):
        return self.add("act", fn, reads, writes)

    def dve(self, fn, reads=(), writes=()):
        return self.add("dve", fn, reads, writes)

    def pool(self, fn, reads=(), writes=()):
        return self.add("pool", fn, reads, writes)

    def barrier(self):
        lastc = []
        for e in self.ENGS:
            comp = [o for o in self.ops[e] if not o.dma and o.fn is not None]
            if comp:
                lastc.append(comp[-1])
            dm = [o for o in self.ops[e] if o.dma]
            lastc.extend(dm[-DMA_RING:])
        for e in self.ENGS:
            op = Op(e, None, False)
            for d in lastc:
                if d.eng == e and not d.dma and e == "pe":
                    continue
                op.deps.append(d)
                d.signal = True
            self.ops[e].append(op)
        self.last_w = {}
        self.readers = {}

    def dma(self, q, out, in_, reads=(), writes=(), **kw):
        return self.add(q, lambda e: e.dma_start(out=out, in_=in_, **kw), reads, writes, dma=True)

    def emit(self):
        nc = self.nc
        with ExitStack() as es:
            sem_pool = {}

            self.nsem = 0

            def new_sem(name):
                self.nsem += 1
                return es.enter_context(nc.semaphore(name))

            for e in self.ENGS:
                cnt = 0
                cur = None
                k = 0
                rings = None
                allr = []
                nd = 0
                for op in self.ops[e]:
                    if op.dma:
                        if rings is None:
                            rings = [[new_sem(f"d_{e}_{i}"), 0, None] for i in range(DMA_RING)]
                            nsem_d = DMA_RING
                        r = rings[nd % DMA_RING]
                        if r[1] + 16 > SEM_CAP:
                            allr.append(tuple(r))
                            op.deps.append(r[2])
                            r[0] = new_sem(f"d_{e}_{nsem_d}")
                            nsem_d += 1
                            r[1] = 0
                        nd += 1
                        op.ringprev = r[1]
                        r[1] += 16
                        op.sem = r[0]
                        op.semval = r[1]
                        r[2] = op
                    elif op.signal:
                        if cur is None or cnt >= SEM_CAP:
                            cur = new_sem(f"c_{e}_{k}")
                            k += 1
                            cnt = 0
                        cnt += 1
                        op.sem = cur
                        op.semval = cnt
                sem_pool[e] = (list(map(tuple, rings)) + allr) if rings else None

            def run_engine(e, h):
                waited = {}
                for op in self.ops[e]:
                    for d in op.deps:
                        key = id(d.sem)
                        if waited.get(key, 0) >= d.semval:
                            continue
                        h.wait_ge(d.sem, d.semval)
                        waited[key] = d.semval
                    if op.dma and op.ringprev > 0:
                        key = id(op.sem)
                        if waited.get(key, 0) < op.ringprev:
                            h.wait_ge(op.sem, op.ringprev)
                            waited[key] = op.ringprev
                    if op.fn is None:
                        continue
                    ins = op.fn(h)
                    if op.dma:
                        ins.then_inc(op.sem, 16)
                    elif op.signal:
                        ins.then_inc(op.sem, 1)
                rings = sem_pool.get(e)
                if rings:
                    for s, v, _ in rings:
                        if v > 0 and waited.get(id(s), 0) < v:
                            h.wait_ge(s, v)

            with nc.Block() as block:
                @block.tensor
                def _(h):
                    run_engine("pe", h)

                @block.scalar
                def _(h):
                    run_engine("act", h)

                @block.vector
                def _(h):
                    run_engine("dve", h)

                @block.gpsimd
                def _(h):
                    run_engine("pool", h)

                @block.sync
                def _(h):
                    run_engine("sp", h)

    def count(self):
        return {e: len(v) for e, v in self.ops.items()}
import numpy as np
import ml_dtypes

D = 2048
NCH = 16
NTOK = 4096
NCTX = 256
NU = NTOK + NCTX
EPS = 1e-6
SCALE = 128 ** -0.5


class K:
    def __init__(self, nc):
        self.nc = nc
        self.S = Sched(nc)
        self.ps = [nc.alloc_psum_tensor(f"ps{i}", [128, 512], F32) for i in range(8)]
        self.psn = 0
        self.uid = 0

    def psum(self):
        while True:
            i = self.psn % 8
            self.psn += 1
            if i not in getattr(self, "ps_skip", ()):
                return i

    def u(self):
        self.uid += 1
        return self.uid

    def din(self, name, shape, dt=F32):
        return self.nc.dram_tensor(name, list(shape), dt, kind="ExternalInput").ap()

    def dout(self, name, shape, dt=F32):
        return self.nc.dram_tensor(name, list(shape), dt, kind="ExternalOutput").ap()

    def dscr(self, name, shape, dt=F32):
        return self.nc.dram_tensor(name, list(shape), dt, kind="Internal").ap()

    def load_consts(self, ident_d, perm_d):
        S = self.S
        self.ones_bf = S.sb("ones_bf", [128, 128], BF16)
        self.ones_f = S.sb("ones_f", [128, 128], F32)
        self.ident_bf = S.sb("ident_bf", [128, 128], BF16)
        self.ident_f = S.sb("ident_f", [128, 128], F32)
        self.perm_bf = S.sb("perm_bf", [128, 128], BF16)
        S.dve(lambda e: e.memset(self.ones_bf[:], 1.0), writes=["ones_bf"])
        S.dve(lambda e: e.memset(self.ones_f[:], 1.0), writes=["ones_f"])
        S.dma("sp", self.ident_f[:], ident_d, writes=["ident_f"])
        S.dma("pool", self.ident_bf[:], ident_d, writes=["ident_bf"])
        S.dma("pool", self.perm_bf[:], perm_d, writes=["perm_bf"])
        self.cb = {}
        for c in (float(D * EPS), float(128 * EPS), float(EPS), 1.0):
            t = S.sb("cbias", [128, 1], F32)
            S.dve(lambda e, t=t, c=c: e.memset(t[:], c), writes=[("cbias", c)])
            self.cb[c] = t

    def ada(self, cT_d, W_d, b_d, mod_sb, tag):
        S = self.S
        S.push()
        c_sb = S.sb("c", [128, 16, 2], F32)
        sc = S.sb("sc", [128, 16, 2], F32)
        b_sb = S.sb("b", [128, 96], F32)
        wbuf = [S.sb(f"w{i}", [128, 16, 512], F32) for i in range(2)]
        t = tag
        S.dma("sp", c_sb[:], cT_d, writes=[(t, "c")])
        S.dma("sp", b_sb[:], b_d, writes=[(t, "b")])
        S.act(lambda e: e.activation(out=sc[:], in_=c_sb[:], func=AF.Silu), reads=[(t, "c")], writes=[(t, "sc")])
        Wv = W_d.rearrange("(k p) n -> p k n", p=128)
        for blk in range(24):
            wb = wbuf[blk % 2]
            q = "sp" if blk % 2 == 0 else "pool"
            S.dma(q, wb[:], Wv[:, :, blk * 512:(blk + 1) * 512], writes=[("adaw", blk % 2)])
            for cc in range(4):
                c = blk * 4 + cc
                pi = self.psum()
                pt = self.ps[pi]
                for k in range(16):
                    S.pe(lambda e, pt=pt, wb=wb, k=k, cc=cc: e.matmul(pt[:, 0:2], wb[:, k, cc * 128:(cc + 1) * 128], sc[:, k, :],
                                                                      start=(k == 0), stop=(k == 15)),
                         reads=[("adaw", blk % 2), (t, "sc")], writes=[("ps", pi)])
                S.dve(lambda e, pt=pt, c=c: e.tensor_scalar(out=mod_sb[:, c, :], in0=pt[:, 0:2], scalar1=b_sb[:, c:c + 1], scalar2=None, op0=ALU.add),
                      reads=[("ps", pi), (t, "b")], writes=[(t, "mod")])
        S.pop()

    def mod_vectors(self, mod_sb, nmix_d, nffn_d, tag):
        S = self.S
        v = {}
        nm = S.sb("nm", [128, 16], F32)
        nf = S.sb("nf", [128, 16], F32)
        S.dma("sp", nm[:], nmix_d, writes=[(tag, "nm")])
        S.dma("sp", nf[:], nffn_d, writes=[(tag, "nf")])
        for nme, nw, i_shift, i_scale, i_gate in (("mix", nm, 0, 1, 2), ("ffn", nf, 3, 4, 5)):
            A = S.sb("A", [128, 16, 2], F32)
            for j in range(2):
                S.dve(lambda e, A=A, j=j, i_scale=i_scale: e.tensor_scalar(out=A[:, :, j], in0=mod_sb[:, i_scale * 16:(i_scale + 1) * 16, j],
                                                                            scalar1=1.0, scalar2=float(np.sqrt(D)), op0=ALU.add, op1=ALU.mult),
                      reads=[(tag, "mod")], writes=[(tag, "A" + nme)])
                S.dve(lambda e, A=A, j=j, nw=nw: e.tensor_tensor(out=A[:, :, j], in0=A[:, :, j], in1=nw[:], op=ALU.mult),
                      reads=[(tag, "nm"), (tag, "nf"), (tag, "A" + nme)], writes=[(tag, "A" + nme)])
            v["A" + nme] = (A, (tag, "A" + nme))
            v["B" + nme] = (mod_sb[:, i_shift * 16:(i_shift + 1) * 16, :], (tag, "mod"))
            v["G" + nme] = (mod_sb[:, i_gate * 16:(i_gate + 1) * 16, :], (tag, "mod"))
        return v

    def rsqrt(self, out_ap, in_ap, c, reads, wtok):
        S = self.S
        cb = self.cb[c]
        np_ = out_ap.shape[0]
        S.act(lambda e: e.activation(out=out_ap, in_=in_ap, func=AF.Sqrt, bias=cb[0:np_, 0:1]), reads=list(reads) + [("cbias", c)], writes=[wtok])
        S.dve(lambda e: e.reciprocal(out=out_ap, in_=out_ap), reads=[wtok], writes=[wtok])

    def rms_mod(self, x_sb, x_tok, T, segs, A, B, h_sb, h_off, h_tok, extra_out=None, x_off=0):
        S = self.S
        sqs = [S.scratch(("rm_sq", i), [128, T], BF16) for i in range(2)]
        rstd, rstd_t = S.scratch("rm_rstd", [128, T], F32)
        tmps = [S.scratch(("rm_tmp", i), [128, T], F32) for i in range(2)]
        pi = self.psum()
        pt = self.ps[pi]
        for c in range(16):
            s, s_t = sqs[c % 2]
            S.act(lambda e, s=s, c=c: e.activation(out=s[:], in_=x_sb[:, c, x_off:x_off + T], func=AF.Square),
                  reads=[x_tok], writes=[s_t])
            S.pe(lambda e, s=s, c=c: e.matmul(pt[:, 0:T], self.ones_bf[:], s[:], start=(c == 0), stop=(c == 15)),
                 reads=[s_t, "ones_bf"], writes=[("ps", pi)])
        self.rsqrt(rstd[:], pt[:, 0:T], float(D * EPS), [("ps", pi)], rstd_t)
        for c in range(16):
            tm, tm_t = tmps[c % 2]
            S.dve(lambda e, tm=tm, c=c: e.tensor_tensor(out=tm[:], in0=x_sb[:, c, x_off:x_off + T], in1=rstd[:], op=ALU.mult),
                  reads=[x_tok, rstd_t], writes=[tm_t])
            for (c0, c1, j) in segs:
                S.act(lambda e, tm=tm, c=c, c0=c0, c1=c1, j=j: e.activation(out=h_sb[:, c, h_off + c0:h_off + c1], in_=tm[:, c0:c1], func=AF.Identity,
                                                                               bias=B[0][:, c, j:j + 1], scale=A[0][:, c, j:j + 1]),
                      reads=[tm_t, A[1], B[1]], writes=[h_tok])
                if extra_out is not None:
                    extra_out(c, c0, c1, j, tm, tm_t)

    def load_vec(self, d_ap, shape, tok, dt=F32, q="sp"):
        t = self.S.sb("vec", shape, dt)
        self.S.dma(q, t[:], d_ap, writes=[tok])
        return t

    def inproj0(self, xcT_d, w_in_d, mv, qnw_d, knw_d, cos_d, sin_d, o):
        S = self.S
        S.push()
        h_all = S.sb("h_all", [128, 16, NU], BF16)
        qnw = self.load_vec(qnw_d, [128, 1], "qnw")
        knw = self.load_vec(knw_d, [128, 1], "knw")
        S.dve(lambda e: e.tensor_scalar(out=qnw[:], in0=qnw[:], scalar1=float(np.sqrt(128.0)), scalar2=None, op0=ALU.mult), reads=["qnw"], writes=["qnw"])
        S.dve(lambda e: e.tensor_scalar(out=knw[:], in0=knw[:], scalar1=float(np.sqrt(128.0)), scalar2=None, op0=ALU.mult), reads=["knw"], writes=["knw"])
        S.push()
        xs = [S.sb("xs", [128, 16, 256], F32) for _ in range(2)]
        xv = xcT_d.rearrange("(k p) t -> p k t", p=128)
        for i in range(NU // 256):
            x_sb = xs[i % 2]
            S.dma("sp", x_sb[:], xv[:, :, i * 256:(i + 1) * 256], writes=[("xs", i % 2)])
            j = 0 if i < NTOK // 256 else 1
            self.rms_mod(x_sb, ("xs", i % 2), 256, [(0, 256, j)], mv["Amix"], mv["Bmix"], h_all, i * 256, ("h_all", i))
        S.pop()
        wbuf = [S.sb("wb", [128, 16, 512], BF16) for _ in range(2)]
        st = [S.sb("st", [128, 512], BF16) for _ in range(4)]
        sq = S.sb("sq", [128, 512], BF16)
        rstd = S.sb("rstd", [128, 512], F32)
        yn = S.sb("yn", [128, 512], BF16)
        t1 = S.sb("t1", [128, 512], F32)
        t2 = S.sb("t2", [128, 512], F32)
        cs = S.sb("cos", [128, 512], F32)
        sn = S.sb("sin", [128, 512], F32)
        Wv = w_in_d.rearrange("(k p) n -> p k n", p=128)
        tiles = [(i * 512, 512, False) for i in range(8)] + [(NTOK, 256, True)]
        nst = [0]

        def h_toks(t0, T):
            return [("h_all", i) for i in range(t0 // 256, (t0 + T) // 256)]

        def next_st():
            i = nst[0] % 4
            nst[0] += 1
            return i

        def fm_chunk(wb, wtok, cc, t0, T):
            pi = self.psum()
            pt = self.ps[pi]
            for k in range(16):
                S.pe(lambda e, pt=pt, wb=wb, k=k, cc=cc, t0=t0, T=T: e.matmul(pt[:, 0:T], wb[:, k, cc * 128:(cc + 1) * 128], h_all[:, k, t0:t0 + T],
                                                                              start=(k == 0), stop=(k == 15)),
                     reads=[wtok] + h_toks(t0, T), writes=[("ps", pi)])
            return pi

        for blk in range(9):
            wb = wbuf[blk % 2]
            wtok = ("wb", blk % 2)
            S.dma("pool", wb[:], Wv[:, :, blk * 512:(blk + 1) * 512], writes=[wtok])
            if blk in (4, 5) or blk == 8:
                if blk == 8:
                    c0, N, dst, dc0 = 256, 256, o["vg"], 0
                else:
                    c0, N, dst, dc0 = 0, 512, o["vna"], (blk - 4) * 512
                for s in range(NU // 128):
                    pi = self.psum()
                    pt = self.ps[pi]
                    for k in range(16):
                        S.pe(lambda e, pt=pt, wb=wb, k=k, s=s, c0=c0, N=N: e.matmul(pt[:, 0:N], h_all[:, k, s * 128:(s + 1) * 128], wb[:, k, c0:c0 + N],
                                                                                    start=(k == 0), stop=(k == 15)),
                             reads=[wtok, ("h_all", s // 2)], writes=[("ps", pi)])
                    si = next_st()
                    eng = S.act if s % 2 == 0 else S.dve
                    if s % 2 == 0:
                        S.act(lambda e, pt=pt, si=si, N=N: e.copy(out=st[si][:, 0:N], in_=pt[:, 0:N]), reads=[("ps", pi)], writes=[("st", si)])
                    else:
                        S.dve(lambda e, pt=pt, si=si, N=N: e.tensor_copy(out=st[si][:, 0:N], in_=pt[:, 0:N]), reads=[("ps", pi)], writes=[("st", si)])
                    S.dma("sp", dst[s * 128:(s + 1) * 128, dc0:dc0 + N], st[si][:, 0:N], reads=[("st", si)])
            if blk in (0, 1, 2, 3):
                for (t0, T, isc) in tiles:
                    for cc in range(4):
                        pi = fm_chunk(wb, wtok, cc, t0, T)
                        pt = self.ps[pi]
                        si = next_st()
                        hh = (blk % 2) * 4 + cc
                        if blk < 2:
                            S.act(lambda e, pt=pt, si=si, T=T: e.mul(out=st[si][:, 0:T], in_=pt[:, 0:T], mul=float(SCALE)), reads=[("ps", pi)], writes=[("st", si)])
                            dst = o["qna"]
                        else:
                            S.dve(lambda e, pt=pt, si=si, T=T: e.tensor_copy(out=st[si][:, 0:T], in_=pt[:, 0:T]), reads=[("ps", pi)], writes=[("st", si)])
                            dst = o["kna"]
                        S.dma("sp", dst[hh, :, t0:t0 + T], st[si][:, 0:T], reads=[("st", si)])
            if blk in (6, 7, 8):
                ncc = 4 if blk < 8 else 2
                for (t0, T, isc) in tiles:
                    if not isc:
                        S.dma("sp", cs[:, 0:T], cos_d[:, t0:t0 + T], writes=["cos"])
                        S.dma("sp", sn[:, 0:T], sin_d[:, t0:t0 + T], writes=["sin"])
                    for cc in range(ncc):
                        pi = fm_chunk(wb, wtok, cc, t0, T)
                        pt = self.ps[pi]
                        si = next_st()
                        if blk < 8:
                            hh, dst, nw, nwt = (blk - 6) * 4 + cc, o["qg"], qnw, "qnw"
                        else:
                            hh, dst, nw, nwt = cc, o["kg"], knw, "knw"
                        S.act(lambda e, pt=pt, T=T: e.activation(out=sq[:, 0:T], in_=pt[:, 0:T], func=AF.Square), reads=[("ps", pi)], writes=["sq_r"])
                        p2 = self.psum()
                        pt2 = self.ps[p2]
                        S.pe(lambda e, pt2=pt2, T=T: e.matmul(pt2[:, 0:T], self.ones_bf[:], sq[:, 0:T], start=True, stop=True),
                             reads=["sq_r", "ones_bf"], writes=[("ps", p2)])
                        self.rsqrt(rstd[:, 0:T], pt2[:, 0:T], float(128 * EPS), [("ps", p2)], "rstd_r")
                        ydst = st[si] if isc else yn
                        ytok = ("st", si) if isc else "yn"
                        S.dve(lambda e, pt=pt, T=T, ydst=ydst, nw=nw: e.scalar_tensor_tensor(out=ydst[:, 0:T], in0=pt[:, 0:T], scalar=nw[:, 0:1], in1=rstd[:, 0:T],
                                                                                             op0=ALU.mult, op1=ALU.mult),
                              reads=[("ps", pi), "rstd_r", nwt], writes=[ytok])
                        if not isc:
                            p3 = self.psum()
                            pt3 = self.ps[p3]
                            S.pe(lambda e, pt3=pt3, T=T: e.matmul(pt3[:, 0:T], self.perm_bf[:], yn[:, 0:T], start=True, stop=True),
                                 reads=["yn", "perm_bf"], writes=[("ps", p3)])
                            S.pool(lambda e, T=T: e.tensor_tensor(out=t1[:, 0:T], in0=yn[:, 0:T], in1=cs[:, 0:T], op=ALU.mult), reads=["yn", "cos"], writes=["t1"])
                            S.dve(lambda e, pt3=pt3, T=T: e.tensor_tensor(out=t2[:, 0:T], in0=pt3[:, 0:T], in1=sn[:, 0:T], op=ALU.mult), reads=[("ps", p3), "sin"], writes=["t2"])
                            S.pool(lambda e, si=si, T=T: e.tensor_tensor(out=st[si][:, 0:T], in0=t1[:, 0:T], in1=t2[:, 0:T], op=ALU.add), reads=["t1", "t2"], writes=[("st", si)])
                        S.dma("sp", dst[hh, :, t0:t0 + T], st[si][:, 0:T], reads=[("st", si)])
        S.pop()

    def psum_pool(self, pool, cnt):
        i = pool[cnt[0] % len(pool)]
        cnt[0] += 1
        return i

    def attn_gqa(self, qg_d, kg_d, vg_d, attnT_d):
        S = self.S
        S.push()
        KT = [S.sb("KT", [128, NU], BF16) for _ in range(2)]
        V = [S.sb("V", [128, NU // 128, 128], BF16) for _ in range(2)]
        QT = [S.sb("QT", [128, NU], BF16) for _ in range(2)]
        P = [S.sb("P", [128, 512], BF16) for _ in range(3)]
        rinv = S.sb("rinv", [128, 512], F32)
        st = [S.sb("st", [128, 512], BF16) for _ in range(2)]
        c_o, c_m, c_s, c_p, c_st = [0], [0], [0], [0], [0]
        qtiles = [(i * 512, 512, list(range(NU // 128))) for i in range(8)] + [(NTOK, 256, [32, 33])]
        for g in range(2):
            kt_sb, v_sb = KT[g % 2], V[g % 2]
            S.dma("sp", kt_sb[:], kg_d[g], writes=[("KT", g % 2)])
            S.dma("sp", v_sb[:], vg_d[:, g * 128:(g + 1) * 128].rearrange("(n p) d -> p n d", p=128), writes=[("V", g % 2)])
            for hq in range(4):
                h = g * 4 + hq
                q_sb = QT[h % 2]
                S.dma("sp", q_sb[:], qg_d[h], writes=[("QT", h % 2)])
                for (t0, T, ktl) in qtiles:
                    po = self.psum_pool([0, 1], c_o)
                    pm = self.psum_pool([2, 3], c_m)
                    for n, kt in enumerate(ktl):
                        pss = self.psum_pool([4, 5, 6, 7], c_s)
                        pi_ = c_p[0] % 3
                        c_p[0] += 1
                        S.pe(lambda e, pss=pss, kt=kt, t0=t0, T=T, kt_sb=kt_sb, q_sb=q_sb: e.matmul(self.ps[pss][:, 0:T], kt_sb[:, kt * 128:(kt + 1) * 128], q_sb[:, t0:t0 + T], start=True, stop=True),
                             reads=[("KT", g % 2), ("QT", h % 2)], writes=[("ps", pss)])
                        S.act(lambda e, pss=pss, pi_=pi_, T=T: e.activation(out=P[pi_][:, 0:T], in_=self.ps[pss][:, 0:T], func=AF.Exp, scale=float(SCALE)),
                              reads=[("ps", pss)], writes=[("P", pi_)])
                        S.pe(lambda e, po=po, kt=kt, pi_=pi_, T=T, n=n, v_sb=v_sb, L=len(ktl): e.matmul(self.ps[po][:, 0:T], v_sb[:, kt, :], P[pi_][:, 0:T], start=(n == 0), stop=(n == L - 1)),
                             reads=[("V", g % 2), ("P", pi_)], writes=[("ps", po)])
                        S.pe(lambda e, pm=pm, pi_=pi_, T=T, n=n, L=len(ktl): e.matmul(self.ps[pm][:, 0:T], self.ones_bf[:], P[pi_][:, 0:T], start=(n == 0), stop=(n == L - 1)),
                             reads=[("P", pi_)], writes=[("ps", pm)])
                    si = c_st[0] % 2
                    c_st[0] += 1
                    S.dve(lambda e, pm=pm, T=T: e.reciprocal(out=rinv[:, 0:T], in_=self.ps[pm][:, 0:T]), reads=[("ps", pm)], writes=["rinv"])
                    S.dve(lambda e, po=po, si=si, T=T: e.tensor_tensor(out=st[si][:, 0:T], in0=self.ps[po][:, 0:T], in1=rinv[:, 0:T], op=ALU.mult),
                          reads=[("ps", po), "rinv"], writes=[("st", si)])
                    S.dma("pool", attnT_d[(8 + h) * 128:(9 + h) * 128, t0:t0 + T], st[si][:, 0:T], reads=[("st", si)])
        S.pop()

    def attn_na(self, qna_d, kna_d, vna_d, bias_d, attnT_d):
        S = self.S
        S.push()
        KT = [S.sb("KT", [128, NU], BF16) for _ in range(2)]
        V = [S.sb("V", [128, NU // 128, 128], BF16) for _ in range(2)]
        QT = [S.sb("QT", [128, NU], BF16) for _ in range(2)]
        bias = S.sb("bias", [128, 5, 8, 5, 128], BF16)
        S.dma("sp", bias[:], bias_d, writes=["bias"])
        P1 = [S.sb("P1", [128, 512], BF16) for _ in range(2)]
        P2 = [S.sb("P2", [128, 384], BF16) for _ in range(2)]
        rinv = S.sb("rinv", [128, 128], F32)
        st = [S.sb("st", [128, 512], BF16) for _ in range(2)]
        c_o, c_m, c_s, c_p = [0], [0], [0], [0]
        for h in range(8):
            kt_sb, v_sb, q_sb = KT[h % 2], V[h % 2], QT[h % 2]
            S.dma("sp", kt_sb[:], kna_d[h], writes=[("KT", h % 2)])
            S.dma("sp", v_sb[:], vna_d[:, h * 128:(h + 1) * 128].rearrange("(n p) d -> p n d", p=128), writes=[("V", h % 2)])
            S.dma("sp", q_sb[:], qna_d[h], writes=[("QT", h % 2)])
            rd = [("KT", h % 2), ("QT", h % 2)]
            for lb in range(34):
                isc = lb >= 32
                if not isc:
                    typ = {0: 0, 1: 1, 30: 3, 31: 4}.get(lb, 2)
                    tile0 = min(max(lb - 2, 0), 27)
                    keys = [(tile0 + j, j) for j in range(5)] + [(32, None), (33, None)]
                else:
                    keys = [(32, None), (33, None)]
                s1 = self.psum_pool([4, 5], c_s)
                s2 = self.psum_pool([6, 7], c_s)
                pi_ = c_p[0] % 2
                c_p[0] += 1
                q0 = lb * 128
                slots = []
                for n, (kt, bj) in enumerate(keys):
                    if isc:
                        bank, col = s1, n * 128
                    else:
                        bank, col = (s1, n * 128) if n < 4 else (s2, (n - 4) * 128)
                    slots.append((bank, col))
                    S.pe(lambda e, bank=bank, col=col, kt=kt, q0=q0, bj=bj, kt_sb=kt_sb, q_sb=q_sb: e.matmul(self.ps[bank][:, col:col + 128], kt_sb[:, kt * 128:(kt + 1) * 128], q_sb[:, q0:q0 + 128],
                                                                                                              start=True, stop=(bj is None)),
                         reads=rd, writes=[("ps", bank)])
                    if bj is not None:
                        S.pe(lambda e, bank=bank, col=col, typ=typ, bj=bj, h=h: e.matmul(self.ps[bank][:, col:col + 128], self.ident_bf[:], bias[:, typ, h, bj, :], start=False, stop=True),
                             reads=["bias", "ident_bf"], writes=[("ps", bank)])
                if isc:
                    S.act(lambda e, s1=s1, pi_=pi_: e.activation(out=P1[pi_][:, 0:256], in_=self.ps[s1][:, 0:256], func=AF.Exp), reads=[("ps", s1)], writes=[("P1", pi_)])
                else:
                    S.act(lambda e, s1=s1, pi_=pi_: e.activation(out=P1[pi_][:], in_=self.ps[s1][:, 0:512], func=AF.Exp), reads=[("ps", s1)], writes=[("P1", pi_)])
                    S.act(lambda e, s2=s2, pi_=pi_: e.activation(out=P2[pi_][:], in_=self.ps[s2][:, 0:384], func=AF.Exp), reads=[("ps", s2)], writes=[("P2", pi_)])
                po = self.psum_pool([0, 1], c_o)
                pm = self.psum_pool([2, 3], c_m)
                L = len(keys)
                for n, (kt, bj) in enumerate(keys):
                    if isc or n < 4:
                        pap, ptok = P1[pi_][:, n * 128:(n + 1) * 128], ("P1", pi_)
                    else:
                        pap, ptok = P2[pi_][:, (n - 4) * 128:(n - 3) * 128], ("P2", pi_)
                    S.pe(lambda e, po=po, kt=kt, pap=pap, n=n, L=L, v_sb=v_sb: e.matmul(self.ps[po][:, 0:128], v_sb[:, kt, :], pap, start=(n == 0), stop=(n == L - 1)),
                         reads=[("V", h % 2), ptok], writes=[("ps", po)])
                    S.pe(lambda e, pm=pm, pap=pap, n=n, L=L: e.matmul(self.ps[pm][:, 0:128], self.ones_bf[:], pap, start=(n == 0), stop=(n == L - 1)),
                         reads=[ptok], writes=[("ps", pm)])
                si = (lb // 4) % 2
                cc = (lb % 4) * 128
                S.dve(lambda e, pm=pm: e.reciprocal(out=rinv[:], in_=self.ps[pm][:, 0:128]), reads=[("ps", pm)], writes=["rinv"])
                S.dve(lambda e, po=po, si=si, cc=cc: e.tensor_tensor(out=st[si][:, cc:cc + 128], in0=self.ps[po][:, 0:128], in1=rinv[:], op=ALU.mult),
                      reads=[("ps", po), "rinv"], writes=[("st", si)])
                if lb % 4 == 3:
                    S.dma("pool", attnT_d[h * 128:(h + 1) * 128, (lb - 3) * 128:(lb + 1) * 128], st[si][:], reads=[("st", si)])
                elif lb == 33:
                    S.dma("pool", attnT_d[h * 128:(h + 1) * 128, 32 * 128:34 * 128], st[si][:, 0:256], reads=[("st", si)])
        S.pop()

    def linear_fm(self, w_d, ncols, h_sb, h_tok, T, evac, wq="pool", blk=512, wkey="lw"):
        S = self.S
        Wv = w_d.rearrange("(k p) n -> p k n", p=128)
        nblk = (ncols + blk - 1) // blk
        wbs = [S.scratch((wkey, i), [128, 16, blk], BF16) for i in range(2)]
        for b in range(nblk):
            wb, wtok = wbs[b % 2]
            c0 = b * blk
            bw = min(blk, ncols - c0)
            S.dma(wq, wb[:, :, 0:bw], Wv[:, :, c0:c0 + bw], writes=[wtok])
            for cc in range((bw + 127) // 128):
                M = min(128, bw - cc * 128)
                for ho in range(0, T, 512):
                    Th = min(512, T - ho)
                    pi = self.psum()
                    pt = self.ps[pi]
                    for k in range(16):
                        S.pe(lambda e, pt=pt, wb=wb, k=k, cc=cc, M=M, ho=ho, Th=Th: e.matmul(pt[0:M, 0:Th], wb[:, k, cc * 128:cc * 128 + M], h_sb[:, k, ho:ho + Th],
                                                                                               start=(k == 0), stop=(k == 15)),
                             reads=[wtok, h_tok], writes=[("ps", pi)])
                    evac(c0 // 128 + cc, M, ho, Th, pi)

    def mlp0(self, xcT_d, attnT_d, w_out_d, mv0, wg_d, wu_d, wd_d, mv1, w_in1_d, x1T_d, p1T_d, tiles=None):
        S = self.S
        FF = 5632
        FB = 256
        xv = xcT_d.rearrange("(k p) t -> p k t", p=128)
        av = attnT_d.rearrange("(k p) t -> p k t", p=128)
        x1v = x1T_d.rearrange("(k p) t -> p k t", p=128)
        if tiles is None:
            tiles = [(i * 1024, 1024, 0) for i in range(3)] + [(3072, 1280, 0)]
        S.push()
        xt = S.sb("xt", [128, 16, 1280], F32)
        hb = S.sb("hb", [128, 16, 1280], BF16)
        for (t0, T, j) in tiles:
            S.dma("sp", xt[:, :, 0:T], xv[:, :, t0:t0 + T], writes=["xt"])
            S.dma("sp", hb[:, :, 0:T], av[:, :, t0:t0 + T], writes=["hb"])

            def make_res_evac(G, j=j, t0=t0):
                def ev(c, M, ho, Th, pi):
                    j = 1 if t0 + ho >= NTOK else 0
                    S.dve(lambda e: e.scalar_tensor_tensor(out=xt[:, c, ho:ho + Th], in0=self.ps[pi][:, 0:Th], scalar=G[0][:, c, j:j + 1], in1=xt[:, c, ho:ho + Th],
                                                           op0=ALU.mult, op1=ALU.add),
                          reads=[("ps", pi), G[1], "xt"], writes=["xt"])
                return ev
            self.linear_fm(w_out_d, 2048, hb, "hb", T, make_res_evac(mv0["Gmix"]), wkey="wg", blk=256)
            for ho in range(0, T, 512):
                Th = min(512, T - ho)
                self.rms_mod(xt, "xt", Th, [(0, Th, 1 if t0 + ho >= NTOK else 0)], mv0["Affn"], mv0["Bffn"], hb, ho, "hb", x_off=ho)
            self.ffn_blocks(hb, "hb", T, wg_d, wu_d, wd_d, make_res_evac(mv0["Gffn"]))
            S.dma("sp", x1v[:, :, t0:t0 + T], xt[:, :, 0:T], reads=["xt"])
            for ho in range(0, T, 512):
                Th = min(512, T - ho)
                self.rms_mod(xt, "xt", Th, [(0, Th, 1 if t0 + ho >= NTOK else 0)], mv1["Amix"], mv1["Bmix"], hb, ho, "hb", x_off=ho)
            sts = [S.scratch(("pst", i), [128, 512], F32) for i in range(2)]
            nst = [0]

            def ev_p1(c, M, ho, Th, pi):
                st, st_t = sts[nst[0] % 2]
                nst[0] += 1
                if nst[0] % 2 == 0:
                    S.act(lambda e: e.copy(out=st[0:M, 0:Th], in_=self.ps[pi][0:M, 0:Th]), reads=[("ps", pi)], writes=[st_t])
                else:
                    S.dve(lambda e: e.tensor_copy(out=st[0:M, 0:Th], in_=self.ps[pi][0:M, 0:Th]), reads=[("ps", pi)], writes=[st_t])
                S.dma("sp", p1T_d[c * 128:c * 128 + M, t0 + ho:t0 + ho + Th], st[0:M, 0:Th], reads=[st_t])
            self.linear_fm(w_in1_d, 8256, hb, "hb", T, ev_p1, wkey="wg", blk=256)
        S.pop()

    def gdn_consts(self, convw_d, gparam_d, masks_d, sel_d):
        S = self.S
        self.g_convw = self.load_vec(convw_d, [128, 48, 4], "g_convw")
        self.g_param = self.load_vec(gparam_d, [128, 4, 16], "g_param")
        self.g_masks = self.load_vec(masks_d, [128, 4, 128], "g_masks")
        self.g_sel = self.load_vec(sel_d, [128, 6, 128], "g_sel")
        self.g_rowm = S.sb("rowm", [128, 2], F32)
        S.dve(lambda e: e.memset(self.g_rowm[:], 0.0), writes=["g_rowm"])
        S.dve(lambda e: e.memset(self.g_rowm[0:64, 0:1], 1.0), writes=["g_rowm"])
        S.dve(lambda e: e.memset(self.g_rowm[64:128, 1:2], 1.0), writes=["g_rowm"])
        self.g_negA = S.sb("negA", [128, 2, 16], F32)
        S.act(lambda e: e.activation(out=self.g_negA[:], in_=self.g_param[:, 0:2, :], func=AF.Exp), reads=["g_param"], writes=["g_negA"])
        S.dve(lambda e: e.tensor_scalar(out=self.g_negA[:], in0=self.g_negA[:], scalar1=-1.0, scalar2=None, op0=ALU.mult), reads=["g_negA"], writes=["g_negA"])

    def gdn_pass(self, p1T_d, d, o_d, units=None, state_out_d=None):
        S = self.S
        S.push()
        pv = p1T_d[0:6144, :].rearrange("(c p) t -> p c t", p=128)
        ident_f, ones_f = self.ident_f, self.ones_f

        def bc_h(ap2):
            n = ap2.shape[1]
            return ap2.rearrange("p (h o) -> p h o", o=1).broadcast_to([128, n, 128])

        def bc_m(ap2, n=4):
            return ap2.rearrange("p (o f) -> p o f", o=1).broadcast_to([128, n, 128])

        MLk, MUk, SLk, SUk = (self.g_masks[:, i, :] for i in range(4))
        if d == 0:
            tri, strict, inclT, selL, selC = MUk, SLk, MUk, self.g_sel[:, 0, :], (self.g_sel[:, 2, :], self.g_sel[:, 3, :])
            order = (0, 1)
        else:
            tri, strict, inclT, selL, selC = MLk, SUk, MLk, self.g_sel[:, 1, :], (self.g_sel[:, 4, :], self.g_sel[:, 5, :])
            order = (1, 0)
        if units is None:
            cu = [(NTOK + 128 * i, NTOK, NU, False) for i in range(2)]
            xu = [(128 * i, 0, NTOK, True) for i in range(32)]
            units = cu + xu if d == 0 else cu[::-1] + xu[::-1]
        St = S.sb("St", [128, 16, 128], F32)
        Sb = S.sb("Sb", [128, 16, 128], BF16)
        S.dve(lambda e: e.memset(St[:], 0.0), writes=[("St", hg) for hg in range(4)])
        S.dve(lambda e: e.memset(Sb[:], 0.0), writes=[("Sb", hg) for hg in range(4)])
        vn = [S.sb("vn", [128, 4, 128], BF16) for _ in range(4)]
        for i in range(4):
            S.dve(lambda e, i=i: e.memset(vn[i][:], 0.0), writes=[("vn", i)])
        P = [S.sb("P", [128, 48, 131], F32) for _ in range(1)]
        Y = S.sb("Y", [128, 48, 128], F32)
        ctmp2 = [S.sb("ctmp", [128, 6, 128], F32) for _ in range(2)]
        qT = S.sb("qT", [128, 16, 128], BF16)
        kT = S.sb("kT", [128, 16, 128], BF16)
        k_tm = S.sb("k_tm", [128, 16, 128], BF16)
        v_tm = S.sb("v_tm", [128, 16, 128], BF16)
        sq4 = S.sb("sq4", [128, 512], BF16)
        rstd4 = S.sb("rstd4", [128, 512], F32)
        gbT = S.sb("gbT", [64, 128], F32)
        beta = S.sb("beta", [128, 16], F32)
        xa = S.sb("xa", [128, 16], F32)
        g = S.sb("g", [128, 16], F32)
        gc = S.sb("gc", [128, 16], F32)
        kdsc = S.sb("kdsc", [128, 16], F32)
        kdsc01 = S.sb("kdsc01", [128, 2, 16], F32)
        scw = S.sb("scw", [128, 16], F32)
        egl = S.sb("egl", [128, 2, 16], F32)
        o_sb = [S.sb("o_sb", [128, 16, 128], F32) for _ in range(1)]
        FS = [[S.sb("fs", [128, 4, 128], F32) for _ in range(8)] for _ in range(4)]
        BS = [[S.sb("bs", [128, 4, 128], BF16) for _ in range(8)] for _ in range(4)]
        nunit = 0
        import os
        stop = int(os.environ.get('GDN_STOP', '99'))
        sub = int(os.environ.get('GDN_SUB', '99'))
        for (t0, slo, shi, has_o) in units:
            Pb = P[0]
            ptok = ("P", 0)
            lo, hi = max(t0 - 2, slo), min(t0 + 129, shi)
            if lo > t0 - 2 or hi < t0 + 129:
                S.pool(lambda e, Pb=Pb: e.memset(Pb[:], 0.0), writes=[ptok])
            for cg in range(6):
                S.dma("sp", Pb[:, cg * 8:(cg + 1) * 8, lo - (t0 - 2):hi - (t0 - 2)], pv[:, cg * 8:(cg + 1) * 8, lo:hi], writes=[ptok])
            S.dma("sp", gbT[:], p1T_d[8192:8256, t0:t0 + 128], writes=["gbT"])
            cw = self.g_convw
            for c in range(48):
                S.act(lambda e, Pb=Pb, c=c: e.activation(out=Y[:, c, :], in_=Pb[:, c, 0:128], func=AF.Copy, scale=cw[:, c, 0:1]), reads=[ptok, "g_convw"], writes=[("Y", c // 6), "Y"] + [("Yk", q) for q in range(4, 8)])
            nprod = 0
            for cq in range(8):
                cs_ = slice(cq * 6, (cq + 1) * 6)
                for i in range(1, 4):
                    ct, ctt = ctmp2[nprod % 2], ("ctmp", nprod % 2)
                    eng = S.dve if nprod % 4 == 3 else S.pool
                    eng(lambda e, Pb=Pb, i=i, cs_=cs_, ct=ct: e.tensor_tensor(out=ct[:], in0=Pb[:, cs_, i:i + 128], in1=bc_h(cw[:, cs_, i]), op=ALU.mult), reads=[ptok, "g_convw"], writes=[ctt])
                    S.dve(lambda e, cs_=cs_, ct=ct: e.tensor_tensor(out=Y[:, cs_, :], in0=Y[:, cs_, :], in1=ct[:], op=ALU.add), reads=[("Y", cq), ctt], writes=[("Y", cq)])
                    nprod += 1
            YT = [("Y", i) for i in range(8)]
            S.act(lambda e: e.activation(out=Y[:], in_=Y[:], func=AF.Silu), reads=YT, writes=["Y"])
            if stop <= 1:
                nunit += 1
                continue
            for c4 in range(8):
                ysl = Y[:, c4 * 4:(c4 + 1) * 4, :]
                if c4 % 2 == 0:
                    sqb, sq_t, rsb, rs_t = sq4, "sq4", rstd4, "rstd4"
                else:
                    sqb, sq_t = BS[3][1][:].rearrange("p h t -> p (h t)"), ("Ybf", 3)
                    rsb, rs_t = FS[3][7][:].rearrange("p h t -> p (h t)"), ("H", 3)
                S.act(lambda e, ysl=ysl, sqb=sqb: e.activation(out=sqb[:].rearrange("p (h t) -> p h t", h=4), in_=ysl, func=AF.Square), reads=["Y"], writes=[sq_t])
                pi = self.psum()
                S.pe(lambda e, pi=pi, sqb=sqb: e.matmul(self.ps[pi][:, :], self.ones_bf[:], sqb[:], start=True, stop=True), reads=[sq_t], writes=[("ps", pi)])
                self.rsqrt(rsb[:], self.ps[pi][:, :], float(EPS), [("ps", pi)], rs_t)
                r4 = rsb[:].rearrange("p (h t) -> p h t", h=4)
                if c4 < 4:
                    S.dve(lambda e, ysl=ysl, c4=c4, r4=r4: e.scalar_tensor_tensor(out=qT[:, c4 * 4:(c4 + 1) * 4, :], in0=ysl, scalar=float(SCALE), in1=r4, op0=ALU.mult, op1=ALU.mult),
                          reads=["Y", rs_t], writes=["qT"])
                else:
                    S.dve(lambda e, ysl=ysl, r4=r4: e.tensor_tensor(out=ysl, in0=ysl, in1=r4, op=ALU.mult), reads=["Y", rs_t], writes=[("Yk", c4)])
                    S.act(lambda e, ysl=ysl, c4=c4: e.copy(out=kT[:, (c4 - 4) * 4:(c4 - 3) * 4, :], in_=ysl), reads=[("Yk", c4)], writes=["kT"])
            for which, base, dst, dtok in (("k", 16, k_tm, "k_tm"), ("v", 32, v_tm, "v_tm")):
                for h4 in range(4):
                    pi = self.psum()
                    for i in range(4):
                        S.pe(lambda e, pi=pi, i=i, base=base, h4=h4: e.matmul(self.ps[pi][:, i * 128:(i + 1) * 128], Y[:, base + h4 * 4 + i, :], ident_f[:], start=True, stop=True),
                             reads=["Y"] + [("Yk", c) for c in range(4, 8)], writes=[("ps", pi)])
                    S.act(lambda e, pi=pi, dst=dst, h4=h4: e.copy(out=dst[:, h4 * 4:(h4 + 1) * 4, :], in_=self.ps[pi][:, :].rearrange("p (h t) -> p h t", h=4)),
                          reads=[("ps", pi)], writes=[dtok])
            if stop <= 3:
                nunit += 1
                continue
            pg = self.psum()
            S.pe(lambda e, pg=pg: e.matmul(self.ps[pg][:, 0:64], gbT[:], ident_f[0:64, 0:64], start=True, stop=True), reads=["gbT"], writes=[("ps", pg)])
            S.act(lambda e, pg=pg: e.activation(out=beta[:], in_=self.ps[pg][:, 16 * d:16 * d + 16], func=AF.Sigmoid), reads=[("ps", pg)], writes=["beta"])
            S.dve(lambda e, pg=pg: e.tensor_tensor(out=xa[:], in0=self.ps[pg][:, 32 + 16 * d:48 + 16 * d], in1=self.g_param[:, 2 + d, :], op=ALU.add), reads=[("ps", pg), "g_param"], writes=["xa"])
            S.act(lambda e: e.activation(out=xa[:], in_=xa[:], func=AF.Exp), reads=["xa"], writes=["xa"])
            S.act(lambda e: e.activation(out=xa[:], in_=xa[:], func=AF.Ln, bias=self.cb[1.0][:, 0:1]), reads=["xa"], writes=["xa"])
            S.dve(lambda e: e.tensor_tensor(out=g[:], in0=xa[:], in1=self.g_negA[:, d, :], op=ALU.mult), reads=["xa", "g_negA"], writes=["g"])
            p1_ = self.psum()
            S.pe(lambda e, p1_=p1_: e.matmul(self.ps[p1_][:, 0:16], tri, g[:], start=True, stop=True), reads=["g", "g_masks"], writes=[("ps", p1_)])
            S.dve(lambda e, p1_=p1_: e.tensor_copy(out=gc[:], in_=self.ps[p1_][:, 0:16]), reads=[("ps", p1_)], writes=["gc"])
            p2_ = self.psum()
            S.pe(lambda e, p2_=p2_: e.matmul(self.ps[p2_][:, 0:16], selL, gc[:], start=True, stop=True), reads=["gc", "g_sel"], writes=[("ps", p2_)])
            S.dve(lambda e, p2_=p2_: e.tensor_tensor(out=kdsc[:], in0=self.ps[p2_][:, 0:16], in1=gc[:], op=ALU.subtract), reads=[("ps", p2_), "gc"], writes=["kdsc"])
            S.act(lambda e: e.activation(out=kdsc[:], in_=kdsc[:], func=AF.Exp), reads=["kdsc"], writes=["kdsc"])
            S.act(lambda e: e.activation(out=scw[:], in_=gc[:], func=AF.Exp), reads=["gc"], writes=["scw"])
            S.dve(lambda e: e.tensor_tensor(out=scw[:], in0=scw[:], in1=beta[:], op=ALU.mult), reads=["scw", "beta"], writes=["scw"])
            p3_ = self.psum()
            for ci in range(2):
                S.pe(lambda e, p3_=p3_, ci=ci: e.matmul(self.ps[p3_][:, ci * 16:(ci + 1) * 16], selC[ci], gc[:], start=True, stop=True), reads=["gc", "g_sel"], writes=[("ps", p3_)])
            S.act(lambda e, p3_=p3_: e.activation(out=egl[:].rearrange("p c h -> p (c h)"), in_=self.ps[p3_][:, 0:32], func=AF.Exp), reads=[("ps", p3_)], writes=["egl"])
            if stop <= 4:
                nunit += 1
                continue
            ob = o_sb[0]
            otok = ("o_sb", 0)
            HG = range(4)
            fl = lambda t: t[:].rearrange("p h t -> p (h t)")
            HS = [slice(hg * 4, hg * 4 + 4) for hg in HG]
            tk = lambda nm, hg: (nm, hg)
            for ci in range(2):
                S.dve(lambda e, ci=ci: e.tensor_scalar(out=kdsc01[:, ci, :], in0=kdsc[:], scalar1=self.g_rowm[:, ci:ci + 1], scalar2=None, op0=ALU.mult), reads=["kdsc", "g_rowm"], writes=["kdsc01"])
            for hg in HG:
                vb4, kbg4, kdm0, kdm1 = BS[hg][2], BS[hg][3], BS[hg][4], BS[hg][5]
                S.pool(lambda e, hg=hg, vb4=vb4: e.tensor_tensor(out=vb4[:], in0=v_tm[:, HS[hg], :], in1=bc_h(beta[:, HS[hg]]), op=ALU.mult), reads=["v_tm", "beta"], writes=[tk("vb4", hg)])
                S.pool(lambda e, hg=hg, kbg4=kbg4: e.tensor_tensor(out=kbg4[:], in0=k_tm[:, HS[hg], :], in1=bc_h(scw[:, HS[hg]]), op=ALU.mult), reads=["k_tm", "scw"], writes=[tk("kbg4", hg)])
                S.pool(lambda e, hg=hg, kdm0=kdm0: e.tensor_tensor(out=kdm0[:], in0=k_tm[:, HS[hg], :], in1=bc_h(kdsc01[:, 0, HS[hg]]), op=ALU.mult), reads=["k_tm", "kdsc01"], writes=[tk("kdm0", hg)])
                S.pool(lambda e, hg=hg, kdm1=kdm1: e.tensor_tensor(out=kdm1[:], in0=k_tm[:, HS[hg], :], in1=bc_h(kdsc01[:, 1, HS[hg]]), op=ALU.mult), reads=["k_tm", "kdsc01"], writes=[tk("kdm1", hg)])
            if stop == 11:
                S.mute = True
            pbs = {}
            for hg in HG:
                Dd = FS[hg][3]
                S.dve(lambda e, hg=hg, Dd=Dd: e.tensor_tensor(out=Dd[:], in0=bc_m(ident_f[:]), in1=bc_h(gc[:, HS[hg]]), op=ALU.mult), reads=["gc"], writes=[tk("D", hg)])
                pb = self.psum()
                pbs[hg] = pb
                S.pe(lambda e, pb=pb, Dd=Dd: e.matmul(self.ps[pb][:, :], ones_f[:], fl(Dd), start=True, stop=True), reads=[tk("D", hg)], writes=[("ps", pb)])
            for hg in HG:
                A_, Dd, pb = FS[hg][0], FS[hg][3], pbs[hg]
                S.act(lambda e, pb=pb, A_=A_: e.copy(out=fl(A_), in_=self.ps[pb][:, :]), reads=[("ps", pb)], writes=[tk("A", hg)])
                S.act(lambda e, pb=pb, Dd=Dd: e.activation(out=fl(Dd), in_=self.ps[pb][:, :], func=AF.Exp), reads=[("ps", pb)], writes=[tk("D", hg)])
            for hg in HG:
                A_, B_, C_ = FS[hg][0], FS[hg][1], FS[hg][2]
                S.dve(lambda e, hg=hg, A_=A_: e.tensor_tensor(out=A_[:], in0=A_[:], in1=bc_h(gc[:, HS[hg]]), op=ALU.subtract), reads=[tk("A", hg), "gc"], writes=[tk("A", hg)])
                S.dve(lambda e, A_=A_, B_=B_: e.tensor_scalar(out=B_[:], in0=A_[:], scalar1=0.0, scalar2=None, op0=ALU.max), reads=[tk("A", hg)], writes=[tk("B", hg)])
                S.dve(lambda e, A_=A_, C_=C_: e.tensor_scalar(out=C_[:], in0=A_[:], scalar1=0.0, scalar2=None, op0=ALU.min), reads=[tk("A", hg)], writes=[tk("C", hg)])
            for hg in HG:
                B_, C_ = FS[hg][1], FS[hg][2]
                S.act(lambda e, B_=B_: e.activation(out=B_[:], in_=B_[:], func=AF.Exp, scale=-1.0), reads=[tk("B", hg)], writes=[tk("B", hg)])
                S.act(lambda e, C_=C_: e.activation(out=C_[:], in_=C_[:], func=AF.Exp), reads=[tk("C", hg)], writes=[tk("C", hg)])
            for hg in HG:
                Dd, qgT4 = FS[hg][3], BS[hg][6]
                S.dve(lambda e, hg=hg, Dd=Dd, qgT4=qgT4: e.tensor_tensor(out=qgT4[:], in0=qT[:, HS[hg], :], in1=Dd[:], op=ALU.mult), reads=["qT", tk("D", hg)], writes=[tk("qgT4", hg)])
            if stop == 12:
                S.mute = True
            pks, pqs = {}, {}
            for hg in HG:
                pk = self.psum()
                pq = self.psum()
                pks[hg], pqs[hg] = pk, pq
                for i in range(4):
                    S.pe(lambda e, pk=pk, i=i, hg=hg: e.matmul(self.ps[pk][:, i * 128:(i + 1) * 128], kT[:, hg * 4 + i, :], kT[:, hg * 4 + i, :], start=True, stop=True), reads=["kT"], writes=[("ps", pk)])
                for i in range(4):
                    S.pe(lambda e, pq=pq, i=i, hg=hg: e.matmul(self.ps[pq][:, i * 128:(i + 1) * 128], kT[:, hg * 4 + i, :], qT[:, hg * 4 + i, :], start=True, stop=True), reads=["kT", "qT"], writes=[("ps", pq)])
                B_, C_, Dd, E_, AT4 = FS[hg][1], FS[hg][2], FS[hg][3], FS[hg][4], BS[hg][0]
                S.dve(lambda e, pk=pk, E_=E_, B_=B_: e.tensor_tensor(out=fl(E_), in0=self.ps[pk][:, :], in1=fl(B_), op=ALU.mult), reads=[("ps", pk), tk("B", hg)], writes=[tk("E", hg)])
                S.pool(lambda e, hg=hg, E_=E_: e.tensor_tensor(out=E_[:], in0=E_[:], in1=bc_h(beta[:, HS[hg]]), op=ALU.mult), reads=[tk("E", hg), "beta"], writes=[tk("E", hg)])
                S.pool(lambda e, E_=E_: e.tensor_tensor(out=E_[:], in0=E_[:], in1=bc_m(strict), op=ALU.mult), reads=[tk("E", hg), "g_masks"], writes=[tk("E", hg)])
                S.dve(lambda e, pq=pq, Dd=Dd, C_=C_: e.tensor_tensor(out=fl(Dd), in0=self.ps[pq][:, :], in1=fl(C_), op=ALU.mult), reads=[("ps", pq), tk("C", hg), tk("qgT4", hg)], writes=[tk("D", hg)])
                S.pool(lambda e, Dd=Dd, AT4=AT4: e.tensor_tensor(out=AT4[:], in0=Dd[:], in1=bc_m(inclT), op=ALU.mult), reads=[tk("D", hg), "g_masks"], writes=[tk("AT4", hg)])
            if stop == 13:
                S.mute = True
            for hg in HG:
                E_, F_, G_ = FS[hg][4], FS[hg][5], FS[hg][6]
                pt_ = self.psum()
                for i in range(4):
                    S.pe(lambda e, pt_=pt_, i=i, E_=E_: e.matmul(self.ps[pt_][:, i * 128:(i + 1) * 128], E_[:, i, :], ident_f[:], start=True, stop=True), reads=[tk("E", hg)], writes=[("ps", pt_)])
                S.act(lambda e, pt_=pt_, F_=F_: e.copy(out=fl(F_), in_=self.ps[pt_][:, :]), reads=[("ps", pt_)], writes=[tk("F", hg)])
                S.dve(lambda e, F_=F_, G_=G_: e.tensor_tensor(out=G_[:], in0=bc_m(ident_f[:]), in1=F_[:], op=ALU.subtract), reads=[tk("F", hg)], writes=[tk("G", hg)])
            if stop == 14:
                S.mute = True
            Mcur = {hg: (FS[hg][4], tk("E", hg)) for hg in HG}
            Mtcur = {hg: (FS[hg][5], tk("F", hg)) for hg in HG}
            Ycur = {hg: 6 for hg in HG}
            for it in range(1, 6):
                for hg in HG:
                    M, Mtok = Mcur[hg]
                    Mt, Mttok = Mtcur[hg]
                    Mn, Mn_t = (FS[hg][3], tk("D", hg)) if it % 2 == 1 else (FS[hg][0], tk("A", hg))
                    pm = self.psum()
                    for i in range(4):
                        S.pe(lambda e, pm=pm, i=i, M=M, Mt=Mt: e.matmul(self.ps[pm][:, i * 128:(i + 1) * 128], Mt[:, i, :], M[:, i, :], start=True, stop=True), reads=[Mtok, Mttok], writes=[("ps", pm)])
                    rd_extra = [tk("AT4", hg), tk("B", hg), tk("C", hg)]
                    S.act(lambda e, pm=pm, Mn=Mn: e.copy(out=fl(Mn), in_=self.ps[pm][:, :]), reads=[("ps", pm)] + rd_extra, writes=[Mn_t])
                    if it < 5:
                        Mtn, Mtn_t = (FS[hg][1], tk("B", hg)) if it % 2 == 1 else (FS[hg][2], tk("C", hg))
                        pm2 = self.psum()
                        for i in range(4):
                            S.pe(lambda e, pm2=pm2, i=i, M=M, Mt=Mt: e.matmul(self.ps[pm2][:, i * 128:(i + 1) * 128], M[:, i, :], Mt[:, i, :], start=True, stop=True), reads=[Mtok, Mttok], writes=[("ps", pm2)])
                        S.dve(lambda e, pm2=pm2, Mtn=Mtn: e.tensor_copy(out=fl(Mtn), in_=self.ps[pm2][:, :]), reads=[("ps", pm2), tk("E", hg)], writes=[Mtn_t])
                    yc = Ycur[hg]
                    yn_ = 13 - yc
                    Yc, Yn = FS[hg][yc], FS[hg][yn_]
                    yct, ynt = tk("G" if yc == 6 else "H", hg), tk("G" if yn_ == 6 else "H", hg)
                    py = self.psum()
                    for i in range(4):
                        S.pe(lambda e, py=py, i=i, Mn=Mn, Yc=Yc: e.matmul(self.ps[py][:, i * 128:(i + 1) * 128], Mn[:, i, :], Yc[:, i, :], start=True, stop=True), reads=[Mn_t, yct], writes=[("ps", py)])
                    S.dve(lambda e, py=py, Yc=Yc, Yn=Yn: e.tensor_tensor(out=fl(Yn), in0=fl(Yc), in1=self.ps[py][:, :], op=ALU.add), reads=[("ps", py), yct], writes=[ynt])
                    Ycur[hg] = yn_
                    Mcur[hg] = (Mn, Mn_t)
                    if it < 5:
                        Mtcur[hg] = (Mtn, Mtn_t)
            if stop == 15:
                S.mute = True
            pus, pws = {}, {}
            for hg in HG:
                yc = Ycur[hg]
                yct = tk("G" if yc == 6 else "H", hg)
                Ybf, vb4, kbg4 = BS[hg][1], BS[hg][2], BS[hg][3]
                S.act(lambda e, hg=hg, yc=yc, Ybf=Ybf: e.copy(out=Ybf[:], in_=FS[hg][yc][:]), reads=[yct], writes=[tk("Ybf", hg)])
                if stop == 21:
                    S.mute = True
                pu = self.psum()
                pw = self.psum()
                pus[hg], pws[hg] = pu, pw
                for i in range(4):
                    S.pe(lambda e, pu=pu, i=i, Ybf=Ybf, vb4=vb4: e.matmul(self.ps[pu][:, i * 128:(i + 1) * 128], Ybf[:, i, :], vb4[:, i, :], start=True, stop=True), reads=[tk("Ybf", hg), tk("vb4", hg)], writes=[("ps", pu)])
                for i in range(4):
                    S.pe(lambda e, pw=pw, i=i, Ybf=Ybf, kbg4=kbg4: e.matmul(self.ps[pw][:, i * 128:(i + 1) * 128], kbg4[:, i, :], Ybf[:, i, :], start=True, stop=True), reads=[tk("Ybf", hg), tk("kbg4", hg)], writes=[("ps", pw)])
            if stop == 22:
                S.mute = True
            for hg in HG:
                u4 = FS[hg][1]
                S.dve(lambda e, hg=hg, u4=u4, pu=pus[hg]: e.tensor_copy(out=fl(u4), in_=self.ps[pu][:, :]), reads=[("ps", pus[hg]), tk("E", hg), tk("F", hg), tk("A", hg), tk("D", hg)], writes=[tk("B", hg)])
            if stop == 23:
                S.mute = True
            for hg in HG:
                wT4 = BS[hg][7]
                if stop == 24 + hg:
                    S.mute = True
                S.dve(lambda e, hg=hg, wT4=wT4, pw=pws[hg]: e.tensor_copy(out=fl(wT4), in_=self.ps[pw][:, :]), reads=[("ps", pws[hg])], writes=[tk("wT4", hg)])
            if stop in (16, 23):
                S.mute = True
            for ci in order:
                pvs, pos_, pSs = {}, {}, {}
                for hg in HG:
                    wT4 = BS[hg][7]
                    pvv = self.psum()
                    pvs[hg] = pvv
                    for i in range(4):
                        S.pe(lambda e, pvv=pvv, i=i, hg=hg, wT4=wT4: e.matmul(self.ps[pvv][:, i * 128:(i + 1) * 128], wT4[:, i, :], Sb[:, hg * 4 + i, :], start=True, stop=True), reads=[tk("wT4", hg), ("Sb", hg)], writes=[("ps", pvv)])
                for hg in HG:
                    u4 = FS[hg][1]
                    S.dve(lambda e, hg=hg, u4=u4, pv_=pvs[hg]: e.tensor_tensor(out=fl(vn[hg]), in0=fl(u4), in1=self.ps[pv_][:, :], op=ALU.subtract), reads=[("ps", pvs[hg]), tk("B", hg)], writes=[("vn", hg)])
                    S.pool(lambda e, hg=hg, ci=ci: e.tensor_tensor(out=St[:, HS[hg], :], in0=St[:, HS[hg], :], in1=bc_h(egl[:, ci, HS[hg]]), op=ALU.mult), reads=[("St", hg), "egl"], writes=[("St", hg)])
                for hg in HG:
                    qgT4, AT4 = BS[hg][6], BS[hg][0]
                    kdm, kdm_t = (BS[hg][4], tk("kdm0", hg)) if ci == 0 else (BS[hg][5], tk("kdm1", hg))
                    po = self.psum()
                    pos_[hg] = po
                    for i in range(4):
                        S.pe(lambda e, po=po, i=i, hg=hg, qgT4=qgT4: e.matmul(self.ps[po][:, i * 128:(i + 1) * 128], qgT4[:, i, :], Sb[:, hg * 4 + i, :], start=True, stop=False), reads=[tk("qgT4", hg), ("Sb", hg)], writes=[("ps", po)])
                        S.pe(lambda e, po=po, i=i, hg=hg, AT4=AT4: e.matmul(self.ps[po][:, i * 128:(i + 1) * 128], AT4[:, i, :], vn[hg][:, i, :], start=False, stop=True), reads=[tk("AT4", hg), ("vn", hg)], writes=[("ps", po)])
                    pS = self.psum()
                    pSs[hg] = pS
                    for i in range(4):
                        S.pe(lambda e, pS=pS, i=i, hg=hg, kdm=kdm: e.matmul(self.ps[pS][:, i * 128:(i + 1) * 128], kdm[:, i, :], vn[hg][:, i, :], start=True, stop=True), reads=[kdm_t, ("vn", hg)], writes=[("ps", pS)])
                for hg in HG:
                    if has_o:
                        R = slice(ci * 64, ci * 64 + 64)
                        S.act(lambda e, hg=hg, R=R, ob=ob, po=pos_[hg]: e.copy(out=ob[R, HS[hg], :], in_=self.ps[po][R, :].rearrange("p (h t) -> p h t", h=4)), reads=[("ps", pos_[hg])], writes=[otok])
                    S.dve(lambda e, hg=hg, pS=pSs[hg]: e.tensor_tensor(out=St[:, HS[hg], :], in0=St[:, HS[hg], :], in1=self.ps[pS][:, :].rearrange("p (h t) -> p h t", h=4), op=ALU.add), reads=[("St", hg), ("ps", pSs[hg])], writes=[("St", hg)])
                    S.act(lambda e, hg=hg: e.copy(out=Sb[:, HS[hg], :], in_=St[:, HS[hg], :]), reads=[("St", hg)], writes=[("Sb", hg)])
            S.mute = False
            if has_o:
                S.dma("pool", o_d[t0:t0 + 128, :], ob[:].rearrange("p h t -> p (h t)"), reads=[otok])
            nunit += 1
        if state_out_d is not None:
            S.dma("sp", state_out_d, St[:], reads=[("St", hg) for hg in range(4)])
        S.pop()

    def ffn_blocks(self, hb, hb_t, T, wg_d, wu_d, wd_d, ev, gate_bc=None, FB=256, FF=5632):
        S = self.S
        wgs = [S.scratch(("wg", i), [128, 16, FB], BF16) for i in range(2)]
        wus = [S.scratch(("wu", i), [128, 16, FB], BF16) for i in range(2)]
        wds = [S.scratch(("wd", i), [128, FB // 128, 2048], BF16) for i in range(2)]
        act, act_t = S.scratch("act", [128, FB // 128, T], BF16)
        sgs = [S.scratch(("sg", i), [128, 512], F32) for i in range(2)]
        Wg = wg_d.rearrange("(k p) n -> p k n", p=128)
        Wu = wu_d.rearrange("(k p) n -> p k n", p=128)
        Wd = wd_d.rearrange("(k p) n -> p k n", p=128)
        if not hasattr(self, "_nffn"):
            self._nffn = 0
        for fb in range(FF // FB):
            par = self._nffn % 2
            self._nffn += 1
            wg, wg_t = wgs[par]
            wu, wu_t = wus[par]
            wd, wd_t = wds[par]
            S.dma("pool", wg[:], Wg[:, :, fb * FB:(fb + 1) * FB], writes=[wg_t])
            S.dma("pool", wu[:], Wu[:, :, fb * FB:(fb + 1) * FB], writes=[wu_t])
            S.dma("pool", wd[:], Wd[:, fb * (FB // 128):(fb + 1) * (FB // 128), :], writes=[wd_t])
            for fc in range(FB // 128):
                for ho in range(0, T, 512):
                    Th = min(512, T - ho)
                    pg = self.psum()
                    pu = self.psum()
                    for k in range(16):
                        S.pe(lambda e, pg=pg, wg=wg, k=k, fc=fc, ho=ho, Th=Th: e.matmul(self.ps[pg][:, 0:Th], wg[:, k, fc * 128:(fc + 1) * 128], hb[:, k, ho:ho + Th], start=(k == 0), stop=(k == 15)),
                             reads=[wg_t, hb_t], writes=[("ps", pg)])
                    for k in range(16):
                        S.pe(lambda e, pu=pu, wu=wu, k=k, fc=fc, ho=ho, Th=Th: e.matmul(self.ps[pu][:, 0:Th], wu[:, k, fc * 128:(fc + 1) * 128], hb[:, k, ho:ho + Th], start=(k == 0), stop=(k == 15)),
                             reads=[wu_t, hb_t], writes=[("ps", pu)])
                    sg, sg_t = sgs[(fc + ho // 512) % 2]
                    S.act(lambda e, sg=sg, pg=pg, Th=Th: e.activation(out=sg[:, 0:Th], in_=self.ps[pg][:, 0:Th], func=AF.Silu), reads=[("ps", pg)], writes=[sg_t])
                    if gate_bc is None:
                        S.dve(lambda e, sg=sg, pu=pu, fc=fc, ho=ho, Th=Th: e.tensor_tensor(out=act[:, fc, ho:ho + Th], in0=sg[:, 0:Th], in1=self.ps[pu][:, 0:Th], op=ALU.mult),
                              reads=[sg_t, ("ps", pu)], writes=[act_t])
                    else:
                        gb, gb_t = gate_bc
                        S.dve(lambda e, sg=sg, pu=pu, Th=Th: e.tensor_tensor(out=sg[:, 0:Th], in0=sg[:, 0:Th], in1=self.ps[pu][:, 0:Th], op=ALU.mult),
                              reads=[sg_t, ("ps", pu)], writes=[sg_t])
                        S.pool(lambda e, sg=sg, fc=fc, ho=ho, Th=Th, gb=gb: e.tensor_tensor(out=act[:, fc, ho:ho + Th], in0=sg[:, 0:Th], in1=gb[:, ho:ho + Th], op=ALU.mult),
                               reads=[sg_t, gb_t], writes=[act_t])
            for c in range(16):
                for ho in range(0, T, 512):
                    Th = min(512, T - ho)
                    pi = self.psum()
                    for kk in range(FB // 128):
                        S.pe(lambda e, pi=pi, wd=wd, kk=kk, c=c, ho=ho, Th=Th: e.matmul(self.ps[pi][:, 0:Th], wd[:, kk, c * 128:(c + 1) * 128], act[:, kk, ho:ho + Th],
                                                                                         start=(kk == 0), stop=(kk == FB // 128 - 1)),
                             reads=[wd_t, act_t], writes=[("ps", pi)])
                    ev(c, 128, ho, Th, pi)

    def tail(self, of_d, ob_d, p1T_d, x1T_d, gnw_d, w_out_d, mv1, rw_d, selE_d, mwg_d, mwu_d, mwd_d, fnw_d, outT_d, tiles=None, n_exp=8, hsel_d=None, routed=None):
        S = self.S
        S.push()
        T = 512
        if tiles is None:
            tiles = [i * 512 for i in range(8 if hsel_d is None else 4)]
        HOFF = NTOK // 2
        if hsel_d is not None:
            hsel = self.load_vec(hsel_d, [128, 2], "hsel")
        zv = p1T_d[6144:8192, :].rearrange("(k p) t -> p k t", p=128)
        x1v = x1T_d.rearrange("(k p) t -> p k t", p=128)
        outv = outT_d.rearrange("(k p) t -> p k t", p=128)
        gnw = self.load_vec(gnw_d, [128, 1], "gnw")
        S.dve(lambda e: e.tensor_scalar(out=gnw[:], in0=gnw[:], scalar1=float(np.sqrt(128.0)), scalar2=None, op0=ALU.mult), reads=["gnw"], writes=["gnw"])
        rw = self.load_vec(rw_d, [128, 16, 8], "rw")
        selE = self.load_vec(selE_d, [8, 8, 128], "selE") if routed is None else None
        Afin = S.sb("Afin", [128, 16, 1], F32)
        Bfin = S.sb("Bfin", [128, 16, 1], F32)
        S.dma("sp", Afin[:, :, 0], fnw_d, writes=["Afin"])
        S.dve(lambda e: e.tensor_scalar(out=Afin[:], in0=Afin[:], scalar1=float(np.sqrt(D)), scalar2=None, op0=ALU.mult), reads=["Afin"], writes=["Afin"])
        S.dve(lambda e: e.memset(Bfin[:], 0.0), writes=["Bfin"])
        if routed is not None:
            gate_all = S.sb("gate_all", [128, 16, 8], F32)
            sel_all = S.sb("sel_all", [128, 16, 8], F32)
        S.push()
        xt = S.sb("xt", [128, 16, T], F32)
        big = S.sb("big", [128, 16, T], F32)
        zt = S.sb("zt", [128, 16, T], F32)
        hb = S.sb("hb", [128, 16, T], BF16)
        ztf = zt[:].rearrange("p k t -> p (k t)")
        sq = S.sb("sq", [128, T], BF16)
        rstd = S.sb("rstd", [128, T], F32)
        zs = S.sb("zs", [128, T], F32)
        gt = S.sb("gt", [128, T], F32)
        h2f = [S.sb("h2f", [128, T], F32) for _ in range(2)]
        lg = S.sb("lg", [128, 4, 8], F32)
        m1 = S.sb("m1", [128, 4], F32)
        m2 = S.sb("m2", [128, 4], F32)
        eq = S.sb("eq", [128, 4, 8], F32)
        l2 = S.sb("l2", [128, 4, 8], F32)
        ee = S.sb("ee", [128, 4, 8], F32)
        ssum = S.sb("ssum", [128, 4], F32)
        gate = S.sb("gate", [128, 4, 8], F32)
        gT = S.sb("gT", [8, T], F32)
        gbc = [S.sb("gbc", [128, T], F32) for _ in range(2)]
        RB = 7

        if routed is not None:
            h2st = [S.sb("h2st", [128, 2048], BF16) for _ in range(2)]
        ntile = [0]

        def bc8(ap2):
            return ap2.rearrange("p (s o) -> p s o", o=1).broadcast_to([128, 4, 8])

        def res_evac(G):
            def ev(c, M, ho, Th, pi):
                S.dve(lambda e: e.scalar_tensor_tensor(out=xt[:, c, ho:ho + Th], in0=self.ps[pi][:, 0:Th], scalar=G[0][:, c, 0:1], in1=xt[:, c, ho:ho + Th], op0=ALU.mult, op1=ALU.add),
                      reads=[("ps", pi), G[1], "xt"], writes=["xt"])
            return ev

        for t0 in tiles:
            S.dma("sp", xt[:], x1v[:, :, t0:t0 + T], writes=["xt"])
            if hsel_d is not None:
                S.dma("sp", big[:], x1v[:, :, HOFF + t0:HOFF + t0 + T], writes=["big"])
                S.act(lambda e: e.activation(out=xt[:], in_=xt[:], func=AF.Copy, scale=hsel[:, 0:1]), reads=["xt", "hsel"], writes=["xt"])
                S.dve(lambda e: e.scalar_tensor_tensor(out=xt[:], in0=big[:], scalar=hsel[:, 1:2], in1=xt[:], op0=ALU.mult, op1=ALU.add), reads=["xt", "big", "hsel"], writes=["xt"])
            ZT = [("zt", i) for i in range(4)]
            for sub in range(4):
                ra, rb = sub % 2, 2 + sub % 2
                a, b_ = ztf[:, ra * 2048:(ra + 1) * 2048], ztf[:, rb * 2048:(rb + 1) * 2048]
                S.dma("sp", a, of_d[t0 + sub * 128:t0 + (sub + 1) * 128, :], writes=[("zt", ra)])
                S.dma("sp", b_, ob_d[t0 + sub * 128:t0 + (sub + 1) * 128, :], writes=[("zt", rb)])
                S.pool(lambda e, a=a, b_=b_: e.tensor_tensor(out=a, in0=a, in1=b_, op=ALU.add), reads=[("zt", ra), ("zt", rb)], writes=[("zt", ra)])
                if hsel_d is not None:
                    S.dve(lambda e, a=a: e.tensor_scalar(out=a, in0=a, scalar1=hsel[:, 0:1], scalar2=None, op0=ALU.mult), reads=[("zt", ra), "hsel"], writes=[("zt", ra)])
                    for src in (of_d, ob_d):
                        S.dma("sp", b_, src[HOFF + t0 + sub * 128:HOFF + t0 + (sub + 1) * 128, :], writes=[("zt", rb)])
                        S.dve(lambda e, a=a, b_=b_: e.scalar_tensor_tensor(out=a, in0=b_, scalar=hsel[:, 1:2], in1=a, op0=ALU.mult, op1=ALU.add),
                              reads=[("zt", ra), ("zt", rb), "hsel"], writes=[("zt", ra)])
                for h4 in range(4):
                    pi = self.psum()
                    for i in range(4):
                        hh = h4 * 4 + i
                        S.pe(lambda e, pi=pi, i=i, hh=hh, a=a: e.matmul(self.ps[pi][:, i * 128:(i + 1) * 128], a[:, hh * 128:(hh + 1) * 128], self.ident_f[:], start=True, stop=True),
                             reads=[("zt", ra)], writes=[("ps", pi)])
                    S.act(lambda e, pi=pi, h4=h4, sub=sub: e.copy(out=big[:, h4 * 4:(h4 + 1) * 4, sub * 128:(sub + 1) * 128], in_=self.ps[pi][:, :].rearrange("p (h t) -> p h t", h=4)),
                          reads=[("ps", pi)], writes=["big"])
            S.dma("sp", zt[:], zv[:, :, t0:t0 + T], writes=ZT)
            if hsel_d is not None:
                pass
            for h in range(16):
                S.act(lambda e, h=h: e.activation(out=sq[:], in_=big[:, h, :], func=AF.Square), reads=["big"], writes=["sq"])
                pi = self.psum()
                S.pe(lambda e, pi=pi: e.matmul(self.ps[pi][:, :], self.ones_bf[:], sq[:], start=True, stop=True), reads=["sq"], writes=[("ps", pi)])
                self.rsqrt(rstd[:], self.ps[pi][:, :], float(128 * EPS), [("ps", pi)], "rstd")
                if hsel_d is not None:
                    S.dma("sp", zs[:], zv[:, h, HOFF + t0:HOFF + t0 + T], writes=["zs"])
                    S.act(lambda e, h=h: e.activation(out=zt[:, h, :], in_=zt[:, h, :], func=AF.Copy, scale=hsel[:, 0:1]), reads=ZT + ["hsel"], writes=ZT)
                    S.dve(lambda e, h=h: e.scalar_tensor_tensor(out=zt[:, h, :], in0=zs[:], scalar=hsel[:, 1:2], in1=zt[:, h, :], op0=ALU.mult, op1=ALU.add),
                          reads=ZT + ["zs", "hsel"], writes=ZT)
                S.act(lambda e, h=h: e.activation(out=zs[:], in_=zt[:, h, :], func=AF.Silu), reads=ZT, writes=["zs"])
                S.dve(lambda e, h=h: e.tensor_tensor(out=gt[:], in0=big[:, h, :], in1=rstd[:], op=ALU.mult), reads=["big", "rstd"], writes=["gt"])
                S.dve(lambda e, h=h: e.scalar_tensor_tensor(out=hb[:, h, :], in0=gt[:], scalar=gnw[:, 0:1], in1=zs[:], op0=ALU.mult, op1=ALU.mult), reads=["gt", "zs", "gnw"], writes=["hb"])
            self.linear_fm(w_out_d, 2048, hb, "hb", T, res_evac(mv1["Gmix"]), wkey="wg", blk=256)
            A, B = mv1["Affn"], mv1["Bffn"]

            def router_cb(c, c0, c1, j, tm, tm_t):
                hf = h2f[c % 2]
                S.act(lambda e: e.activation(out=hf[:], in_=tm[:], func=AF.Identity, bias=B[0][:, c, 0:1], scale=A[0][:, c, 0:1]), reads=[tm_t, A[1], B[1]], writes=[("h2f", c % 2)])
                pr = self.psum()
                for sub in range(4):
                    S.pe(lambda e, sub=sub, pr=pr: e.matmul(self.ps[pr][:, sub * 8:(sub + 1) * 8], hf[:, sub * 128:(sub + 1) * 128], rw[:, c, :], start=True, stop=True),
                         reads=[("h2f", c % 2), "rw"], writes=[("ps", pr)])
                lgf = lg[:].rearrange("p s e -> p (s e)")
                if c == 0:
                    S.dve(lambda e, pr=pr: e.tensor_copy(out=lgf, in_=self.ps[pr][:, 0:32]), reads=[("ps", pr)], writes=["lg"])
                else:
                    S.dve(lambda e, pr=pr: e.tensor_tensor(out=lgf, in0=lgf, in1=self.ps[pr][:, 0:32], op=ALU.add), reads=[("ps", pr), "lg"], writes=["lg"])
            self.rms_mod(xt, "xt", T, [(0, T, 0)], A, B, hb, 0, "hb", extra_out=router_cb)
            S.dve(lambda e: e.tensor_reduce(out=m1[:], in_=lg[:], axis=AX.X, op=ALU.max), reads=["lg"], writes=["m1"])
            S.dve(lambda e: e.tensor_tensor(out=eq[:], in0=lg[:], in1=bc8(m1[:]), op=ALU.is_equal), reads=["lg", "m1"], writes=["eq"])
            S.dve(lambda e: e.scalar_tensor_tensor(out=l2[:], in0=eq[:], scalar=-1e30, in1=lg[:], op0=ALU.mult, op1=ALU.add), reads=["eq", "lg"], writes=["l2"])
            S.dve(lambda e: e.tensor_reduce(out=m2[:], in_=l2[:], axis=AX.X, op=ALU.max), reads=["l2"], writes=["m2"])
            S.dve(lambda e: e.tensor_tensor(out=eq[:], in0=lg[:], in1=bc8(m2[:]), op=ALU.is_ge), reads=["lg", "m2", "l2"], writes=["eq"])
            S.dve(lambda e: e.tensor_tensor(out=l2[:], in0=lg[:], in1=bc8(m1[:]), op=ALU.subtract), reads=["lg", "m1", "eq"], writes=["l2"])
            S.act(lambda e: e.activation(out=ee[:], in_=l2[:], func=AF.Exp), reads=["l2"], writes=["ee"])
            S.dve(lambda e: e.tensor_tensor(out=ee[:], in0=ee[:], in1=eq[:], op=ALU.mult), reads=["ee", "eq"], writes=["ee"])
            S.dve(lambda e: e.tensor_reduce(out=ssum[:], in_=ee[:], axis=AX.X, op=ALU.add), reads=["ee"], writes=["ssum"])
            S.dve(lambda e: e.reciprocal(out=ssum[:], in_=ssum[:]), reads=["ssum"], writes=["ssum"])
            S.dve(lambda e: e.tensor_tensor(out=gate[:], in0=ee[:], in1=bc8(ssum[:]), op=ALU.mult), reads=["ee", "ssum"], writes=["gate"])
            if routed is not None:
                ti = ntile[0]
                ntile[0] += 1
                x2v = routed["x2T"].rearrange("(k p) t -> p k t", p=128)
                S.dma("sp", x2v[:, :, ti * T:(ti + 1) * T], xt[:], reads=["xt"])
                S.dve(lambda e, ti=ti: e.tensor_copy(out=gate_all[:, ti * 4:(ti + 1) * 4, :], in_=gate[:]), reads=["gate"], writes=["gate_all"])
                S.dve(lambda e, ti=ti: e.tensor_copy(out=sel_all[:, ti * 4:(ti + 1) * 4, :], in_=eq[:]), reads=["eq"], writes=["sel_all"])
                for sub in range(4):
                    hst = h2st[sub % 2]
                    for c4 in range(4):
                        pi = self.psum()
                        for i_ in range(4):
                            S.pe(lambda e, pi=pi, i_=i_, c4=c4, sub=sub: e.matmul(self.ps[pi][:, i_ * 128:(i_ + 1) * 128], hb[:, c4 * 4 + i_, sub * 128:(sub + 1) * 128], self.ident_bf[:], start=True, stop=True),
                                 reads=["hb"], writes=[("ps", pi)])
                        if c4 % 2 == 0:
                            S.act(lambda e, pi=pi, c4=c4, hst=hst: e.copy(out=hst[:, c4 * 512:(c4 + 1) * 512], in_=self.ps[pi][:, :]), reads=[("ps", pi)], writes=[("h2st", sub % 2)])
                        else:
                            S.dve(lambda e, pi=pi, c4=c4, hst=hst: e.tensor_copy(out=hst[:, c4 * 512:(c4 + 1) * 512], in_=self.ps[pi][:, :]), reads=[("ps", pi)], writes=[("h2st", sub % 2)])
                    S.dma("sp", routed["h2tm"][(ti * 4 + sub) * 128:(ti * 4 + sub + 1) * 128, :], hst[:], reads=[("h2st", sub % 2)])
                continue
            pgt = self.psum()
            for sub in range(4):
                S.pe(lambda e, sub=sub, pgt=pgt: e.matmul(self.ps[pgt][0:8, sub * 128:(sub + 1) * 128], gate[:, sub, :], self.ident_f[:], start=True, stop=True), reads=["gate"], writes=[("ps", pgt)])
            S.dve(lambda e, pgt=pgt: e.tensor_copy(out=gT[:], in_=self.ps[pgt][0:8, :]), reads=[("ps", pgt)], writes=["gT"])
            ev = res_evac(mv1["Gffn"])
            for ex in range(n_exp):
                gb = gbc[ex % 2]
                pgb = self.psum()
                S.pe(lambda e, pgb=pgb, ex=ex: e.matmul(self.ps[pgb][:, :], selE[:, ex, :], gT[:], start=True, stop=True), reads=["gT", "selE"], writes=[("ps", pgb)])
                S.act(lambda e, pgb=pgb, gb=gb: e.copy(out=gb[:], in_=self.ps[pgb][:, :]), reads=[("ps", pgb)], writes=[("gbc", ex % 2)])
                self.ffn_blocks(hb, "hb", T, mwg_d[ex], mwu_d[ex], mwd_d[ex], ev, gate_bc=(gb, ("gbc", ex % 2)))
            self.rms_mod(xt, "xt", T, [(0, T, 0)], (Afin, "Afin"), (Bfin, "Bfin"), big, 0, "big")
            S.dma("sp", outv[:, :, t0:t0 + T], big[:], reads=["big"])
        S.pop()
        if routed is not None:
            self.moe_routed(routed, gate_all, sel_all, mwg_d, mwu_d, mwd_d, mv1, Afin, Bfin, outv, n_exp)
        self.ps_skip = set()
        S.pop()

    def moe_routed(self, r, gate_all, sel_all, mwg_d, mwu_d, mwd_d, mv1, Afin, Bfin, outv, n_exp=8):
        S = self.S
        S.barrier()
        CAP = 1280
        NS = CAP // 128
        NT = 16
        S.push()
        pos = S.sb("pos", [128, NT, 8], F32)
        base = S.sb("base", [128, 8], F32)
        S.push()
        rtc0 = self.load_vec(r["rtc"][:, 0:128], [128, 128], "rtc0")
        tris = rtc0[:, 0:128]
        S.dve(lambda e: e.memset(base[:], 0.0), writes=["base"])
        for j in range(NT):
            pi = self.psum()
            S.pe(lambda e, pi=pi, j=j: e.matmul(self.ps[pi][:, 0:8], tris, sel_all[:, j, :], start=True, stop=True), reads=["sel_all", "rtc0"], writes=[("ps", pi)])
            S.pe(lambda e, pi=pi, j=j: e.matmul(self.ps[pi][:, 8:16], self.ones_f[:], sel_all[:, j, :], start=True, stop=True), reads=["sel_all"], writes=[("ps", pi)])
            S.dve(lambda e, pi=pi, j=j: e.tensor_tensor(out=pos[:, j, :], in0=self.ps[pi][:, 0:8], in1=base[:], op=ALU.add), reads=[("ps", pi), "base"], writes=["pos"])
            S.dve(lambda e, j=j: e.scalar_tensor_tensor(out=pos[:, j, :], in0=pos[:, j, :], scalar=1.0, in1=sel_all[:, j, :], op0=ALU.add, op1=ALU.mult), reads=["pos", "sel_all"], writes=["pos"])
            S.dve(lambda e, j=j: e.tensor_scalar(out=pos[:, j, :], in0=pos[:, j, :], scalar1=-1.0, scalar2=None, op0=ALU.add), reads=["pos"], writes=["pos"])
            S.dve(lambda e, pi=pi: e.tensor_tensor(out=base[:], in0=base[:], in1=self.ps[pi][:, 8:16], op=ALU.add), reads=[("ps", pi), "base"], writes=["base"])
        S.pop()
        hg = S.sb("hg", [128, 16, CAP], BF16)
        st2 = [S.sb("st2", [128, 2048], BF16) for _ in range(2)]
        nst = [0]
        h2v = r["h2tm"].rearrange("(j p) f -> p j f", p=128)
        for ex in range(n_exp):
            S.push()
            iot = self.load_vec(r["rtc"][:, 128:128 + CAP], [128, CAP], "iota")
            iota = iot[:, :]
            selb = S.sb("selb", [128, NT, CAP], BF16)
            h2sb = S.sb("h2sb", [128, NT, 2048], BF16)
            for jq in range(4):
                S.dma("sp", h2sb[:, jq * 4:(jq + 1) * 4, :], h2v[:, jq * 4:(jq + 1) * 4, :], writes=["h2sb"])
            for j in range(NT):
                S.dve(lambda e, j=j, ex=ex: e.tensor_scalar(out=selb[:, j, :], in0=iota, scalar1=pos[:, j, ex:ex + 1], scalar2=gate_all[:, j, ex:ex + 1], op0=ALU.is_equal, op1=ALU.mult),
                      reads=["pos", "gate_all", "iota"], writes=["selb"])
            for s_ in range(NS):
                si = nst[0] % 2
                nst[0] += 1
                for j4 in range(4):
                    pi = self.psum()
                    for i_ in range(4):
                        S.pe(lambda e, pi=pi, i_=i_, j4=j4, s_=s_: e.matmul(self.ps[pi][:, i_ * 128:(i_ + 1) * 128], selb[:, j4 * 4 + i_, s_ * 128:(s_ + 1) * 128], self.ident_bf[:], start=True, stop=True),
                             reads=["selb"], writes=[("ps", pi)])
                    S.act(lambda e, pi=pi, j4=j4, si=si: e.copy(out=st2[si][:, j4 * 512:(j4 + 1) * 512], in_=self.ps[pi][:, :]), reads=[("ps", pi)], writes=[("st2", si)])
                S.dma("sp", r["sgt"][(ex * NS + s_) * 128:(ex * NS + s_ + 1) * 128, :], st2[si][:], reads=[("st2", si)])
            for j in range(NT):
                S.dve(lambda e, j=j, ex=ex: e.tensor_scalar(out=selb[:, j, :], in0=iota, scalar1=pos[:, j, ex:ex + 1], scalar2=None, op0=ALU.is_equal),
                      reads=["pos", "iota"], writes=["selb"])
            for c in range(16):
                for (h0, hn) in ((0, 512), (512, 512), (1024, 256)):
                    pi = self.psum()
                    for j in range(NT):
                        S.pe(lambda e, pi=pi, j=j, c=c, h0=h0, hn=hn: e.matmul(self.ps[pi][:, 0:hn], h2sb[:, j, c * 128:(c + 1) * 128], selb[:, j, h0:h0 + hn], start=(j == 0), stop=(j == NT - 1)),
                             reads=["h2sb", "selb"], writes=[("ps", pi)])
                    if c % 2 == 0:
                        S.act(lambda e, pi=pi, c=c, h0=h0, hn=hn: e.copy(out=hg[:, c, h0:h0 + hn], in_=self.ps[pi][:, 0:hn]), reads=[("ps", pi)], writes=["hg"])
                    else:
                        S.dve(lambda e, pi=pi, c=c, h0=h0, hn=hn: e.tensor_copy(out=hg[:, c, h0:h0 + hn], in_=self.ps[pi][:, 0:hn]), reads=[("ps", pi)], writes=["hg"])
            S.pop()
            S.push()
            yeT = S.sb("yeT", [128, 16, CAP], F32)
            seen = set()

            def ev_acc(c, M, ho, Th, pi, yeT=yeT, seen=seen):
                if (c, ho) not in seen:
                    seen.add((c, ho))
                    S.dve(lambda e: e.tensor_copy(out=yeT[:, c, ho:ho + Th], in_=self.ps[pi][:, 0:Th]), reads=[("ps", pi)], writes=["yeT"])
                else:
                    S.dve(lambda e: e.tensor_tensor(out=yeT[:, c, ho:ho + Th], in0=yeT[:, c, ho:ho + Th], in1=self.ps[pi][:, 0:Th], op=ALU.add), reads=[("ps", pi), "yeT"], writes=["yeT"])
            self.ffn_blocks(hg, "hg", CAP, mwg_d[ex], mwu_d[ex], mwd_d[ex], ev_acc)
            for s_ in range(NS):
                si = nst[0] % 2
                nst[0] += 1
                for c4 in range(4):
                    pi = self.psum()
                    for i_ in range(4):
                        S.pe(lambda e, pi=pi, i_=i_, c4=c4, s_=s_, yeT=yeT: e.matmul(self.ps[pi][:, i_ * 128:(i_ + 1) * 128], yeT[:, c4 * 4 + i_, s_ * 128:(s_ + 1) * 128], self.ident_f[:], start=True, stop=True),
                             reads=["yeT"], writes=[("ps", pi)])
                    S.act(lambda e, pi=pi, c4=c4, si=si: e.copy(out=st2[si][:, c4 * 512:(c4 + 1) * 512], in_=self.ps[pi][:, :]), reads=[("ps", pi)], writes=[("st2", si)])
                S.dma("sp", r["ye"][(ex * NS + s_) * 128:(ex * NS + s_ + 1) * 128, :], st2[si][:], reads=[("st2", si)])
            S.pop()
        S.pop()
        S.push()
        T = 256
        NSL = n_exp * NS
        xt = S.sb("xt", [128, 16, T], F32)
        big = S.sb("big", [128, 16, T], F32)
        sgt = S.sb("sgt", [128, NSL, T], BF16)
        yec = [S.sb("yec", [128, NSL, 128], BF16) for _ in range(2)]
        x2v = r["x2T"].rearrange("(k p) t -> p k t", p=128)
        sgv = r["sgt"].rearrange("(n p) t -> p n t", p=128)
        yev = r["ye"].rearrange("(n p) f -> p n f", p=128)
        G = mv1["Gffn"]
        for ti in range(2048 // T):
            S.dma("sp", xt[:], x2v[:, :, ti * T:(ti + 1) * T], writes=["xt"])
            for q in range(4):
                S.dma("sp", sgt[:, q * (NSL // 4):(q + 1) * (NSL // 4), :], sgv[:, q * (NSL // 4):(q + 1) * (NSL // 4), ti * T:(ti + 1) * T], writes=["sgt"])
            for c in range(16):
                yb = yec[c % 2]
                for q in range(2):
                    S.dma("pool", yb[:, q * (NSL // 2):(q + 1) * (NSL // 2), :], yev[:, q * (NSL // 2):(q + 1) * (NSL // 2), c * 128:(c + 1) * 128], writes=[("yec", c % 2)])
                pi = self.psum()
                for n in range(NSL):
                    S.pe(lambda e, pi=pi, n=n, yb=yb: e.matmul(self.ps[pi][:, 0:T], yb[:, n, :], sgt[:, n, :], start=(n == 0), stop=(n == NSL - 1)),
                         reads=[("yec", c % 2), "sgt"], writes=[("ps", pi)])
                S.dve(lambda e, pi=pi, c=c: e.scalar_tensor_tensor(out=xt[:, c, :], in0=self.ps[pi][:, 0:T], scalar=G[0][:, c, 0:1], in1=xt[:, c, :], op0=ALU.mult, op1=ALU.add),
                      reads=[("ps", pi), "xt"], writes=["xt"])
            self.rms_mod(xt, "xt", T, [(0, T, 0)], (Afin, "Afin"), (Bfin, "Bfin"), big, 0, "big")
            S.dma("sp", outv[:, :, ti * T:(ti + 1) * T], big[:], reads=["big"])
        S.pop()

def _host_consts():
    ident = np.eye(128, dtype=np.float32)
    P = np.zeros((128, 128), np.float32)
    for m in range(128):
        if (m % 64) < 32:
            P[m + 32, m] = -1.0
        else:
            P[m - 32, m] = 1.0
    t = np.arange(4096)
    row = (t // 64).astype(np.float32)
    col = (t % 64).astype(np.float32)
    inv = (np.float32(10000.0) ** (-np.arange(32, dtype=np.float32) / np.float32(32))).astype(np.float32)
    cos = np.zeros((128, 4096), np.float32)
    sin = np.zeros((128, 4096), np.float32)
    for d in range(128):
        pos = row if d < 64 else col
        ang = (pos * inv[d % 32]).astype(np.float32)
        cos[d] = np.cos(ang)
        sin[d] = np.sin(ang)
    p = np.arange(128)[:, None]
    f = np.arange(128)[None, :]
    same = (p // 64) == (f // 64)
    masks = np.stack([same & (f <= p), same & (f >= p), same & (f < p), same & (f > p)], 1).astype(np.float32).copy()
    k = np.arange(128)[:, None]
    pp = np.arange(128)[None, :]
    sel = np.stack([k == (pp // 64) * 64 + 63, k == (pp // 64) * 64, (k == 63) & (pp >= 0), (k == 127) & (pp >= 0),
                    (k == 0) & (pp >= 0), (k == 64) & (pp >= 0)], 1).astype(np.float32).copy()
    selE = (np.arange(8)[:, None, None] == np.arange(8)[None, :, None]).astype(np.float32).repeat(128, 2).copy()
    rtc = np.concatenate([(np.arange(128)[:, None] < np.arange(128)[None, :]).astype(np.float32),
                          np.arange(1280, dtype=np.float32)[None, :].repeat(128, 0)], axis=1).copy()
    return dict(ident=ident, perm=P, cos=cos, sin=sin, masks=masks, sel=sel, selE=selE, rtc=rtc)


def _na_bias(rpb):
    out = np.full((128, 5, 8, 5, 128), -30000.0, np.float32)
    for ti, lb in enumerate([0, 1, 2, 30, 31]):
        tile0 = min(max(lb - 2, 0), 27)
        q = lb * 128 + np.arange(128)
        r = q // 64
        c = q % 64
        rs = np.clip(r - 4, 0, 56)
        cs = np.clip(c - 8, 0, 48)
        for j in range(5):
            kk = (tile0 + j) * 128 + np.arange(128)
            kr = kk // 64
            kc = kk % 64
            inw = (kr[:, None] >= rs[None, :]) & (kr[:, None] < rs[None, :] + 8) & (kc[:, None] >= cs[None, :]) & (kc[:, None] < cs[None, :] + 16)
            ro = np.clip(kr[:, None] - r[None, :] + 7, 0, 14)
            co = np.clip(kc[:, None] - c[None, :] + 15, 0, 30)
            for h in range(8):
                out[:, ti, h, j, :] = np.where(inw, rpb[h][ro, co], -30000.0)
    return out.astype(ml_dtypes.bfloat16)


def _chunked(v, n=16):
    return np.ascontiguousarray(np.asarray(v, np.float32).reshape(n, 128).T)


def build_nc():
    nc = bass.Bass("TRN2", target_bir_lowering=False)
    k = K(nc)
    i = {}
    i["ident"] = k.din("ident", [128, 128]); i["perm"] = k.din("perm", [128, 128])
    i["cT"] = k.din("cT", [128, 16, 2])
    i["ada_w0"] = k.din("ada_w0", [2048, 12288]); i["ada_b0"] = k.din("ada_b0", [128, 96])
    i["ada_w1"] = k.din("ada_w1", [2048, 12288]); i["ada_b1"] = k.din("ada_b1", [128, 96])
    for nm in ("nmix0", "nffn0", "nmix1", "nffn1", "fnw"):
        i[nm] = k.din(nm, [128, 16])
    i["xcT"] = k.din("xcT", [2048, NU]); i["w_in0"] = k.din("w_in0", [2048, 4608])
    i["qnw"] = k.din("qnw", [128, 1]); i["knw"] = k.din("knw", [128, 1]); i["gnw"] = k.din("gnw", [128, 1])
    i["cos"] = k.din("cos", [128, 4096]); i["sin"] = k.din("sin", [128, 4096])
    i["nabias"] = k.din("nabias", [128, 5, 8, 5, 128], BF16)
    i["w_out0"] = k.din("w_out0", [2048, 2048]); i["wg"] = k.din("wg", [2048, 5632]); i["wu"] = k.din("wu", [2048, 5632]); i["wd"] = k.din("wd", [5632, 2048])
    i["w_in1"] = k.din("w_in1", [2048, 8256])
    i["convw"] = k.din("convw", [128, 48, 4]); i["gparam"] = k.din("gparam", [128, 4, 16]); i["masks"] = k.din("masks", [128, 4, 128]); i["sel"] = k.din("sel", [128, 6, 128])
    i["w_out1"] = k.din("w_out1", [2048, 2048]); i["rw"] = k.din("rw", [128, 16, 8]); i["selE"] = k.din("selE", [8, 8, 128])
    i["mwg"] = k.din("mwg", [8, 2048, 5632]); i["mwu"] = k.din("mwu", [8, 2048, 5632]); i["mwd"] = k.din("mwd", [8, 5632, 2048])
    outT = k.dout("outT", [2048, NTOK // 2])
    i["hsel"] = k.din("hsel", [128, 2])
    sc = {"qna": k.dscr("qna", [8, 128, NU], BF16), "kna": k.dscr("kna", [8, 128, NU], BF16), "vna": k.dscr("vna", [NU, 1024], BF16),
          "qg": k.dscr("qg", [8, 128, NU], BF16), "kg": k.dscr("kg", [2, 128, NU], BF16), "vg": k.dscr("vg", [NU, 256], BF16)}
    attnT = k.dscr("attnT", [2048, NU], BF16)
    x1T = k.dscr("x1T", [2048, NU]); p1T = k.dscr("p1T", [8256, NU])
    o_f = k.dscr("o_f", [NTOK, 2048]); o_b = k.dscr("o_b", [NTOK, 2048])
    routed = {"x2T": k.dscr("x2T", [2048, NTOK // 2]), "h2tm": k.dscr("h2tm", [NTOK // 2, 2048], BF16),
              "sgt": k.dscr("sgt", [8 * 1280, NTOK // 2], BF16), "ye": k.dscr("ye", [8 * 1280, 2048], BF16), "rtc": k.din("rtc", [128, 128 + 1280])}
    S = k.S
    k.load_consts(i["ident"], i["perm"])
    mod0 = S.sb("mod0", [128, 96, 2], F32)
    mod1 = S.sb("mod1", [128, 96, 2], F32)
    k.ada(i["cT"], i["ada_w0"], i["ada_b0"], mod0, "ada0")
    k.ada(i["cT"], i["ada_w1"], i["ada_b1"], mod1, "ada1")
    mv0 = k.mod_vectors(mod0, i["nmix0"], i["nffn0"], "ada0")
    mv1 = k.mod_vectors(mod1, i["nmix1"], i["nffn1"], "ada1")
    k.gdn_consts(i["convw"], i["gparam"], i["masks"], i["sel"])
    k.inproj0(i["xcT"], i["w_in0"], mv0, i["qnw"], i["knw"], i["cos"], i["sin"], sc)
    k.attn_gqa(sc["qg"], sc["kg"], sc["vg"], attnT)
    k.attn_na(sc["qna"], sc["kna"], sc["vna"], i["nabias"], attnT)
    k.mlp0(i["xcT"], attnT, i["w_out0"], mv0, i["wg"], i["wu"], i["wd"], mv1, i["w_in1"], x1T, p1T)
    k.gdn_pass(p1T, 0, o_f)
    k.gdn_pass(p1T, 1, o_b)
    k.tail(o_f, o_b, p1T, x1T, i["gnw"], i["w_out1"], mv1, i["rw"], i["selE"], i["mwg"], i["mwu"], i["mwd"], i["fnw"], outT, hsel_d=i["hsel"], routed=routed)
    S.emit()
    return nc


_NC_CACHE = {}


def kernel(x, c, ctx, c_ctx, ada_w0, ada_b0, norm_mix0, norm_ffn0, w_in0, w_out0, na_rpb, q_norm_w, k_norm_w,
           ffn_w_gate, ffn_w_up, ffn_w_down, ada_w1, ada_b1, norm_mix1, norm_ffn1, w_in1, conv_w1,
           a_log_fwd, a_log_bwd, dt_bias_fwd, dt_bias_bwd, gdn_norm_w, w_out1, router_w,
           moe_w_gate, moe_w_up, moe_w_down, final_norm_w):
    f32 = lambda a: np.ascontiguousarray(np.asarray(a, dtype=np.float32))
    x, c, ctx, c_ctx = f32(x), f32(c), f32(ctx), f32(c_ctx)
    hc = _host_consts()
    shared = {
        "ident": hc["ident"], "perm": hc["perm"], "cos": hc["cos"], "sin": hc["sin"], "masks": hc["masks"], "sel": hc["sel"], "selE": hc["selE"], "rtc": hc["rtc"],
        "ada_w0": f32(ada_w0), "ada_b0": _chunked(ada_b0, 96), "ada_w1": f32(ada_w1), "ada_b1": _chunked(ada_b1, 96),
        "nmix0": _chunked(norm_mix0), "nffn0": _chunked(norm_ffn0), "nmix1": _chunked(norm_mix1), "nffn1": _chunked(norm_ffn1), "fnw": _chunked(final_norm_w),
        "w_in0": f32(w_in0), "qnw": f32(q_norm_w).reshape(128, 1), "knw": f32(k_norm_w).reshape(128, 1), "gnw": f32(gdn_norm_w).reshape(128, 1),
        "nabias": _na_bias(f32(na_rpb)),
        "w_out0": f32(w_out0), "wg": f32(ffn_w_gate), "wu": f32(ffn_w_up), "wd": f32(ffn_w_down), "w_in1": f32(w_in1),
        "convw": np.ascontiguousarray(f32(conv_w1).reshape(4, 48, 128).transpose(2, 1, 0)),
        "gparam": np.ascontiguousarray(np.stack([f32(a_log_fwd), f32(a_log_bwd), f32(dt_bias_fwd), f32(dt_bias_bwd)], 0)[None].repeat(128, 0)),
        "w_out1": f32(w_out1), "rw": np.ascontiguousarray(f32(router_w).reshape(16, 128, 8).transpose(1, 0, 2)),
        "mwg": f32(moe_w_gate), "mwu": f32(moe_w_up), "mwd": f32(moe_w_down),
    }
    in_maps = []
    for core in range(8):
        b = core % 4
        m = dict(shared)
        m["cT"] = np.ascontiguousarray(np.stack([c[b], c_ctx], -1).reshape(16, 128, 2).transpose(1, 0, 2))
        m["xcT"] = np.ascontiguousarray(np.concatenate([x[b].T, ctx[b].T], axis=1))
        hs = np.zeros((128, 2), np.float32)
        hs[:, core // 4] = 1.0
        m["hsel"] = hs
        in_maps.append(m)
    if "nc" not in _NC_CACHE:
        _NC_CACHE["nc"] = build_nc()
    res = run_bass_kernel_spmd(_NC_CACHE["nc"], in_maps, core_ids=list(range(8)))
    out = np.stack([np.concatenate([np.asarray(res.results[b]["outT"]).T, np.asarray(res.results[b + 4]["outT"]).T], axis=0) for b in range(4)], 0)
    return np.ascontiguousarray(out.astype(np.float32))
```

```python
import numpy as np
import concourse.bass as bass
import concourse.mybir as mybir
from concourse.bass_utils import run_bass_kernel_spmd
from contextlib import ExitStack

F32 = mybir.dt.float32
BF16 = mybir.dt.bfloat16
ALU = mybir.AluOpType
AF = mybir.ActivationFunctionType
AX = mybir.AxisListType

SEM_CAP = 4000
DMA_RING = 6


class Op:
    __slots__ = ("eng", "fn", "deps", "dma", "signal", "sem", "semval", "idx", "ring", "ringprev")

    def __init__(self, eng, fn, dma):
        self.eng = eng
        self.fn = fn
        self.deps = []
        self.dma = dma
        self.signal = dma
        self.sem = None
        self.semval = 0
        self.ring = None
        self.ringprev = 0


class Sched:
    ENGS = ("pe", "act", "dve", "pool", "sp")

    def __init__(self, nc, strict_same_engine=True):
        self.nc = nc
        self.ops = {e: [] for e in self.ENGS}
        self.last_w = {}
        self.readers = {}
        self.strict = strict_same_engine
        self.sb_off = 16512
        self.sb_mark = []
        self.scr = {}
        self.n_alloc = 0

    def sb(self, name, shape, dtype):
        esz = 4 if dtype == F32 else 2
        n = 1
        for s in shape[1:]:
            n *= s
        nbytes = (n * esz + 63) // 64 * 64
        off = self.sb_off
        self.sb_off += nbytes
        assert self.sb_off <= 229344, f"SBUF overflow at {name}: {self.sb_off}"
        self.n_alloc += 1
        return self.nc.alloc_sbuf_tensor_at(f"{name}_{self.n_alloc}", list(shape), dtype, offset=off)

    def push(self):
        self.sb_mark.append((self.sb_off, dict(self.scr)))

    def pop(self, barrier=True):
        if barrier:
            self.barrier()
        self.sb_off, self.scr = self.sb_mark.pop()

    def scratch(self, key, shape, dtype):
        k = (key, tuple(shape), str(dtype))
        if k not in self.scr:
            self.scr[k] = self.sb(str(key), shape, dtype)
        return self.scr[k], ("scr",) + k

    mute = False

    def add(self, eng, fn, reads=(), writes=(), dma=False):
        if self.mute:
            return None
        op = Op(eng, fn, dma)
        deps = []
        for t in reads:
            w = self.last_w.get(t)
            if w is not None:
                deps.append((w, True))
        for t in writes:
            w = self.last_w.get(t)
            if w is not None:
                deps.append((w, False))
            rd = self.readers.get(t)
            if rd:
                for r in rd[0].values():
                    deps.append((r, False))
                for r in rd[1]:
                    deps.append((r, False))
        seen = set()
        for d, raw in deps:
            if d is op or id(d) in seen:
                continue
            if d.eng == eng and not d.dma:
                if eng == "pe" or not self.strict or not raw:
                    continue
            seen.add(id(d))
            op.deps.append(d)
            d.signal = True
        for t in reads:
            rd = self.readers.get(t)
            if rd is None:
                rd = self.readers[t] = ({}, [])
            if dma:
                rd[1].append(op)
            else:
                rd[0][eng] = op
        for t in writes:
            self.last_w[t] = op
            self.readers[t] = ({}, [])
        self.ops[eng].append(op)
        return op

    def pe(self, fn, reads=(), writes=()):
        return self.add("pe", fn, reads, writes)

    def act(self, fn, reads=(), writes=()):
        return self.add("act", fn, reads, writes)

    def dve(self, fn, reads=(), writes=()):
        return self.add("dve", fn, reads, writes)

    def pool(self, fn, reads=(), writes=()):
        return self.add("pool", fn, reads, writes)

    def barrier(self):
        lastc = []
        for e in self.ENGS:
            comp = [o for o in self.ops[e] if not o.dma and o.fn is not None]
            if comp:
                lastc.append(comp[-1])
            dm = [o for o in self.ops[e] if o.dma]
            lastc.extend(dm[-DMA_RING:])
        for e in self.ENGS:
            op = Op(e, None, False)
            for d in lastc:
                if d.eng == e and not d.dma and e == "pe":
                    continue
                op.deps.append(d)
                d.signal = True
            self.ops[e].append(op)
        self.last_w = {}
        self.readers = {}

    def dma(self, q, out, in_, reads=(), writes=(), **kw):
        return self.add(q, lambda e: e.dma_start(out=out, in_=in_, **kw), reads, writes, dma=True)

    def emit(self):
        nc = self.nc
        with ExitStack() as es:
            sem_pool = {}

            self.nsem = 0

            def new_sem(name):
                self.nsem += 1
                return es.enter_context(nc.semaphore(name))

            for e in self.ENGS:
                cnt = 0
                cur = None
                k = 0
                rings = None
                allr = []
                nd = 0
                for op in self.ops[e]:
                    if op.dma:
                        if rings is None:
                            rings = [[new_sem(f"d_{e}_{i}"), 0, None] for i in range(DMA_RING)]
                            nsem_d = DMA_RING
                        r = rings[nd % DMA_RING]
                        if r[1] + 16 > SEM_CAP:
                            allr.append(tuple(r))
                            op.deps.append(r[2])
                            r[0] = new_sem(f"d_{e}_{nsem_d}")
                            nsem_d += 1
                            r[1] = 0
                        nd += 1
                        op.ringprev = r[1]
                        r[1] += 16
                        op.sem = r[0]
                        op.semval = r[1]
                        r[2] = op
                    elif op.signal:
                        if cur is None or cnt >= SEM_CAP:
                            cur = new_sem(f"c_{e}_{k}")
                            k += 1
                            cnt = 0
                        cnt += 1
                        op.sem = cur
                        op.semval = cnt
                sem_pool[e] = (list(map(tuple, rings)) + allr) if rings else None

            def run_engine(e, h):
                waited = {}
                for op in self.ops[e]:
                    for d in op.deps:
                        key = id(d.sem)
                        if waited.get(key, 0) >= d.semval:
                            continue
                        h.wait_ge(d.sem, d.semval)
                        waited[key] = d.semval
                    if op.dma and op.ringprev > 0:
                        key = id(op.sem)
                        if waited.get(key, 0) < op.ringprev:
                            h.wait_ge(op.sem, op.ringprev)
                            waited[key] = op.ringprev
                    if op.fn is None:
                        continue
                    ins = op.fn(h)
                    if op.dma:
                        ins.then_inc(op.sem, 16)
                    elif op.signal:
                        ins.then_inc(op.sem, 1)
                rings = sem_pool.get(e)
                if rings:
                    for s, v, _ in rings:
                        if v > 0 and waited.get(id(s), 0) < v:
                            h.wait_ge(s, v)

            with nc.Block() as block:
                @block.tensor
                def _(h):
                    run_engine("pe", h)

                @block.scalar
                def _(h):
                    run_engine("act", h)

                @block.vector
                def _(h):
                    run_engine("dve", h)

                @block.gpsimd
                def _(h):
                    run_engine("pool", h)

                @block.sync
                def _(h):
                    run_engine("sp", h)

    def count(self):
        return {e: len(v) for e, v in self.ops.items()}
import numpy as np
import ml_dtypes

D = 2048
NCH = 16
NTOK = 4096
NCTX = 256
NU = NTOK + NCTX
EPS = 1e-6
SCALE = 128 ** -0.5


class K:
    def __init__(self, nc):
        self.nc = nc
        self.S = Sched(nc)
        self.ps = [nc.alloc_psum_tensor(f"ps{i}", [128, 512], F32) for i in range(8)]
        self.psn = 0
        self.uid = 0

    def psum(self):
        while True:
            i = self.psn % 8
            self.psn += 1
            if i not in getattr(self, "ps_skip", ()):
                return i

    def u(self):
        self.uid += 1
        return self.uid

    def din(self, name, shape, dt=F32):
        return self.nc.dram_tensor(name, list(shape), dt, kind="ExternalInput").ap()

    def dout(self, name, shape, dt=F32):
        return self.nc.dram_tensor(name, list(shape), dt, kind="ExternalOutput").ap()

    def dscr(self, name, shape, dt=F32):
        return self.nc.dram_tensor(name, list(shape), dt, kind="Internal").ap()

    def load_consts(self, ident_d, perm_d):
        S = self.S
        self.ones_bf = S.sb("ones_bf", [128, 128], BF16)
        self.ones_f = S.sb("ones_f", [128, 128], F32)
        self.ident_bf = S.sb("ident_bf", [128, 128], BF16)
        self.ident_f = S.sb("ident_f", [128, 128], F32)
        self.perm_bf = S.sb("perm_bf", [128, 128], BF16)
        S.dve(lambda e: e.memset(self.ones_bf[:], 1.0), writes=["ones_bf"])
        S.dve(lambda e: e.memset(self.ones_f[:], 1.0), writes=["ones_f"])
        S.dma("sp", self.ident_f[:], ident_d, writes=["ident_f"])
        S.dma("pool", self.ident_bf[:], ident_d, writes=["ident_bf"])
        S.dma("pool", self.perm_bf[:], perm_d, writes=["perm_bf"])
        self.cb = {}
        for c in (float(D * EPS), float(128 * EPS), float(EPS), 1.0):
            t = S.sb("cbias", [128, 1], F32)
            S.dve(lambda e, t=t, c=c: e.memset(t[:], c), writes=[("cbias", c)])
            self.cb[c] = t

    def ada(self, cT_d, W_d, b_d, mod_sb, tag):
        S = self.S
        S.push()
        c_sb = S.sb("c", [128, 16, 2], F32)
        sc = S.sb("sc", [128, 16, 2], F32)
        b_sb = S.sb("b", [128, 96], F32)
        wbuf = [S.sb(f"w{i}", [128, 16, 512], F32) for i in range(2)]
        t = tag
        S.dma("sp", c_sb[:], cT_d, writes=[(t, "c")])
        S.dma("sp", b_sb[:], b_d, writes=[(t, "b")])
        S.act(lambda e: e.activation(out=sc[:], in_=c_sb[:], func=AF.Silu), reads=[(t, "c")], writes=[(t, "sc")])
        Wv = W_d.rearrange("(k p) n -> p k n", p=128)
        for blk in range(24):
            wb = wbuf[blk % 2]
            q = "sp" if blk % 2 == 0 else "pool"
            S.dma(q, wb[:], Wv[:, :, blk * 512:(blk + 1) * 512], writes=[("adaw", blk % 2)])
            for cc in range(4):
                c = blk * 4 + cc
                pi = self.psum()
                pt = self.ps[pi]
                for k in range(16):
                    S.pe(lambda e, pt=pt, wb=wb, k=k, cc=cc: e.matmul(pt[:, 0:2], wb[:, k, cc * 128:(cc + 1) * 128], sc[:, k, :],
                                                                      start=(k == 0), stop=(k == 15)),
                         reads=[("adaw", blk % 2), (t, "sc")], writes=[("ps", pi)])
                S.dve(lambda e, pt=pt, c=c: e.tensor_scalar(out=mod_sb[:, c, :], in0=pt[:, 0:2], scalar1=b_sb[:, c:c + 1], scalar2=None, op0=ALU.add),
                      reads=[("ps", pi), (t, "b")], writes=[(t, "mod")])
        S.pop()

    def mod_vectors(self, mod_sb, nmix_d, nffn_d, tag):
        S = self.S
        v = {}
        nm = S.sb("nm", [128, 16], F32)
        nf = S.sb("nf", [128, 16], F32)
        S.dma("sp", nm[:], nmix_d, writes=[(tag, "nm")])
        S.dma("sp", nf[:], nffn_d, writes=[(tag, "nf")])
        for nme, nw, i_shift, i_scale, i_gate in (("mix", nm, 0, 1, 2), ("ffn", nf, 3, 4, 5)):
            A = S.sb("A", [128, 16, 2], F32)
            for j in range(2):
                S.dve(lambda e, A=A, j=j, i_scale=i_scale: e.tensor_scalar(out=A[:, :, j], in0=mod_sb[:, i_scale * 16:(i_scale + 1) * 16, j],
                                                                            scalar1=1.0, scalar2=float(np.sqrt(D)), op0=ALU.add, op1=ALU.mult),
                      reads=[(tag, "mod")], writes=[(tag, "A" + nme)])
                S.dve(lambda e, A=A, j=j, nw=nw: e.tensor_tensor(out=A[:, :, j], in0=A[:, :, j], in1=nw[:], op=ALU.mult),
                      reads=[(tag, "nm"), (tag, "nf"), (tag, "A" + nme)], writes=[(tag, "A" + nme)])
            v["A" + nme] = (A, (tag, "A" + nme))
            v["B" + nme] = (mod_sb[:, i_shift * 16:(i_shift + 1) * 16, :], (tag, "mod"))
            v["G" + nme] = (mod_sb[:, i_gate * 16:(i_gate + 1) * 16, :], (tag, "mod"))
        return v

    def rsqrt(self, out_ap, in_ap, c, reads, wtok):
        S = self.S
        cb = self.cb[c]
        np_ = out_ap.shape[0]
        S.act(lambda e: e.activation(out=out_ap, in_=in_ap, func=AF.Sqrt, bias=cb[0:np_, 0:1]), reads=list(reads) + [("cbias", c)], writes=[wtok])
        S.dve(lambda e: e.reciprocal(out=out_ap, in_=out_ap), reads=[wtok], writes=[wtok])

    def rms_mod(self, x_sb, x_tok, T, segs, A, B, h_sb, h_off, h_tok, extra_out=None, x_off=0):
        S = self.S
        sqs = [S.scratch(("rm_sq", i), [128, T], BF16) for i in range(2)]
        rstd, rstd_t = S.scratch("rm_rstd", [128, T], F32)
        tmps = [S.scratch(("rm_tmp", i), [128, T], F32) for i in range(2)]
        pi = self.psum()
        pt = self.ps[pi]
        for c in range(16):
            s, s_t = sqs[c % 2]
            S.act(lambda e, s=s, c=c: e.activation(out=s[:], in_=x_sb[:, c, x_off:x_off + T], func=AF.Square),
                  reads=[x_tok], writes=[s_t])
            S.pe(lambda e, s=s, c=c: e.matmul(pt[:, 0:T], self.ones_bf[:], s[:], start=(c == 0), stop=(c == 15)),
                 reads=[s_t, "ones_bf"], writes=[("ps", pi)])
        self.rsqrt(rstd[:], pt[:, 0:T], float(D * EPS), [("ps", pi)], rstd_t)
        for c in range(16):
            tm, tm_t = tmps[c % 2]
            S.dve(lambda e, tm=tm, c=c: e.tensor_tensor(out=tm[:], in0=x_sb[:, c, x_off:x_off + T], in1=rstd[:], op=ALU.mult),
                  reads=[x_tok, rstd_t], writes=[tm_t])
            for (c0, c1, j) in segs:
                S.act(lambda e, tm=tm, c=c, c0=c0, c1=c1, j=j: e.activation(out=h_sb[:, c, h_off + c0:h_off + c1], in_=tm[:, c0:c1], func=AF.Identity,
                                                                               bias=B[0][:, c, j:j + 1], scale=A[0][:, c, j:j + 1]),
                      reads=[tm_t, A[1], B[1]], writes=[h_tok])
                if extra_out is not None:
                    extra_out(c, c0, c1, j, tm, tm_t)

    def load_vec(self, d_ap, shape, tok, dt=F32, q="sp"):
        t = self.S.sb("vec", shape, dt)
        self.S.dma(q, t[:], d_ap, writes=[tok])
        return t

    def inproj0(self, xcT_d, w_in_d, mv, qnw_d, knw_d, cos_d, sin_d, o):
        S = self.S
        S.push()
        h_all = S.sb("h_all", [128, 16, NU], BF16)
        qnw = self.load_vec(qnw_d, [128, 1], "qnw")
        knw = self.load_vec(knw_d, [128, 1], "knw")
        S.dve(lambda e: e.tensor_scalar(out=qnw[:], in0=qnw[:], scalar1=float(np.sqrt(128.0)), scalar2=None, op0=ALU.mult), reads=["qnw"], writes=["qnw"])
        S.dve(lambda e: e.tensor_scalar(out=knw[:], in0=knw[:], scalar1=float(np.sqrt(128.0)), scalar2=None, op0=ALU.mult), reads=["knw"], writes=["knw"])
        S.push()
        xs = [S.sb("xs", [128, 16, 256], F32) for _ in range(2)]
        xv = xcT_d.rearrange("(k p) t -> p k t", p=128)
        for i in range(NU // 256):
            x_sb = xs[i % 2]
            S.dma("sp", x_sb[:], xv[:, :, i * 256:(i + 1) * 256], writes=[("xs", i % 2)])
            j = 0 if i < NTOK // 256 else 1
            self.rms_mod(x_sb, ("xs", i % 2), 256, [(0, 256, j)], mv["Amix"], mv["Bmix"], h_all, i * 256, ("h_all", i))
        S.pop()
        wbuf = [S.sb("wb", [128, 16, 512], BF16) for _ in range(2)]
        st = [S.sb("st", [128, 512], BF16) for _ in range(4)]
        sq = S.sb("sq", [128, 512], BF16)
        rstd = S.sb("rstd", [128, 512], F32)
        yn = S.sb("yn", [128, 512], BF16)
        t1 = S.sb("t1", [128, 512], F32)
        t2 = S.sb("t2", [128, 512], F32)
        cs = S.sb("cos", [128, 512], F32)
        sn = S.sb("sin", [128, 512], F32)
        Wv = w_in_d.rearrange("(k p) n -> p k n", p=128)
        tiles = [(i * 512, 512, False) for i in range(8)] + [(NTOK, 256, True)]
        nst = [0]

        def h_toks(t0, T):
            return [("h_all", i) for i in range(t0 // 256, (t0 + T) // 256)]

        def next_st():
            i = nst[0] % 4
            nst[0] += 1
            return i

        def fm_chunk(wb, wtok, cc, t0, T):
            pi = self.psum()
            pt = self.ps[pi]
            for k in range(16):
                S.pe(lambda e, pt=pt, wb=wb, k=k, cc=cc, t0=t0, T=T: e.matmul(pt[:, 0:T], wb[:, k, cc * 128:(cc + 1) * 128], h_all[:, k, t0:t0 + T],
                                                                              start=(k == 0), stop=(k == 15)),
                     reads=[wtok] + h_toks(t0, T), writes=[("ps", pi)])
            return pi

        for blk in range(9):
            wb = wbuf[blk % 2]
            wtok = ("wb", blk % 2)
            S.dma("pool", wb[:], Wv[:, :, blk * 512:(blk + 1) * 512], writes=[wtok])
            if blk in (4, 5) or blk == 8:
                if blk == 8:
                    c0, N, dst, dc0 = 256, 256, o["vg"], 0
                else:
                    c0, N, dst, dc0 = 0, 512, o["vna"], (blk - 4) * 512
                for s in range(NU // 128):
                    pi = self.psum()
                    pt = self.ps[pi]
                    for k in range(16):
                        S.pe(lambda e, pt=pt, wb=wb, k=k, s=s, c0=c0, N=N: e.matmul(pt[:, 0:N], h_all[:, k, s * 128:(s + 1) * 128], wb[:, k, c0:c0 + N],
                                                                                    start=(k == 0), stop=(k == 15)),
                             reads=[wtok, ("h_all", s // 2)], writes=[("ps", pi)])
                    si = next_st()
                    eng = S.act if s % 2 == 0 else S.dve
                    if s % 2 == 0:
                        S.act(lambda e, pt=pt, si=si, N=N: e.copy(out=st[si][:, 0:N], in_=pt[:, 0:N]), reads=[("ps", pi)], writes=[("st", si)])
                    else:
                        S.dve(lambda e, pt=pt, si=si, N=N: e.tensor_copy(out=st[si][:, 0:N], in_=pt[:, 0:N]), reads=[("ps", pi)], writes=[("st", si)])
                    S.dma("sp", dst[s * 128:(s + 1) * 128, dc0:dc0 + N], st[si][:, 0:N], reads=[("st", si)])
            if blk in (0, 1, 2, 3):
                for (t0, T, isc) in tiles:
                    for cc in range(4):
                        pi = fm_chunk(wb, wtok, cc, t0, T)
                        pt = self.ps[pi]
                        si = next_st()
                        hh = (blk % 2) * 4 + cc
                        if blk < 2:
                            S.act(lambda e, pt=pt, si=si, T=T: e.mul(out=st[si][:, 0:T], in_=pt[:, 0:T], mul=float(SCALE)), reads=[("ps", pi)], writes=[("st", si)])
                            dst = o["qna"]
                        else:
                            S.dve(lambda e, pt=pt, si=si, T=T: e.tensor_copy(out=st[si][:, 0:T], in_=pt[:, 0:T]), reads=[("ps", pi)], writes=[("st", si)])
                            dst = o["kna"]
                        S.dma("sp", dst[hh, :, t0:t0 + T], st[si][:, 0:T], reads=[("st", si)])
            if blk in (6, 7, 8):
                ncc = 4 if blk < 8 else 2
                for (t0, T, isc) in tiles:
                    if not isc:
                        S.dma("sp", cs[:, 0:T], cos_d[:, t0:t0 + T], writes=["cos"])
                        S.dma("sp", sn[:, 0:T], sin_d[:, t0:t0 + T], writes=["sin"])
                    for cc in range(ncc):
                        pi = fm_chunk(wb, wtok, cc, t0, T)
                        pt = self.ps[pi]
                        si = next_st()
                        if blk < 8:
                            hh, dst, nw, nwt = (blk - 6) * 4 + cc, o["qg"], qnw, "qnw"
                        else:
                            hh, dst, nw, nwt = cc, o["kg"], knw, "knw"
                        S.act(lambda e, pt=pt, T=T: e.activation(out=sq[:, 0:T], in_=pt[:, 0:T], func=AF.Square), reads=[("ps", pi)], writes=["sq_r"])
                        p2 = self.psum()
                        pt2 = self.ps[p2]
                        S.pe(lambda e, pt2=pt2, T=T: e.matmul(pt2[:, 0:T], self.ones_bf[:], sq[:, 0:T], start=True, stop=True),
                             reads=["sq_r", "ones_bf"], writes=[("ps", p2)])
                        self.rsqrt(rstd[:, 0:T], pt2[:, 0:T], float(128 * EPS), [("ps", p2)], "rstd_r")
                        ydst = st[si] if isc else yn
                        ytok = ("st", si) if isc else "yn"
                        S.dve(lambda e, pt=pt, T=T, ydst=ydst, nw=nw: e.scalar_tensor_tensor(out=ydst[:, 0:T], in0=pt[:, 0:T], scalar=nw[:, 0:1], in1=rstd[:, 0:T],
                                                                                             op0=ALU.mult, op1=ALU.mult),
                              reads=[("ps", pi), "rstd_r", nwt], writes=[ytok])
                        if not isc:
                            p3 = self.psum()
                            pt3 = self.ps[p3]
                            S.pe(lambda e, pt3=pt3, T=T: e.matmul(pt3[:, 0:T], self.perm_bf[:], yn[:, 0:T], start=True, stop=True),
                                 reads=["yn", "perm_bf"], writes=[("ps", p3)])
                            S.pool(lambda e, T=T: e.tensor_tensor(out=t1[:, 0:T], in0=yn[:, 0:T], in1=cs[:, 0:T], op=ALU.mult), reads=["yn", "cos"], writes=["t1"])
                            S.dve(lambda e, pt3=pt3, T=T: e.tensor_tensor(out=t2[:, 0:T], in0=pt3[:, 0:T], in1=sn[:, 0:T], op=ALU.mult), reads=[("ps", p3), "sin"], writes=["t2"])
                            S.pool(lambda e, si=si, T=T: e.tensor_tensor(out=st[si][:, 0:T], in0=t1[:, 0:T], in1=t2[:, 0:T], op=ALU.add), reads=["t1", "t2"], writes=[("st", si)])
                        S.dma("sp", dst[hh, :, t0:t0 + T], st[si][:, 0:T], reads=[("st", si)])
        S.pop()

    def psum_pool(self, pool, cnt):
        i = pool[cnt[0] % len(pool)]
        cnt[0] += 1
        return i

    def attn_gqa(self, qg_d, kg_d, vg_d, attnT_d):
        S = self.S
        S.push()
        KT = [S.sb("KT", [128, NU], BF16) for _ in range(2)]
        V = [S.sb("V", [128, NU // 128, 128], BF16) for _ in range(2)]
        QT = [S.sb("QT", [128, NU], BF16) for _ in range(2)]
        P = [S.sb("P", [128, 512], BF16) for _ in range(3)]
        rinv = S.sb("rinv", [128, 512], F32)
        st = [S.sb("st", [128, 512], BF16) for _ in range(2)]
        c_o, c_m, c_s, c_p, c_st = [0], [0], [0], [0], [0]
        qtiles = [(i * 512, 512, list(range(NU // 128))) for i in range(8)] + [(NTOK, 256, [32, 33])]
        for g in range(2):
            kt_sb, v_sb = KT[g % 2], V[g % 2]
            S.dma("sp", kt_sb[:], kg_d[g], writes=[("KT", g % 2)])
            S.dma("sp", v_sb[:], vg_d[:, g * 128:(g + 1) * 128].rearrange("(n p) d -> p n d", p=128), writes=[("V", g % 2)])
            for hq in range(4):
                h = g * 4 + hq
                q_sb = QT[h % 2]
                S.dma("sp", q_sb[:], qg_d[h], writes=[("QT", h % 2)])
                for (t0, T, ktl) in qtiles:
                    po = self.psum_pool([0, 1], c_o)
                    pm = self.psum_pool([2, 3], c_m)
                    for n, kt in enumerate(ktl):
                        pss = self.psum_pool([4, 5, 6, 7], c_s)
                        pi_ = c_p[0] % 3
                        c_p[0] += 1
                        S.pe(lambda e, pss=pss, kt=kt, t0=t0, T=T, kt_sb=kt_sb, q_sb=q_sb: e.matmul(self.ps[pss][:, 0:T], kt_sb[:, kt * 128:(kt + 1) * 128], q_sb[:, t0:t0 + T], start=True, stop=True),
                             reads=[("KT", g % 2), ("QT", h % 2)], writes=[("ps", pss)])
                        S.act(lambda e, pss=pss, pi_=pi_, T=T: e.activation(out=P[pi_][:, 0:T], in_=self.ps[pss][:, 0:T], func=AF.Exp, scale=float(SCALE)),
                              reads=[("ps", pss)], writes=[("P", pi_)])
                        S.pe(lambda e, po=po, kt=kt, pi_=pi_, T=T, n=n, v_sb=v_sb, L=len(ktl): e.matmul(self.ps[po][:, 0:T], v_sb[:, kt, :], P[pi_][:, 0:T], start=(n == 0), stop=(n == L - 1)),
                             reads=[("V", g % 2), ("P", pi_)], writes=[("ps", po)])
                        S.pe(lambda e, pm=pm, pi_=pi_, T=T, n=n, L=len(ktl): e.matmul(self.ps[pm][:, 0:T], self.ones_bf[:], P[pi_][:, 0:T], start=(n == 0), stop=(n == L - 1)),
                             reads=[("P", pi_)], writes=[("ps", pm)])
                    si = c_st[0] % 2
                    c_st[0] += 1
                    S.dve(lambda e, pm=pm, T=T: e.reciprocal(out=rinv[:, 0:T], in_=self.ps[pm][:, 0:T]), reads=[("ps", pm)], writes=["rinv"])
                    S.dve(lambda e, po=po, si=si, T=T: e.tensor_tensor(out=st[si][:, 0:T], in0=self.ps[po][:, 0:T], in1=rinv[:, 0:T], op=ALU.mult),
                          reads=[("ps", po), "rinv"], writes=[("st", si)])
                    S.dma("pool", attnT_d[(8 + h) * 128:(9 + h) * 128, t0:t0 + T], st[si][:, 0:T], reads=[("st", si)])
        S.pop()

    def attn_na(self, qna_d, kna_d, vna_d, bias_d, attnT_d):
        S = self.S
        S.push()
        KT = [S.sb("KT", [128, NU], BF16) for _ in range(2)]
        V = [S.sb("V", [128, NU // 128, 128], BF16) for _ in range(2)]
        QT = [S.sb("QT", [128, NU], BF16) for _ in range(2)]
        bias = S.sb("bias", [128, 5, 8, 5, 128], BF16)
        S.dma("sp", bias[:], bias_d, writes=["bias"])
        P1 = [S.sb("P1", [128, 512], BF16) for _ in range(2)]
        P2 = [S.sb("P2", [128, 384], BF16) for _ in range(2)]
        rinv = S.sb("rinv", [128, 128], F32)
        st = [S.sb("st", [128, 512], BF16) for _ in range(2)]
        c_o, c_m, c_s, c_p = [0], [0], [0], [0]
        c_s2 = [0]
        for h in range(8):
            kt_sb, v_sb, q_sb = KT[h % 2], V[h % 2], QT[h % 2]
            S.dma("sp", kt_sb[:], kna_d[h], writes=[("KT", h % 2)])
            S.dma("sp", v_sb[:], vna_d[:, h * 128:(h + 1) * 128].rearrange("(n p) d -> p n d", p=128), writes=[("V", h % 2)])
            S.dma("sp", q_sb[:], qna_d[h], writes=[("QT", h % 2)])
            rd = [("KT", h % 2), ("QT", h % 2)]
            for lb in range(34):
                isc = lb >= 32
                if not isc:
                    typ = {0: 0, 1: 1, 30: 3, 31: 4}.get(lb, 2)
                    tile0 = min(max(lb - 2, 0), 27)
                    keys = [(tile0 + j, j) for j in range(5)] + [(32, None), (33, None)]
                else:
                    keys = [(32, None), (33, None)]
                s1 = self.psum_pool([4, 5], c_s)
                s2 = self.psum_pool([6, 7], c_s2)
                pi_ = c_p[0] % 2
                c_p[0] += 1
                q0 = lb * 128
                slots = []
                for n, (kt, bj) in enumerate(keys):
                    if isc:
                        bank, col = s1, n * 128
                    else:
                        bank, col = (s1, n * 128) if n < 4 else (s2, (n - 4) * 128)
                    slots.append((bank, col))
                    S.pe(lambda e, bank=bank, col=col, kt=kt, q0=q0, bj=bj, kt_sb=kt_sb, q_sb=q_sb: e.matmul(self.ps[bank][:, col:col + 128], kt_sb[:, kt * 128:(kt + 1) * 128], q_sb[:, q0:q0 + 128],
                                                                                                              start=True, stop=(bj is None)),
                         reads=rd, writes=[("ps", bank)])
                    if bj is not None:
                        S.pe(lambda e, bank=bank, col=col, typ=typ, bj=bj, h=h: e.matmul(self.ps[bank][:, col:col + 128], self.ident_bf[:], bias[:, typ, h, bj, :], start=False, stop=True),
                             reads=["bias", "ident_bf"], writes=[("ps", bank)])
                if isc:
                    S.act(lambda e, s1=s1, pi_=pi_: e.activation(out=P1[pi_][:, 0:256], in_=self.ps[s1][:, 0:256], func=AF.Exp), reads=[("ps", s1)], writes=[("P1", pi_)])
                else:
                    S.act(lambda e, s1=s1, pi_=pi_: e.activation(out=P1[pi_][:], in_=self.ps[s1][:, 0:512], func=AF.Exp), reads=[("ps", s1)], writes=[("P1", pi_)])
                    S.act(lambda e, s2=s2, pi_=pi_: e.activation(out=P2[pi_][:], in_=self.ps[s2][:, 0:384], func=AF.Exp), reads=[("ps", s2)], writes=[("P2", pi_)])
                po = self.psum_pool([0, 1], c_o)
                pm = self.psum_pool([2, 3], c_m)
                L = len(keys)
                for n, (kt, bj) in enumerate(keys):
                    if isc or n < 4:
                        pap, ptok = P1[pi_][:, n * 128:(n + 1) * 128], ("P1", pi_)
                    else:
                        pap, ptok = P2[pi_][:, (n - 4) * 128:(n - 3) * 128], ("P2", pi_)
                    S.pe(lambda e, po=po, kt=kt, pap=pap, n=n, L=L, v_sb=v_sb: e.matmul(self.ps[po][:, 0:128], v_sb[:, kt, :], pap, start=(n == 0), stop=(n == L - 1)),
                         reads=[("V", h % 2), ptok], writes=[("ps", po)])
                    S.pe(lambda e, pm=pm, pap=pap, n=n, L=L: e.matmul(self.ps[pm][:, 0:128], self.ones_bf[:], pap, start=(n == 0), stop=(n == L - 1)),
                         reads=[ptok], writes=[("ps", pm)])
                si = (lb // 4) % 2
                cc = (lb % 4) * 128
                S.dve(lambda e, pm=pm: e.reciprocal(out=rinv[:], in_=self.ps[pm][:, 0:128]), reads=[("ps", pm)], writes=["rinv"])
                S.dve(lambda e, po=po, si=si, cc=cc: e.tensor_tensor(out=st[si][:, cc:cc + 128], in0=self.ps[po][:, 0:128], in1=rinv[:], op=ALU.mult),
                      reads=[("ps", po), "rinv"], writes=[("st", si)])
                if lb % 4 == 3:
                    S.dma("pool", attnT_d[h * 128:(h + 1) * 128, (lb - 3) * 128:(lb + 1) * 128], st[si][:], reads=[("st", si)])
                elif lb == 33:
                    S.dma("pool", attnT_d[h * 128:(h + 1) * 128, 32 * 128:34 * 128], st[si][:, 0:256], reads=[("st", si)])
        S.pop()

    def linear_fm(self, w_d, ncols, h_sb, h_tok, T, evac, wq="pool", blk=512, wkey="lw"):
        S = self.S
        Wv = w_d.rearrange("(k p) n -> p k n", p=128)
        nblk = (ncols + blk - 1) // blk
        wbs = [S.scratch((wkey, i), [128, 16, blk], BF16) for i in range(2)]
        for b in range(nblk):
            wb, wtok = wbs[b % 2]
            c0 = b * blk
            bw = min(blk, ncols - c0)
            S.dma(wq, wb[:, :, 0:bw], Wv[:, :, c0:c0 + bw], writes=[wtok])
            for cc in range((bw + 127) // 128):
                M = min(128, bw - cc * 128)
                for ho in range(0, T, 512):
                    Th = min(512, T - ho)
                    pi = self.psum()
                    pt = self.ps[pi]
                    for k in range(16):
                        S.pe(lambda e, pt=pt, wb=wb, k=k, cc=cc, M=M, ho=ho, Th=Th: e.matmul(pt[0:M, 0:Th], wb[:, k, cc * 128:cc * 128 + M], h_sb[:, k, ho:ho + Th],
                                                                                               start=(k == 0), stop=(k == 15)),
                             reads=[wtok, h_tok], writes=[("ps", pi)])
                    evac(c0 // 128 + cc, M, ho, Th, pi)

    def mlp0(self, xcT_d, attnT_d, w_out_d, mv0, wg_d, wu_d, wd_d, mv1, w_in1_d, x1T_d, p1T_d, tiles=None):
        S = self.S
        FF = 5632
        FB = 256
        xv = xcT_d.rearrange("(k p) t -> p k t", p=128)
        av = attnT_d.rearrange("(k p) t -> p k t", p=128)
        x1v = x1T_d.rearrange("(k p) t -> p k t", p=128)
        if tiles is None:
            tiles = [(i * 1024, 1024, 0) for i in range(3)] + [(3072, 1280, 0)]
        S.push()
        xt = S.sb("xt", [128, 16, 1280], F32)
        hb = S.sb("hb", [128, 16, 1280], BF16)
        for (t0, T, j) in tiles:
            S.dma("sp", xt[:, :, 0:T], xv[:, :, t0:t0 + T], writes=["xt"])
            S.dma("sp", hb[:, :, 0:T], av[:, :, t0:t0 + T], writes=["hb"])

            def make_res_evac(G, j=j, t0=t0):
                def ev(c, M, ho, Th, pi):
                    j = 1 if t0 + ho >= NTOK else 0
                    S.dve(lambda e: e.scalar_tensor_tensor(out=xt[:, c, ho:ho + Th], in0=self.ps[pi][:, 0:Th], scalar=G[0][:, c, j:j + 1], in1=xt[:, c, ho:ho + Th],
                                                           op0=ALU.mult, op1=ALU.add),
                          reads=[("ps", pi), G[1], "xt"], writes=["xt"])
                return ev
            self.linear_fm(w_out_d, 2048, hb, "hb", T, make_res_evac(mv0["Gmix"]), wkey="wg", blk=256)
            for ho in range(0, T, 512):
                Th = min(512, T - ho)
                self.rms_mod(xt, "xt", Th, [(0, Th, 1 if t0 + ho >= NTOK else 0)], mv0["Affn"], mv0["Bffn"], hb, ho, "hb", x_off=ho)
            self.ffn_blocks(hb, "hb", T, wg_d, wu_d, wd_d, make_res_evac(mv0["Gffn"]))
            S.dma("sp", x1v[:, :, t0:t0 + T], xt[:, :, 0:T], reads=["xt"])
            for ho in range(0, T, 512):
                Th = min(512, T - ho)
                self.rms_mod(xt, "xt", Th, [(0, Th, 1 if t0 + ho >= NTOK else 0)], mv1["Amix"], mv1["Bmix"], hb, ho, "hb", x_off=ho)
            sts = [S.scratch(("pst", i), [128, 512], F32) for i in range(2)]
            nst = [0]

            def ev_p1(c, M, ho, Th, pi):
                st, st_t = sts[nst[0] % 2]
                nst[0] += 1
                if nst[0] % 2 == 0:
                    S.act(lambda e: e.copy(out=st[0:M, 0:Th], in_=self.ps[pi][0:M, 0:Th]), reads=[("ps", pi)], writes=[st_t])
                else:
                    S.dve(lambda e: e.tensor_copy(out=st[0:M, 0:Th], in_=self.ps[pi][0:M, 0:Th]), reads=[("ps", pi)], writes=[st_t])
                S.dma("sp", p1T_d[c * 128:c * 128 + M, t0 + ho:t0 + ho + Th], st[0:M, 0:Th], reads=[st_t])
            self.linear_fm(w_in1_d, 8256, hb, "hb", T, ev_p1, wkey="wg", blk=256)
        S.pop()

    def gdn_consts(self, convw_d, gparam_d, masks_d, sel_d):
        S = self.S
        self.g_convw = self.load_vec(convw_d, [128, 48, 4], "g_convw")
        self.g_param = self.load_vec(gparam_d, [128, 4, 16], "g_param")
        self.g_masks = self.load_vec(masks_d, [128, 4, 128], "g_masks")
        self.g_sel = self.load_vec(sel_d, [128, 6, 128], "g_sel")
        self.g_rowm = S.sb("rowm", [128, 2], F32)
        S.dve(lambda e: e.memset(self.g_rowm[:], 0.0), writes=["g_rowm"])
        S.dve(lambda e: e.memset(self.g_rowm[0:64, 0:1], 1.0), writes=["g_rowm"])
        S.dve(lambda e: e.memset(self.g_rowm[64:128, 1:2], 1.0), writes=["g_rowm"])
        self.g_negA = S.sb("negA", [128, 2, 16], F32)
        S.act(lambda e: e.activation(out=self.g_negA[:], in_=self.g_param[:, 0:2, :], func=AF.Exp), reads=["g_param"], writes=["g_negA"])
        S.dve(lambda e: e.tensor_scalar(out=self.g_negA[:], in0=self.g_negA[:], scalar1=-1.0, scalar2=None, op0=ALU.mult), reads=["g_negA"], writes=["g_negA"])

    def gdn_pass(self, p1T_d, d, o_d, units=None, state_out_d=None, cache_d=None, cache_mode=None):
        S = self.S
        S.push()
        pv = p1T_d[0:6144, :].rearrange("(c p) t -> p c t", p=128)
        ident_f, ones_f = self.ident_f, self.ones_f

        def bc_h(ap2):
            n = ap2.shape[1]
            return ap2.rearrange("p (h o) -> p h o", o=1).broadcast_to([128, n, 128])

        def bc_m(ap2, n=4):
            return ap2.rearrange("p (o f) -> p o f", o=1).broadcast_to([128, n, 128])

        MLk, MUk, SLk, SUk = (self.g_masks[:, i, :] for i in range(4))
        if d == 0:
            tri, strict, inclT, selL, selC = MUk, SLk, MUk, self.g_sel[:, 0, :], (self.g_sel[:, 2, :], self.g_sel[:, 3, :])
            order = (0, 1)
        else:
            tri, strict, inclT, selL, selC = MLk, SUk, MLk, self.g_sel[:, 1, :], (self.g_sel[:, 4, :], self.g_sel[:, 5, :])
            order = (1, 0)
        if units is None:
            cu = [(NTOK + 128 * i, NTOK, NU, False) for i in range(2)]
            xu = [(128 * i, 0, NTOK, True) for i in range(32)]
            units = cu + xu if d == 0 else cu[::-1] + xu[::-1]
        St = S.sb("St", [128, 16, 128], F32)
        Sb = S.sb("Sb", [128, 16, 128], BF16)
        S.dve(lambda e: e.memset(St[:], 0.0), writes=[("St", hg) for hg in range(4)])
        S.dve(lambda e: e.memset(Sb[:], 0.0), writes=[("Sb", hg) for hg in range(4)])
        vn = [S.sb("vn", [128, 4, 128], BF16) for _ in range(4)]
        for i in range(4):
            S.dve(lambda e, i=i: e.memset(vn[i][:], 0.0), writes=[("vn", i)])
        P = [S.sb("P", [128, 48, 131], F32) for _ in range(1)]
        Y = S.sb("Y", [128, 48, 128], F32)
        ctmp2 = [S.sb("ctmp", [128, 6, 128], F32) for _ in range(2)]
        qT = S.sb("qT", [128, 16, 128], BF16)
        kT = S.sb("kT", [128, 16, 128], BF16)
        k_tm = S.sb("k_tm", [128, 16, 128], BF16)
        v_tm = S.sb("v_tm", [128, 16, 128], BF16)
        sq4 = S.sb("sq4", [128, 512], BF16)
        rstd4 = S.sb("rstd4", [128, 512], F32)
        gbT = S.sb("gbT", [64, 128], F32)
        beta = S.sb("beta", [128, 16], F32)
        xa = S.sb("xa", [128, 16], F32)
        g = S.sb("g", [128, 16], F32)
        gc = S.sb("gc", [128, 16], F32)
        kdsc = S.sb("kdsc", [128, 16], F32)
        kdsc01 = S.sb("kdsc01", [128, 2, 16], F32)
        scw = S.sb("scw", [128, 16], F32)
        egl = S.sb("egl", [128, 2, 16], F32)
        o_sb = [S.sb("o_sb", [128, 16, 128], F32) for _ in range(1)]
        FS = [[S.sb("fs", [128, 4, 128], F32) for _ in range(8)] for _ in range(4)]
        BS = [[S.sb("bs", [128, 4, 128], BF16) for _ in range(8)] for _ in range(4)]
        nunit = 0
        import os
        stop = int(os.environ.get('GDN_STOP', '99'))
        sub = int(os.environ.get('GDN_SUB', '99'))
        for (t0, slo, shi, has_o) in units:
            S.dma("sp", gbT[:], p1T_d[8192:8256, t0:t0 + 128], writes=["gbT"])
            cslot = (t0 // 128) if t0 < NTOK else 32 + (t0 - NTOK) // 128
            if cache_mode == "read":
                for qi, (buf, tokn) in enumerate(((qT, "qT"), (kT, "kT"), (k_tm, "k_tm"), (v_tm, "v_tm"))):
                    S.dma("sp", buf[:], cache_d[cslot, qi], writes=[tokn])
            else:
                Pb = P[0]
                ptok = ("P", 0)
                lo, hi = max(t0 - 2, slo), min(t0 + 129, shi)
                if lo > t0 - 2 or hi < t0 + 129:
                    S.pool(lambda e, Pb=Pb: e.memset(Pb[:], 0.0), writes=[ptok])
                for cg in range(6):
                    S.dma("sp", Pb[:, cg * 8:(cg + 1) * 8, lo - (t0 - 2):hi - (t0 - 2)], pv[:, cg * 8:(cg + 1) * 8, lo:hi], writes=[ptok])
                cw = self.g_convw
                for c in range(48):
                    S.act(lambda e, Pb=Pb, c=c: e.activation(out=Y[:, c, :], in_=Pb[:, c, 0:128], func=AF.Copy, scale=cw[:, c, 0:1]), reads=[ptok, "g_convw"], writes=[("Y", c // 6), "Y"] + [("Yk", q) for q in range(4, 8)])
                nprod = 0
                for cq in range(8):
                    cs_ = slice(cq * 6, (cq + 1) * 6)
                    for i in range(1, 4):
                        ct, ctt = ctmp2[nprod % 2], ("ctmp", nprod % 2)
                        eng = S.dve if nprod % 4 == 3 else S.pool
                        eng(lambda e, Pb=Pb, i=i, cs_=cs_, ct=ct: e.tensor_tensor(out=ct[:], in0=Pb[:, cs_, i:i + 128], in1=bc_h(cw[:, cs_, i]), op=ALU.mult), reads=[ptok, "g_convw"], writes=[ctt])
                        S.dve(lambda e, cs_=cs_, ct=ct: e.tensor_tensor(out=Y[:, cs_, :], in0=Y[:, cs_, :], in1=ct[:], op=ALU.add), reads=[("Y", cq), ctt], writes=[("Y", cq)])
                        nprod += 1
                YT = [("Y", i) for i in range(8)]
                S.act(lambda e: e.activation(out=Y[:], in_=Y[:], func=AF.Silu), reads=YT, writes=["Y"])
                for c4 in range(8):
                    ysl = Y[:, c4 * 4:(c4 + 1) * 4, :]
                    if c4 % 2 == 0:
                        sqb, sq_t, rsb, rs_t = sq4, "sq4", rstd4, "rstd4"
                    else:
                        sqb, sq_t = BS[3][1][:].rearrange("p h t -> p (h t)"), ("Ybf", 3)
                        rsb, rs_t = FS[3][7][:].rearrange("p h t -> p (h t)"), ("H", 3)
                    S.act(lambda e, ysl=ysl, sqb=sqb: e.activation(out=sqb[:].rearrange("p (h t) -> p h t", h=4), in_=ysl, func=AF.Square), reads=["Y"], writes=[sq_t])
                    pi = self.psum()
                    S.pe(lambda e, pi=pi, sqb=sqb: e.matmul(self.ps[pi][:, :], self.ones_bf[:], sqb[:], start=True, stop=True), reads=[sq_t], writes=[("ps", pi)])
                    self.rsqrt(rsb[:], self.ps[pi][:, :], float(EPS), [("ps", pi)], rs_t)
                    r4 = rsb[:].rearrange("p (h t) -> p h t", h=4)
                    if c4 < 4:
                        S.dve(lambda e, ysl=ysl, c4=c4, r4=r4: e.scalar_tensor_tensor(out=qT[:, c4 * 4:(c4 + 1) * 4, :], in0=ysl, scalar=float(SCALE), in1=r4, op0=ALU.mult, op1=ALU.mult),
                              reads=["Y", rs_t], writes=["qT"])
                    else:
                        S.dve(lambda e, ysl=ysl, r4=r4: e.tensor_tensor(out=ysl, in0=ysl, in1=r4, op=ALU.mult), reads=["Y", rs_t], writes=[("Yk", c4)])
                        S.act(lambda e, ysl=ysl, c4=c4: e.copy(out=kT[:, (c4 - 4) * 4:(c4 - 3) * 4, :], in_=ysl), reads=[("Yk", c4)], writes=["kT"])
                for which, base, dst, dtok in (("k", 16, k_tm, "k_tm"), ("v", 32, v_tm, "v_tm")):
                    for h4 in range(4):
                        pi = self.psum()
                        for i in range(4):
                            S.pe(lambda e, pi=pi, i=i, base=base, h4=h4: e.matmul(self.ps[pi][:, i * 128:(i + 1) * 128], Y[:, base + h4 * 4 + i, :], ident_f[:], start=True, stop=True),
                                 reads=["Y"] + [("Yk", c) for c in range(4, 8)], writes=[("ps", pi)])
                        S.act(lambda e, pi=pi, dst=dst, h4=h4: e.copy(out=dst[:, h4 * 4:(h4 + 1) * 4, :], in_=self.ps[pi][:, :].rearrange("p (h t) -> p h t", h=4)),
                              reads=[("ps", pi)], writes=[dtok])

                if cache_mode == "write":
                    for qi, (buf, tokn) in enumerate(((qT, "qT"), (kT, "kT"), (k_tm, "k_tm"), (v_tm, "v_tm"))):
                        S.dma("sp", cache_d[cslot, qi], buf[:], reads=[tokn])
            pg = self.psum()
            S.pe(lambda e, pg=pg: e.matmul(self.ps[pg][:, 0:64], gbT[:], ident_f[0:64, 0:64], start=True, stop=True), reads=["gbT"], writes=[("ps", pg)])
            S.act(lambda e, pg=pg: e.activation(out=beta[:], in_=self.ps[pg][:, 16 * d:16 * d + 16], func=AF.Sigmoid), reads=[("ps", pg)], writes=["beta"])
            S.dve(lambda e, pg=pg: e.tensor_tensor(out=xa[:], in0=self.ps[pg][:, 32 + 16 * d:48 + 16 * d], in1=self.g_param[:, 2 + d, :], op=ALU.add), reads=[("ps", pg), "g_param"], writes=["xa"])
            S.act(lambda e: e.activation(out=xa[:], in_=xa[:], func=AF.Exp), reads=["xa"], writes=["xa"])
            S.act(lambda e: e.activation(out=xa[:], in_=xa[:], func=AF.Ln, bias=self.cb[1.0][:, 0:1]), reads=["xa"], writes=["xa"])
            S.dve(lambda e: e.tensor_tensor(out=g[:], in0=xa[:], in1=self.g_negA[:, d, :], op=ALU.mult), reads=["xa", "g_negA"], writes=["g"])
            p1_ = self.psum()
            S.pe(lambda e, p1_=p1_: e.matmul(self.ps[p1_][:, 0:16], tri, g[:], start=True, stop=True), reads=["g", "g_masks"], writes=[("ps", p1_)])
            S.dve(lambda e, p1_=p1_: e.tensor_copy(out=gc[:], in_=self.ps[p1_][:, 0:16]), reads=[("ps", p1_)], writes=["gc"])
            p2_ = self.psum()
            S.pe(lambda e, p2_=p2_: e.matmul(self.ps[p2_][:, 0:16], selL, gc[:], start=True, stop=True), reads=["gc", "g_sel"], writes=[("ps", p2_)])
            S.dve(lambda e, p2_=p2_: e.tensor_tensor(out=kdsc[:], in0=self.ps[p2_][:, 0:16], in1=gc[:], op=ALU.subtract), reads=[("ps", p2_), "gc"], writes=["kdsc"])
            S.act(lambda e: e.activation(out=kdsc[:], in_=kdsc[:], func=AF.Exp), reads=["kdsc"], writes=["kdsc"])
            S.act(lambda e: e.activation(out=scw[:], in_=gc[:], func=AF.Exp), reads=["gc"], writes=["scw"])
            S.dve(lambda e: e.tensor_tensor(out=scw[:], in0=scw[:], in1=beta[:], op=ALU.mult), reads=["scw", "beta"], writes=["scw"])
            p3_ = self.psum()
            for ci in range(2):
                S.pe(lambda e, p3_=p3_, ci=ci: e.matmul(self.ps[p3_][:, ci * 16:(ci + 1) * 16], selC[ci], gc[:], start=True, stop=True), reads=["gc", "g_sel"], writes=[("ps", p3_)])
            S.act(lambda e, p3_=p3_: e.activation(out=egl[:].rearrange("p c h -> p (c h)"), in_=self.ps[p3_][:, 0:32], func=AF.Exp), reads=[("ps", p3_)], writes=["egl"])
            if stop <= 4:
                nunit += 1
                continue
            ob = o_sb[0]
            otok = ("o_sb", 0)
            HG = range(4)
            fl = lambda t: t[:].rearrange("p h t -> p (h t)")
            HS = [slice(hg * 4, hg * 4 + 4) for hg in HG]
            tk = lambda nm, hg: (nm, hg)
            for ci in range(2):
                S.dve(lambda e, ci=ci: e.tensor_scalar(out=kdsc01[:, ci, :], in0=kdsc[:], scalar1=self.g_rowm[:, ci:ci + 1], scalar2=None, op0=ALU.mult), reads=["kdsc", "g_rowm"], writes=["kdsc01"])
            for hg in HG:
                vb4, kbg4, kdm0, kdm1 = BS[hg][2], BS[hg][3], BS[hg][4], BS[hg][5]
                S.pool(lambda e, hg=hg, vb4=vb4: e.tensor_tensor(out=vb4[:], in0=v_tm[:, HS[hg], :], in1=bc_h(beta[:, HS[hg]]), op=ALU.mult), reads=["v_tm", "beta"], writes=[tk("vb4", hg)])
                S.pool(lambda e, hg=hg, kbg4=kbg4: e.tensor_tensor(out=kbg4[:], in0=k_tm[:, HS[hg], :], in1=bc_h(scw[:, HS[hg]]), op=ALU.mult), reads=["k_tm", "scw"], writes=[tk("kbg4", hg)])
                S.pool(lambda e, hg=hg, kdm0=kdm0: e.tensor_tensor(out=kdm0[:], in0=k_tm[:, HS[hg], :], in1=bc_h(kdsc01[:, 0, HS[hg]]), op=ALU.mult), reads=["k_tm", "kdsc01"], writes=[tk("kdm0", hg)])
                S.pool(lambda e, hg=hg, kdm1=kdm1: e.tensor_tensor(out=kdm1[:], in0=k_tm[:, HS[hg], :], in1=bc_h(kdsc01[:, 1, HS[hg]]), op=ALU.mult), reads=["k_tm", "kdsc01"], writes=[tk("kdm1", hg)])
            if stop == 11:
                S.mute = True
            pbs = {}
            for hg in HG:
                Dd = FS[hg][3]
                S.dve(lambda e, hg=hg, Dd=Dd: e.tensor_tensor(out=Dd[:], in0=bc_m(ident_f[:]), in1=bc_h(gc[:, HS[hg]]), op=ALU.mult), reads=["gc"], writes=[tk("D", hg)])
                pb = self.psum()
                pbs[hg] = pb
                S.pe(lambda e, pb=pb, Dd=Dd: e.matmul(self.ps[pb][:, :], ones_f[:], fl(Dd), start=True, stop=True), reads=[tk("D", hg)], writes=[("ps", pb)])
            for hg in HG:
                A_, Dd, pb = FS[hg][0], FS[hg][3], pbs[hg]
                S.act(lambda e, pb=pb, A_=A_: e.copy(out=fl(A_), in_=self.ps[pb][:, :]), reads=[("ps", pb)], writes=[tk("A", hg)])
                S.act(lambda e, pb=pb, Dd=Dd: e.activation(out=fl(Dd), in_=self.ps[pb][:, :], func=AF.Exp), reads=[("ps", pb)], writes=[tk("D", hg)])
            for hg in HG:
                A_, B_, C_ = FS[hg][0], FS[hg][1], FS[hg][2]
                S.dve(lambda e, hg=hg, A_=A_: e.tensor_tensor(out=A_[:], in0=A_[:], in1=bc_h(gc[:, HS[hg]]), op=ALU.subtract), reads=[tk("A", hg), "gc"], writes=[tk("A", hg)])
                S.dve(lambda e, A_=A_, B_=B_: e.tensor_scalar(out=B_[:], in0=A_[:], scalar1=0.0, scalar2=None, op0=ALU.max), reads=[tk("A", hg)], writes=[tk("B", hg)])
                S.dve(lambda e, A_=A_, C_=C_: e.tensor_scalar(out=C_[:], in0=A_[:], scalar1=0.0, scalar2=None, op0=ALU.min), reads=[tk("A", hg)], writes=[tk("C", hg)])
            for hg in HG:
                B_, C_ = FS[hg][1], FS[hg][2]
                S.act(lambda e, B_=B_: e.activation(out=B_[:], in_=B_[:], func=AF.Exp, scale=-1.0), reads=[tk("B", hg)], writes=[tk("B", hg)])
                S.act(lambda e, C_=C_: e.activation(out=C_[:], in_=C_[:], func=AF.Exp), reads=[tk("C", hg)], writes=[tk("C", hg)])
            for hg in HG:
                Dd, qgT4 = FS[hg][3], BS[hg][6]
                S.dve(lambda e, hg=hg, Dd=Dd, qgT4=qgT4: e.tensor_tensor(out=qgT4[:], in0=qT[:, HS[hg], :], in1=Dd[:], op=ALU.mult), reads=["qT", tk("D", hg)], writes=[tk("qgT4", hg)])
            if stop == 12:
                S.mute = True
            pks, pqs = {}, {}
            for hg in HG:
                pk = self.psum()
                pq = self.psum()
                pks[hg], pqs[hg] = pk, pq
                for i in range(4):
                    S.pe(lambda e, pk=pk, i=i, hg=hg: e.matmul(self.ps[pk][:, i * 128:(i + 1) * 128], kT[:, hg * 4 + i, :], kT[:, hg * 4 + i, :], start=True, stop=True), reads=["kT"], writes=[("ps", pk)])
                for i in range(4):
                    S.pe(lambda e, pq=pq, i=i, hg=hg: e.matmul(self.ps[pq][:, i * 128:(i + 1) * 128], kT[:, hg * 4 + i, :], qT[:, hg * 4 + i, :], start=True, stop=True), reads=["kT", "qT"], writes=[("ps", pq)])
                B_, C_, Dd, E_, AT4 = FS[hg][1], FS[hg][2], FS[hg][3], FS[hg][4], BS[hg][0]
                S.dve(lambda e, pk=pk, E_=E_, B_=B_: e.tensor_tensor(out=fl(E_), in0=self.ps[pk][:, :], in1=fl(B_), op=ALU.mult), reads=[("ps", pk), tk("B", hg)], writes=[tk("E", hg)])
                S.pool(lambda e, hg=hg, E_=E_: e.tensor_tensor(out=E_[:], in0=E_[:], in1=bc_h(beta[:, HS[hg]]), op=ALU.mult), reads=[tk("E", hg), "beta"], writes=[tk("E", hg)])
                S.pool(lambda e, E_=E_: e.tensor_tensor(out=E_[:], in0=E_[:], in1=bc_m(strict), op=ALU.mult), reads=[tk("E", hg), "g_masks"], writes=[tk("E", hg)])
                S.dve(lambda e, pq=pq, Dd=Dd, C_=C_: e.tensor_tensor(out=fl(Dd), in0=self.ps[pq][:, :], in1=fl(C_), op=ALU.mult), reads=[("ps", pq), tk("C", hg), tk("qgT4", hg)], writes=[tk("D", hg)])
                S.pool(lambda e, Dd=Dd, AT4=AT4: e.tensor_tensor(out=AT4[:], in0=Dd[:], in1=bc_m(inclT), op=ALU.mult), reads=[tk("D", hg), "g_masks"], writes=[tk("AT4", hg)])
            if stop == 13:
                S.mute = True
            for hg in HG:
                E_, F_, G_ = FS[hg][4], FS[hg][5], FS[hg][6]
                pt_ = self.psum()
                for i in range(4):
                    S.pe(lambda e, pt_=pt_, i=i, E_=E_: e.matmul(self.ps[pt_][:, i * 128:(i + 1) * 128], E_[:, i, :], ident_f[:], start=True, stop=True), reads=[tk("E", hg)], writes=[("ps", pt_)])
                S.act(lambda e, pt_=pt_, F_=F_: e.copy(out=fl(F_), in_=self.ps[pt_][:, :]), reads=[("ps", pt_)], writes=[tk("F", hg)])
                S.dve(lambda e, F_=F_, G_=G_: e.tensor_tensor(out=G_[:], in0=bc_m(ident_f[:]), in1=F_[:], op=ALU.subtract), reads=[tk("F", hg)], writes=[tk("G", hg)])
            if stop == 14:
                S.mute = True
            Mcur = {hg: (FS[hg][4], tk("E", hg)) for hg in HG}
            Mtcur = {hg: (FS[hg][5], tk("F", hg)) for hg in HG}
            Ycur = {hg: 6 for hg in HG}
            for it in range(1, 6):
                for hg in HG:
                    M, Mtok = Mcur[hg]
                    Mt, Mttok = Mtcur[hg]
                    Mn, Mn_t = (FS[hg][3], tk("D", hg)) if it % 2 == 1 else (FS[hg][0], tk("A", hg))
                    pm = self.psum()
                    for i in range(4):
                        S.pe(lambda e, pm=pm, i=i, M=M, Mt=Mt: e.matmul(self.ps[pm][:, i * 128:(i + 1) * 128], Mt[:, i, :], M[:, i, :], start=True, stop=True), reads=[Mtok, Mttok], writes=[("ps", pm)])
                    rd_extra = [tk("AT4", hg), tk("B", hg), tk("C", hg)]
                    S.act(lambda e, pm=pm, Mn=Mn: e.copy(out=fl(Mn), in_=self.ps[pm][:, :]), reads=[("ps", pm)] + rd_extra, writes=[Mn_t])
                    if it < 5:
                        Mtn, Mtn_t = (FS[hg][1], tk("B", hg)) if it % 2 == 1 else (FS[hg][2], tk("C", hg))
                        pm2 = self.psum()
                        for i in range(4):
                            S.pe(lambda e, pm2=pm2, i=i, M=M, Mt=Mt: e.matmul(self.ps[pm2][:, i * 128:(i + 1) * 128], M[:, i, :], Mt[:, i, :], start=True, stop=True), reads=[Mtok, Mttok], writes=[("ps", pm2)])
                        S.dve(lambda e, pm2=pm2, Mtn=Mtn: e.tensor_copy(out=fl(Mtn), in_=self.ps[pm2][:, :]), reads=[("ps", pm2), tk("E", hg)], writes=[Mtn_t])
                    yc = Ycur[hg]
                    yn_ = 13 - yc
                    Yc, Yn = FS[hg][yc], FS[hg][yn_]
                    yct, ynt = tk("G" if yc == 6 else "H", hg), tk("G" if yn_ == 6 else "H", hg)
                    py = self.psum()
                    for i in range(4):
                        S.pe(lambda e, py=py, i=i, Mn=Mn, Yc=Yc: e.matmul(self.ps[py][:, i * 128:(i + 1) * 128], Mn[:, i, :], Yc[:, i, :], start=True, stop=True), reads=[Mn_t, yct], writes=[("ps", py)])
                    S.dve(lambda e, py=py, Yc=Yc, Yn=Yn: e.tensor_tensor(out=fl(Yn), in0=fl(Yc), in1=self.ps[py][:, :], op=ALU.add), reads=[("ps", py), yct], writes=[ynt])
                    Ycur[hg] = yn_
                    Mcur[hg] = (Mn, Mn_t)
                    if it < 5:
                        Mtcur[hg] = (Mtn, Mtn_t)
            if stop == 15:
                S.mute = True
            pus, pws = {}, {}
            for hg in HG:
                yc = Ycur[hg]
                yct = tk("G" if yc == 6 else "H", hg)
                Ybf, vb4, kbg4 = BS[hg][1], BS[hg][2], BS[hg][3]
                S.act(lambda e, hg=hg, yc=yc, Ybf=Ybf: e.copy(out=Ybf[:], in_=FS[hg][yc][:]), reads=[yct], writes=[tk("Ybf", hg)])
                if stop == 21:
                    S.mute = True
                pu = self.psum()
                pw = self.psum()
                pus[hg], pws[hg] = pu, pw
                for i in range(4):
                    S.pe(lambda e, pu=pu, i=i, Ybf=Ybf, vb4=vb4: e.matmul(self.ps[pu][:, i * 128:(i + 1) * 128], Ybf[:, i, :], vb4[:, i, :], start=True, stop=True), reads=[tk("Ybf", hg), tk("vb4", hg)], writes=[("ps", pu)])
                for i in range(4):
                    S.pe(lambda e, pw=pw, i=i, Ybf=Ybf, kbg4=kbg4: e.matmul(self.ps[pw][:, i * 128:(i + 1) * 128], kbg4[:, i, :], Ybf[:, i, :], start=True, stop=True), reads=[tk("Ybf", hg), tk("kbg4", hg)], writes=[("ps", pw)])
            if stop == 22:
                S.mute = True
            for hg in HG:
                u4 = FS[hg][1]
                S.dve(lambda e, hg=hg, u4=u4, pu=pus[hg]: e.tensor_copy(out=fl(u4), in_=self.ps[pu][:, :]), reads=[("ps", pus[hg]), tk("E", hg), tk("F", hg), tk("A", hg), tk("D", hg)], writes=[tk("B", hg)])
            if stop == 23:
                S.mute = True
            for hg in HG:
                wT4 = BS[hg][7]
                if stop == 24 + hg:
                    S.mute = True
                S.dve(lambda e, hg=hg, wT4=wT4, pw=pws[hg]: e.tensor_copy(out=fl(wT4), in_=self.ps[pw][:, :]), reads=[("ps", pws[hg])], writes=[tk("wT4", hg)])
            if stop in (16, 23):
                S.mute = True
            for ci in order:
                pvs, pos_, pSs = {}, {}, {}
                for hg in HG:
                    wT4 = BS[hg][7]
                    pvv = self.psum()
                    pvs[hg] = pvv
                    for i in range(4):
                        S.pe(lambda e, pvv=pvv, i=i, hg=hg, wT4=wT4: e.matmul(self.ps[pvv][:, i * 128:(i + 1) * 128], wT4[:, i, :], Sb[:, hg * 4 + i, :], start=True, stop=True), reads=[tk("wT4", hg), ("Sb", hg)], writes=[("ps", pvv)])
                for hg in HG:
                    u4 = FS[hg][1]
                    S.dve(lambda e, hg=hg, u4=u4, pv_=pvs[hg]: e.tensor_tensor(out=fl(vn[hg]), in0=fl(u4), in1=self.ps[pv_][:, :], op=ALU.subtract), reads=[("ps", pvs[hg]), tk("B", hg)], writes=[("vn", hg)])
                    S.pool(lambda e, hg=hg, ci=ci: e.tensor_tensor(out=St[:, HS[hg], :], in0=St[:, HS[hg], :], in1=bc_h(egl[:, ci, HS[hg]]), op=ALU.mult), reads=[("St", hg), "egl"], writes=[("St", hg)])
                for hg in HG:
                    qgT4, AT4 = BS[hg][6], BS[hg][0]
                    kdm, kdm_t = (BS[hg][4], tk("kdm0", hg)) if ci == 0 else (BS[hg][5], tk("kdm1", hg))
                    po = self.psum()
                    pos_[hg] = po
                    for i in range(4):
                        S.pe(lambda e, po=po, i=i, hg=hg, qgT4=qgT4: e.matmul(self.ps[po][:, i * 128:(i + 1) * 128], qgT4[:, i, :], Sb[:, hg * 4 + i, :], start=True, stop=False), reads=[tk("qgT4", hg), ("Sb", hg)], writes=[("ps", po)])
                        S.pe(lambda e, po=po, i=i, hg=hg, AT4=AT4: e.matmul(self.ps[po][:, i * 128:(i + 1) * 128], AT4[:, i, :], vn[hg][:, i, :], start=False, stop=True), reads=[tk("AT4", hg), ("vn", hg)], writes=[("ps", po)])
                    pS = self.psum()
                    pSs[hg] = pS
                    for i in range(4):
                        S.pe(lambda e, pS=pS, i=i, hg=hg, kdm=kdm: e.matmul(self.ps[pS][:, i * 128:(i + 1) * 128], kdm[:, i, :], vn[hg][:, i, :], start=True, stop=True), reads=[kdm_t, ("vn", hg)], writes=[("ps", pS)])
                for hg in HG:
                    if has_o:
                        R = slice(ci * 64, ci * 64 + 64)
                        S.act(lambda e, hg=hg, R=R, ob=ob, po=pos_[hg]: e.copy(out=ob[R, HS[hg], :], in_=self.ps[po][R, :].rearrange("p (h t) -> p h t", h=4)), reads=[("ps", pos_[hg])], writes=[otok])
                    S.dve(lambda e, hg=hg, pS=pSs[hg]: e.tensor_tensor(out=St[:, HS[hg], :], in0=St[:, HS[hg], :], in1=self.ps[pS][:, :].rearrange("p (h t) -> p h t", h=4), op=ALU.add), reads=[("St", hg), ("ps", pSs[hg])], writes=[("St", hg)])
                    S.act(lambda e, hg=hg: e.copy(out=Sb[:, HS[hg], :], in_=St[:, HS[hg], :]), reads=[("St", hg)], writes=[("Sb", hg)])
            S.mute = False
            if has_o:
                S.dma("sp", o_d[t0:t0 + 128, :], ob[:].rearrange("p h t -> p (h t)"), reads=[otok])
            nunit += 1
        if state_out_d is not None:
            S.dma("sp", state_out_d, St[:], reads=[("St", hg) for hg in range(4)])
        S.pop()

    def ffn_blocks(self, hb, hb_t, T, wg_d, wu_d, wd_d, ev, gate_bc=None, FB=256, FF=5632):
        S = self.S
        wgs = [S.scratch(("wg", i), [128, 16, FB], BF16) for i in range(2)]
        wus = [S.scratch(("wu", i), [128, 16, FB], BF16) for i in range(2)]
        wds = [S.scratch(("wd", i), [128, FB // 128, 2048], BF16) for i in range(2)]
        act, act_t = S.scratch("act", [128, FB // 128, T], BF16)
        sgs = [S.scratch(("sg", i), [128, 512], F32) for i in range(2)]
        Wg = wg_d.rearrange("(k p) n -> p k n", p=128)
        Wu = wu_d.rearrange("(k p) n -> p k n", p=128)
        Wd = wd_d.rearrange("(k p) n -> p k n", p=128)
        if not hasattr(self, "_nffn"):
            self._nffn = 0
        for fb in range(FF // FB):
            par = self._nffn % 2
            self._nffn += 1
            wg, wg_t = wgs[par]
            wu, wu_t = wus[par]
            wd, wd_t = wds[par]
            S.dma("pool", wg[:], Wg[:, :, fb * FB:(fb + 1) * FB], writes=[wg_t])
            S.dma("pool", wu[:], Wu[:, :, fb * FB:(fb + 1) * FB], writes=[wu_t])
            S.dma("pool", wd[:], Wd[:, fb * (FB // 128):(fb + 1) * (FB // 128), :], writes=[wd_t])
            for fc in range(FB // 128):
                for ho in range(0, T, 512):
                    Th = min(512, T - ho)
                    pg = self.psum()
                    pu = self.psum()
                    for k in range(16):
                        S.pe(lambda e, pg=pg, wg=wg, k=k, fc=fc, ho=ho, Th=Th: e.matmul(self.ps[pg][:, 0:Th], wg[:, k, fc * 128:(fc + 1) * 128], hb[:, k, ho:ho + Th], start=(k == 0), stop=(k == 15)),
                             reads=[wg_t, hb_t], writes=[("ps", pg)])
                    for k in range(16):
                        S.pe(lambda e, pu=pu, wu=wu, k=k, fc=fc, ho=ho, Th=Th: e.matmul(self.ps[pu][:, 0:Th], wu[:, k, fc * 128:(fc + 1) * 128], hb[:, k, ho:ho + Th], start=(k == 0), stop=(k == 15)),
                             reads=[wu_t, hb_t], writes=[("ps", pu)])
                    sg, sg_t = sgs[(fc + ho // 512) % 2]
                    S.act(lambda e, sg=sg, pg=pg, Th=Th: e.activation(out=sg[:, 0:Th], in_=self.ps[pg][:, 0:Th], func=AF.Silu), reads=[("ps", pg)], writes=[sg_t])
                    if gate_bc is None:
                        S.dve(lambda e, sg=sg, pu=pu, fc=fc, ho=ho, Th=Th: e.tensor_tensor(out=act[:, fc, ho:ho + Th], in0=sg[:, 0:Th], in1=self.ps[pu][:, 0:Th], op=ALU.mult),
                              reads=[sg_t, ("ps", pu)], writes=[act_t])
                    else:
                        gb, gb_t = gate_bc
                        S.dve(lambda e, sg=sg, pu=pu, Th=Th: e.tensor_tensor(out=sg[:, 0:Th], in0=sg[:, 0:Th], in1=self.ps[pu][:, 0:Th], op=ALU.mult),
                              reads=[sg_t, ("ps", pu)], writes=[sg_t])
                        S.pool(lambda e, sg=sg, fc=fc, ho=ho, Th=Th, gb=gb: e.tensor_tensor(out=act[:, fc, ho:ho + Th], in0=sg[:, 0:Th], in1=gb[:, ho:ho + Th], op=ALU.mult),
                               reads=[sg_t, gb_t], writes=[act_t])
            for c in range(16):
                for ho in range(0, T, 512):
                    Th = min(512, T - ho)
                    pi = self.psum()
                    for kk in range(FB // 128):
                        S.pe(lambda e, pi=pi, wd=wd, kk=kk, c=c, ho=ho, Th=Th: e.matmul(self.ps[pi][:, 0:Th], wd[:, kk, c * 128:(c + 1) * 128], act[:, kk, ho:ho + Th],
                                                                                         start=(kk == 0), stop=(kk == FB // 128 - 1)),
                             reads=[wd_t, act_t], writes=[("ps", pi)])
                    ev(c, 128, ho, Th, pi)

    def tail(self, of_d, ob_d, p1T_d, x1T_d, gnw_d, w_out_d, mv1, rw_d, selE_d, mwg_d, mwu_d, mwd_d, fnw_d, outT_d, tiles=None, n_exp=8, hsel_d=None, routed=None):
        S = self.S
        S.push()
        T = 512
        if tiles is None:
            tiles = [i * 512 for i in range(8 if hsel_d is None else 4)]
        HOFF = NTOK // 2
        if hsel_d is not None:
            hsel = self.load_vec(hsel_d, [128, 2], "hsel")
        zv = p1T_d[6144:8192, :].rearrange("(k p) t -> p k t", p=128)
        x1v = x1T_d.rearrange("(k p) t -> p k t", p=128)
        outv = outT_d.rearrange("(k p) t -> p k t", p=128)
        gnw = self.load_vec(gnw_d, [128, 1], "gnw")
        S.dve(lambda e: e.tensor_scalar(out=gnw[:], in0=gnw[:], scalar1=float(np.sqrt(128.0)), scalar2=None, op0=ALU.mult), reads=["gnw"], writes=["gnw"])
        rw = self.load_vec(rw_d, [128, 16, 8], "rw")
        selE = self.load_vec(selE_d, [8, 8, 128], "selE") if routed is None else None
        Afin = S.sb("Afin", [128, 16, 1], F32)
        Bfin = S.sb("Bfin", [128, 16, 1], F32)
        S.dma("sp", Afin[:, :, 0], fnw_d, writes=["Afin"])
        S.dve(lambda e: e.tensor_scalar(out=Afin[:], in0=Afin[:], scalar1=float(np.sqrt(D)), scalar2=None, op0=ALU.mult), reads=["Afin"], writes=["Afin"])
        S.dve(lambda e: e.memset(Bfin[:], 0.0), writes=["Bfin"])
        if routed is not None:
            gate_all = S.sb("gate_all", [128, 16, 8], F32)
            sel_all = S.sb("sel_all", [128, 16, 8], F32)
        S.push()
        xt = S.sb("xt", [128, 16, T], F32)
        big = S.sb("big", [128, 16, T], F32)
        zt = S.sb("zt", [128, 16, T], F32)
        hb = S.sb("hb", [128, 16, T], BF16)
        ztf = zt[:].rearrange("p k t -> p (k t)")
        sq = S.sb("sq", [128, T], BF16)
        rstd = S.sb("rstd", [128, T], F32)
        zs = S.sb("zs", [128, T], F32)
        gt = S.sb("gt", [128, T], F32)
        h2f = [S.sb("h2f", [128, T], F32) for _ in range(2)]
        lg = S.sb("lg", [128, 4, 8], F32)
        m1 = S.sb("m1", [128, 4], F32)
        m2 = S.sb("m2", [128, 4], F32)
        eq = S.sb("eq", [128, 4, 8], F32)
        l2 = S.sb("l2", [128, 4, 8], F32)
        ee = S.sb("ee", [128, 4, 8], F32)
        ssum = S.sb("ssum", [128, 4], F32)
        gate = S.sb("gate", [128, 4, 8], F32)
        gT = S.sb("gT", [8, T], F32)
        gbc = [S.sb("gbc", [128, T], F32) for _ in range(2)]
        RB = 7

        if routed is not None:
            h2st = [S.sb("h2st", [128, 2048], BF16) for _ in range(2)]
        ntile = [0]

        def bc8(ap2):
            return ap2.rearrange("p (s o) -> p s o", o=1).broadcast_to([128, 4, 8])

        def res_evac(G):
            def ev(c, M, ho, Th, pi):
                S.dve(lambda e: e.scalar_tensor_tensor(out=xt[:, c, ho:ho + Th], in0=self.ps[pi][:, 0:Th], scalar=G[0][:, c, 0:1], in1=xt[:, c, ho:ho + Th], op0=ALU.mult, op1=ALU.add),
                      reads=[("ps", pi), G[1], "xt"], writes=["xt"])
            return ev

        for t0 in tiles:
            S.dma("sp", xt[:], x1v[:, :, t0:t0 + T], writes=["xt"])
            if hsel_d is not None:
                S.dma("sp", big[:], x1v[:, :, HOFF + t0:HOFF + t0 + T], writes=["big"])
                S.act(lambda e: e.activation(out=xt[:], in_=xt[:], func=AF.Copy, scale=hsel[:, 0:1]), reads=["xt", "hsel"], writes=["xt"])
                S.dve(lambda e: e.scalar_tensor_tensor(out=xt[:], in0=big[:], scalar=hsel[:, 1:2], in1=xt[:], op0=ALU.mult, op1=ALU.add), reads=["xt", "big", "hsel"], writes=["xt"])
            ZT = [("zt", i) for i in range(4)]
            for sub in range(4):
                ra, rb = sub % 2, 2 + sub % 2
                a, b_ = ztf[:, ra * 2048:(ra + 1) * 2048], ztf[:, rb * 2048:(rb + 1) * 2048]
                S.dma("sp", a, of_d[t0 + sub * 128:t0 + (sub + 1) * 128, :], writes=[("zt", ra)])
                S.dma("sp", b_, ob_d[t0 + sub * 128:t0 + (sub + 1) * 128, :], writes=[("zt", rb)])
                S.pool(lambda e, a=a, b_=b_: e.tensor_tensor(out=a, in0=a, in1=b_, op=ALU.add), reads=[("zt", ra), ("zt", rb)], writes=[("zt", ra)])
                if hsel_d is not None:
                    S.dve(lambda e, a=a: e.tensor_scalar(out=a, in0=a, scalar1=hsel[:, 0:1], scalar2=None, op0=ALU.mult), reads=[("zt", ra), "hsel"], writes=[("zt", ra)])
                    for src in (of_d, ob_d):
                        S.dma("sp", b_, src[HOFF + t0 + sub * 128:HOFF + t0 + (sub + 1) * 128, :], writes=[("zt", rb)])
                        S.dve(lambda e, a=a, b_=b_: e.scalar_tensor_tensor(out=a, in0=b_, scalar=hsel[:, 1:2], in1=a, op0=ALU.mult, op1=ALU.add),
                              reads=[("zt", ra), ("zt", rb), "hsel"], writes=[("zt", ra)])
                for h4 in range(4):
                    pi = self.psum()
                    for i in range(4):
                        hh = h4 * 4 + i
                        S.pe(lambda e, pi=pi, i=i, hh=hh, a=a: e.matmul(self.ps[pi][:, i * 128:(i + 1) * 128], a[:, hh * 128:(hh + 1) * 128], self.ident_f[:], start=True, stop=True),
                             reads=[("zt", ra)], writes=[("ps", pi)])
                    S.act(lambda e, pi=pi, h4=h4, sub=sub: e.copy(out=big[:, h4 * 4:(h4 + 1) * 4, sub * 128:(sub + 1) * 128], in_=self.ps[pi][:, :].rearrange("p (h t) -> p h t", h=4)),
                          reads=[("ps", pi)], writes=["big"])
            S.dma("sp", zt[:], zv[:, :, t0:t0 + T], writes=ZT)
            if hsel_d is not None:
                pass
            for h in range(16):
                S.act(lambda e, h=h: e.activation(out=sq[:], in_=big[:, h, :], func=AF.Square), reads=["big"], writes=["sq"])
                pi = self.psum()
                S.pe(lambda e, pi=pi: e.matmul(self.ps[pi][:, :], self.ones_bf[:], sq[:], start=True, stop=True), reads=["sq"], writes=[("ps", pi)])
                self.rsqrt(rstd[:], self.ps[pi][:, :], float(128 * EPS), [("ps", pi)], "rstd")
                if hsel_d is not None:
                    S.dma("sp", zs[:], zv[:, h, HOFF + t0:HOFF + t0 + T], writes=["zs"])
                    S.act(lambda e, h=h: e.activation(out=zt[:, h, :], in_=zt[:, h, :], func=AF.Copy, scale=hsel[:, 0:1]), reads=ZT + ["hsel"], writes=ZT)
                    S.dve(lambda e, h=h: e.scalar_tensor_tensor(out=zt[:, h, :], in0=zs[:], scalar=hsel[:, 1:2], in1=zt[:, h, :], op0=ALU.mult, op1=ALU.add),
                          reads=ZT + ["zs", "hsel"], writes=ZT)
                S.act(lambda e, h=h: e.activation(out=zs[:], in_=zt[:, h, :], func=AF.Silu), reads=ZT, writes=["zs"])
                S.dve(lambda e, h=h: e.tensor_tensor(out=gt[:], in0=big[:, h, :], in1=rstd[:], op=ALU.mult), reads=["big", "rstd"], writes=["gt"])
                S.dve(lambda e, h=h: e.scalar_tensor_tensor(out=hb[:, h, :], in0=gt[:], scalar=gnw[:, 0:1], in1=zs[:], op0=ALU.mult, op1=ALU.mult), reads=["gt", "zs", "gnw"], writes=["hb"])
            self.linear_fm(w_out_d, 2048, hb, "hb", T, res_evac(mv1["Gmix"]), wkey="wg", blk=256)
            A, B = mv1["Affn"], mv1["Bffn"]

            def router_cb(c, c0, c1, j, tm, tm_t):
                hf = h2f[c % 2]
                S.act(lambda e: e.activation(out=hf[:], in_=tm[:], func=AF.Identity, bias=B[0][:, c, 0:1], scale=A[0][:, c, 0:1]), reads=[tm_t, A[1], B[1]], writes=[("h2f", c % 2)])
                pr = self.psum()
                for sub in range(4):
                    S.pe(lambda e, sub=sub, pr=pr: e.matmul(self.ps[pr][:, sub * 8:(sub + 1) * 8], hf[:, sub * 128:(sub + 1) * 128], rw[:, c, :], start=True, stop=True),
                         reads=[("h2f", c % 2), "rw"], writes=[("ps", pr)])
                lgf = lg[:].rearrange("p s e -> p (s e)")
                if c == 0:
                    S.dve(lambda e, pr=pr: e.tensor_copy(out=lgf, in_=self.ps[pr][:, 0:32]), reads=[("ps", pr)], writes=["lg"])
                else:
                    S.dve(lambda e, pr=pr: e.tensor_tensor(out=lgf, in0=lgf, in1=self.ps[pr][:, 0:32], op=ALU.add), reads=[("ps", pr), "lg"], writes=["lg"])
            self.rms_mod(xt, "xt", T, [(0, T, 0)], A, B, hb, 0, "hb", extra_out=router_cb)
            S.dve(lambda e: e.tensor_reduce(out=m1[:], in_=lg[:], axis=AX.X, op=ALU.max), reads=["lg"], writes=["m1"])
            S.dve(lambda e: e.tensor_tensor(out=eq[:], in0=lg[:], in1=bc8(m1[:]), op=ALU.is_equal), reads=["lg", "m1"], writes=["eq"])
            S.dve(lambda e: e.scalar_tensor_tensor(out=l2[:], in0=eq[:], scalar=-1e30, in1=lg[:], op0=ALU.mult, op1=ALU.add), reads=["eq", "lg"], writes=["l2"])
            S.dve(lambda e: e.tensor_reduce(out=m2[:], in_=l2[:], axis=AX.X, op=ALU.max), reads=["l2"], writes=["m2"])
            S.dve(lambda e: e.tensor_tensor(out=eq[:], in0=lg[:], in1=bc8(m2[:]), op=ALU.is_ge), reads=["lg", "m2", "l2"], writes=["eq"])
            S.dve(lambda e: e.tensor_tensor(out=l2[:], in0=lg[:], in1=bc8(m1[:]), op=ALU.subtract), reads=["lg", "m1", "eq"], writes=["l2"])
            S.act(lambda e: e.activation(out=ee[:], in_=l2[:], func=AF.Exp), reads=["l2"], writes=["ee"])
            S.dve(lambda e: e.tensor_tensor(out=ee[:], in0=ee[:], in1=eq[:], op=ALU.mult), reads=["ee", "eq"], writes=["ee"])
            S.dve(lambda e: e.tensor_reduce(out=ssum[:], in_=ee[:], axis=AX.X, op=ALU.add), reads=["ee"], writes=["ssum"])
            S.dve(lambda e: e.reciprocal(out=ssum[:], in_=ssum[:]), reads=["ssum"], writes=["ssum"])
            S.dve(lambda e: e.tensor_tensor(out=gate[:], in0=ee[:], in1=bc8(ssum[:]), op=ALU.mult), reads=["ee", "ssum"], writes=["gate"])
            if routed is not None:
                ti = ntile[0]
                ntile[0] += 1
                x2v = routed["x2T"].rearrange("(k p) t -> p k t", p=128)
                S.dma("sp", x2v[:, :, ti * T:(ti + 1) * T], xt[:], reads=["xt"])
                S.dve(lambda e, ti=ti: e.tensor_copy(out=gate_all[:, ti * 4:(ti + 1) * 4, :], in_=gate[:]), reads=["gate"], writes=["gate_all"])
                S.dve(lambda e, ti=ti: e.tensor_copy(out=sel_all[:, ti * 4:(ti + 1) * 4, :], in_=eq[:]), reads=["eq"], writes=["sel_all"])
                for sub in range(4):
                    hst = h2st[sub % 2]
                    for c4 in range(4):
                        pi = self.psum()
                        for i_ in range(4):
                            S.pe(lambda e, pi=pi, i_=i_, c4=c4, sub=sub: e.matmul(self.ps[pi][:, i_ * 128:(i_ + 1) * 128], hb[:, c4 * 4 + i_, sub * 128:(sub + 1) * 128], self.ident_bf[:], start=True, stop=True),
                                 reads=["hb"], writes=[("ps", pi)])
                        if c4 % 2 == 0:
                            S.act(lambda e, pi=pi, c4=c4, hst=hst: e.copy(out=hst[:, c4 * 512:(c4 + 1) * 512], in_=self.ps[pi][:, :]), reads=[("ps", pi)], writes=[("h2st", sub % 2)])
                        else:
                            S.dve(lambda e, pi=pi, c4=c4, hst=hst: e.tensor_copy(out=hst[:, c4 * 512:(c4 + 1) * 512], in_=self.ps[pi][:, :]), reads=[("ps", pi)], writes=[("h2st", sub % 2)])
                    S.dma("sp", routed["h2tm"][(ti * 4 + sub) * 128:(ti * 4 + sub + 1) * 128, :], hst[:], reads=[("h2st", sub % 2)])
                continue
            pgt = self.psum()
            for sub in range(4):
                S.pe(lambda e, sub=sub, pgt=pgt: e.matmul(self.ps[pgt][0:8, sub * 128:(sub + 1) * 128], gate[:, sub, :], self.ident_f[:], start=True, stop=True), reads=["gate"], writes=[("ps", pgt)])
            S.dve(lambda e, pgt=pgt: e.tensor_copy(out=gT[:], in_=self.ps[pgt][0:8, :]), reads=[("ps", pgt)], writes=["gT"])
            ev = res_evac(mv1["Gffn"])
            for ex in range(n_exp):
                gb = gbc[ex % 2]
                pgb = self.psum()
                S.pe(lambda e, pgb=pgb, ex=ex: e.matmul(self.ps[pgb][:, :], selE[:, ex, :], gT[:], start=True, stop=True), reads=["gT", "selE"], writes=[("ps", pgb)])
                S.act(lambda e, pgb=pgb, gb=gb: e.copy(out=gb[:], in_=self.ps[pgb][:, :]), reads=[("ps", pgb)], writes=[("gbc", ex % 2)])
                self.ffn_blocks(hb, "hb", T, mwg_d[ex], mwu_d[ex], mwd_d[ex], ev, gate_bc=(gb, ("gbc", ex % 2)))
            self.rms_mod(xt, "xt", T, [(0, T, 0)], (Afin, "Afin"), (Bfin, "Bfin"), big, 0, "big")
            S.dma("sp", outv[:, :, t0:t0 + T], big[:], reads=["big"])
        S.pop()
        if routed is not None:
            self.moe_routed(routed, gate_all, sel_all, mwg_d, mwu_d, mwd_d, mv1, Afin, Bfin, outv, n_exp)
        self.ps_skip = set()
        S.pop()

    def moe_routed(self, r, gate_all, sel_all, mwg_d, mwu_d, mwd_d, mv1, Afin, Bfin, outv, n_exp=8):
        S = self.S
        S.barrier()
        CAP = 1280
        NS = CAP // 128
        NT = 16
        S.push()
        pos = S.sb("pos", [128, NT, 8], F32)
        base = S.sb("base", [128, 8], F32)
        S.push()
        rtc0 = self.load_vec(r["rtc"][:, 0:128], [128, 128], "rtc0")
        tris = rtc0[:, 0:128]
        S.dve(lambda e: e.memset(base[:], 0.0), writes=["base"])
        for j in range(NT):
            pi = self.psum()
            S.pe(lambda e, pi=pi, j=j: e.matmul(self.ps[pi][:, 0:8], tris, sel_all[:, j, :], start=True, stop=True), reads=["sel_all", "rtc0"], writes=[("ps", pi)])
            S.pe(lambda e, pi=pi, j=j: e.matmul(self.ps[pi][:, 8:16], self.ones_f[:], sel_all[:, j, :], start=True, stop=True), reads=["sel_all"], writes=[("ps", pi)])
            S.dve(lambda e, pi=pi, j=j: e.tensor_tensor(out=pos[:, j, :], in0=self.ps[pi][:, 0:8], in1=base[:], op=ALU.add), reads=[("ps", pi), "base"], writes=["pos"])
            S.dve(lambda e, j=j: e.scalar_tensor_tensor(out=pos[:, j, :], in0=pos[:, j, :], scalar=1.0, in1=sel_all[:, j, :], op0=ALU.add, op1=ALU.mult), reads=["pos", "sel_all"], writes=["pos"])
            S.dve(lambda e, j=j: e.tensor_scalar(out=pos[:, j, :], in0=pos[:, j, :], scalar1=-1.0, scalar2=None, op0=ALU.add), reads=["pos"], writes=["pos"])
            S.dve(lambda e, pi=pi: e.tensor_tensor(out=base[:], in0=base[:], in1=self.ps[pi][:, 8:16], op=ALU.add), reads=[("ps", pi), "base"], writes=["base"])
        S.pop()
        hg = S.sb("hg", [128, 16, CAP], BF16)
        st2 = [S.sb("st2", [128, 2048], BF16) for _ in range(2)]
        nst = [0]
        h2v = r["h2tm"].rearrange("(j p) f -> p j f", p=128)
        for ex in range(n_exp):
            S.push()
            iot = self.load_vec(r["rtc"][:, 128:128 + CAP], [128, CAP], "iota")
            iota = iot[:, :]
            selb = S.sb("selb", [128, NT, CAP], BF16)
            h2sb = S.sb("h2sb", [128, NT, 2048], BF16)
            for jq in range(4):
                S.dma("sp", h2sb[:, jq * 4:(jq + 1) * 4, :], h2v[:, jq * 4:(jq + 1) * 4, :], writes=["h2sb"])
            for j in range(NT):
                S.dve(lambda e, j=j, ex=ex: e.tensor_scalar(out=selb[:, j, :], in0=iota, scalar1=pos[:, j, ex:ex + 1], scalar2=gate_all[:, j, ex:ex + 1], op0=ALU.is_equal, op1=ALU.mult),
                      reads=["pos", "gate_all", "iota"], writes=["selb"])
            for s_ in range(NS):
                si = nst[0] % 2
                nst[0] += 1
                for j4 in range(4):
                    pi = self.psum()
                    for i_ in range(4):
                        S.pe(lambda e, pi=pi, i_=i_, j4=j4, s_=s_: e.matmul(self.ps[pi][:, i_ * 128:(i_ + 1) * 128], selb[:, j4 * 4 + i_, s_ * 128:(s_ + 1) * 128], self.ident_bf[:], start=True, stop=True),
                             reads=["selb"], writes=[("ps", pi)])
                    S.act(lambda e, pi=pi, j4=j4, si=si: e.copy(out=st2[si][:, j4 * 512:(j4 + 1) * 512], in_=self.ps[pi][:, :]), reads=[("ps", pi)], writes=[("st2", si)])
                S.dma("sp", r["sgt"][(ex * NS + s_) * 128:(ex * NS + s_ + 1) * 128, :], st2[si][:], reads=[("st2", si)])
            for j in range(NT):
                S.dve(lambda e, j=j, ex=ex: e.tensor_scalar(out=selb[:, j, :], in0=iota, scalar1=pos[:, j, ex:ex + 1], scalar2=None, op0=ALU.is_equal),
                      reads=["pos", "iota"], writes=["selb"])
            for c in range(16):
                for (h0, hn) in ((0, 512), (512, 512), (1024, 256)):
                    pi = self.psum()
                    for j in range(NT):
                        S.pe(lambda e, pi=pi, j=j, c=c, h0=h0, hn=hn: e.matmul(self.ps[pi][:, 0:hn], h2sb[:, j, c * 128:(c + 1) * 128], selb[:, j, h0:h0 + hn], start=(j == 0), stop=(j == NT - 1)),
                             reads=["h2sb", "selb"], writes=[("ps", pi)])
                    if c % 2 == 0:
                        S.act(lambda e, pi=pi, c=c, h0=h0, hn=hn: e.copy(out=hg[:, c, h0:h0 + hn], in_=self.ps[pi][:, 0:hn]), reads=[("ps", pi)], writes=["hg"])
                    else:
                        S.dve(lambda e, pi=pi, c=c, h0=h0, hn=hn: e.tensor_copy(out=hg[:, c, h0:h0 + hn], in_=self.ps[pi][:, 0:hn]), reads=[("ps", pi)], writes=["hg"])
            S.pop()
            S.push()
            yeT = S.sb("yeT", [128, 16, CAP], F32)
            seen = set()

            def ev_acc(c, M, ho, Th, pi, yeT=yeT, seen=seen):
                if (c, ho) not in seen:
                    seen.add((c, ho))
                    S.dve(lambda e: e.tensor_copy(out=yeT[:, c, ho:ho + Th], in_=self.ps[pi][:, 0:Th]), reads=[("ps", pi)], writes=["yeT"])
                else:
                    S.dve(lambda e: e.tensor_tensor(out=yeT[:, c, ho:ho + Th], in0=yeT[:, c, ho:ho + Th], in1=self.ps[pi][:, 0:Th], op=ALU.add), reads=[("ps", pi), "yeT"], writes=["yeT"])
            self.ffn_blocks(hg, "hg", CAP, mwg_d[ex], mwu_d[ex], mwd_d[ex], ev_acc)
            for s_ in range(NS):
                si = nst[0] % 2
                nst[0] += 1
                for c4 in range(4):
                    pi = self.psum()
                    for i_ in range(4):
                        S.pe(lambda e, pi=pi, i_=i_, c4=c4, s_=s_, yeT=yeT: e.matmul(self.ps[pi][:, i_ * 128:(i_ + 1) * 128], yeT[:, c4 * 4 + i_, s_ * 128:(s_ + 1) * 128], self.ident_f[:], start=True, stop=True),
                             reads=["yeT"], writes=[("ps", pi)])
                    S.act(lambda e, pi=pi, c4=c4, si=si: e.copy(out=st2[si][:, c4 * 512:(c4 + 1) * 512], in_=self.ps[pi][:, :]), reads=[("ps", pi)], writes=[("st2", si)])
                S.dma("sp", r["ye"][(ex * NS + s_) * 128:(ex * NS + s_ + 1) * 128, :], st2[si][:], reads=[("st2", si)])
            S.pop()
        S.pop()
        S.push()
        T = 256
        NSL = n_exp * NS
        xt = S.sb("xt", [128, 16, T], F32)
        big = S.sb("big", [128, 16, T], F32)
        sgt = S.sb("sgt", [128, NSL, T], BF16)
        yec = [S.sb("yec", [128, NSL, 128], BF16) for _ in range(2)]
        x2v = r["x2T"].rearrange("(k p) t -> p k t", p=128)
        sgv = r["sgt"].rearrange("(n p) t -> p n t", p=128)
        yev = r["ye"].rearrange("(n p) f -> p n f", p=128)
        G = mv1["Gffn"]
        for ti in range(2048 // T):
            S.dma("sp", xt[:], x2v[:, :, ti * T:(ti + 1) * T], writes=["xt"])
            for q in range(4):
                S.dma("sp", sgt[:, q * (NSL // 4):(q + 1) * (NSL // 4), :], sgv[:, q * (NSL // 4):(q + 1) * (NSL // 4), ti * T:(ti + 1) * T], writes=["sgt"])
            for c in range(16):
                yb = yec[c % 2]
                for q in range(2):
                    S.dma("pool", yb[:, q * (NSL // 2):(q + 1) * (NSL // 2), :], yev[:, q * (NSL // 2):(q + 1) * (NSL // 2), c * 128:(c + 1) * 128], writes=[("yec", c % 2)])
                pi = self.psum()
                for n in range(NSL):
                    S.pe(lambda e, pi=pi, n=n, yb=yb: e.matmul(self.ps[pi][:, 0:T], yb[:, n, :], sgt[:, n, :], start=(n == 0), stop=(n == NSL - 1)),
                         reads=[("yec", c % 2), "sgt"], writes=[("ps", pi)])
                S.dve(lambda e, pi=pi, c=c: e.scalar_tensor_tensor(out=xt[:, c, :], in0=self.ps[pi][:, 0:T], scalar=G[0][:, c, 0:1], in1=xt[:, c, :], op0=ALU.mult, op1=ALU.add),
                      reads=[("ps", pi), "xt"], writes=["xt"])
            self.rms_mod(xt, "xt", T, [(0, T, 0)], (Afin, "Afin"), (Bfin, "Bfin"), big, 0, "big")
            S.dma("sp", outv[:, :, ti * T:(ti + 1) * T], big[:], reads=["big"])
        S.pop()

def _host_consts():
    ident = np.eye(128, dtype=np.float32)
    P = np.zeros((128, 128), np.float32)
    for m in range(128):
        if (m % 64) < 32:
            P[m + 32, m] = -1.0
        else:
            P[m - 32, m] = 1.0
    t = np.arange(4096)
    row = (t // 64).astype(np.float32)
    col = (t % 64).astype(np.float32)
    inv = (np.float32(10000.0) ** (-np.arange(32, dtype=np.float32) / np.float32(32))).astype(np.float32)
    cos = np.zeros((128, 4096), np.float32)
    sin = np.zeros((128, 4096), np.float32)
    for d in range(128):
        pos = row if d < 64 else col
        ang = (pos * inv[d % 32]).astype(np.float32)
        cos[d] = np.cos(ang)
        sin[d] = np.sin(ang)
    p = np.arange(128)[:, None]
    f = np.arange(128)[None, :]
    same = (p // 64) == (f // 64)
    masks = np.stack([same & (f <= p), same & (f >= p), same & (f < p), same & (f > p)], 1).astype(np.float32).copy()
    k = np.arange(128)[:, None]
    pp = np.arange(128)[None, :]
    sel = np.stack([k == (pp // 64) * 64 + 63, k == (pp // 64) * 64, (k == 63) & (pp >= 0), (k == 127) & (pp >= 0),
                    (k == 0) & (pp >= 0), (k == 64) & (pp >= 0)], 1).astype(np.float32).copy()
    selE = (np.arange(8)[:, None, None] == np.arange(8)[None, :, None]).astype(np.float32).repeat(128, 2).copy()
    rtc = np.concatenate([(np.arange(128)[:, None] < np.arange(128)[None, :]).astype(np.float32),
                          np.arange(1280, dtype=np.float32)[None, :].repeat(128, 0)], axis=1).copy()
    return dict(ident=ident, perm=P, cos=cos, sin=sin, masks=masks, sel=sel, selE=selE, rtc=rtc)


def _na_bias(rpb):
    out = np.full((128, 5, 8, 5, 128), -30000.0, np.float32)
    for ti, lb in enumerate([0, 1, 2, 30, 31]):
        tile0 = min(max(lb - 2, 0), 27)
        q = lb * 128 + np.arange(128)
        r = q // 64
        c = q % 64
        rs = np.clip(r - 4, 0, 56)
        cs = np.clip(c - 8, 0, 48)
        for j in range(5):
            kk = (tile0 + j) * 128 + np.arange(128)
            kr = kk // 64
            kc = kk % 64
            inw = (kr[:, None] >= rs[None, :]) & (kr[:, None] < rs[None, :] + 8) & (kc[:, None] >= cs[None, :]) & (kc[:, None] < cs[None, :] + 16)
            ro = np.clip(kr[:, None] - r[None, :] + 7, 0, 14)
            co = np.clip(kc[:, None] - c[None, :] + 15, 0, 30)
            for h in range(8):
                out[:, ti, h, j, :] = np.where(inw, rpb[h][ro, co], -30000.0)
    return out.astype(ml_dtypes.bfloat16)


def _chunked(v, n=16):
    return np.ascontiguousarray(np.asarray(v, np.float32).reshape(n, 128).T)


def build_nc():
    nc = bass.Bass("TRN2", target_bir_lowering=False)
    k = K(nc)
    i = {}
    i["ident"] = k.din("ident", [128, 128]); i["perm"] = k.din("perm", [128, 128])
    i["cT"] = k.din("cT", [128, 16, 2])
    i["ada_w0"] = k.din("ada_w0", [2048, 12288]); i["ada_b0"] = k.din("ada_b0", [128, 96])
    i["ada_w1"] = k.din("ada_w1", [2048, 12288]); i["ada_b1"] = k.din("ada_b1", [128, 96])
    for nm in ("nmix0", "nffn0", "nmix1", "nffn1", "fnw"):
        i[nm] = k.din(nm, [128, 16])
    i["xcT"] = k.din("xcT", [2048, NU]); i["w_in0"] = k.din("w_in0", [2048, 4608])
    i["qnw"] = k.din("qnw", [128, 1]); i["knw"] = k.din("knw", [128, 1]); i["gnw"] = k.din("gnw", [128, 1])
    i["cos"] = k.din("cos", [128, 4096]); i["sin"] = k.din("sin", [128, 4096])
    i["nabias"] = k.din("nabias", [128, 5, 8, 5, 128], BF16)
    i["w_out0"] = k.din("w_out0", [2048, 2048]); i["wg"] = k.din("wg", [2048, 5632]); i["wu"] = k.din("wu", [2048, 5632]); i["wd"] = k.din("wd", [5632, 2048])
    i["w_in1"] = k.din("w_in1", [2048, 8256])
    i["convw"] = k.din("convw", [128, 48, 4]); i["gparam"] = k.din("gparam", [128, 4, 16]); i["masks"] = k.din("masks", [128, 4, 128]); i["sel"] = k.din("sel", [128, 6, 128])
    i["w_out1"] = k.din("w_out1", [2048, 2048]); i["rw"] = k.din("rw", [128, 16, 8]); i["selE"] = k.din("selE", [8, 8, 128])
    i["mwg"] = k.din("mwg", [8, 2048, 5632]); i["mwu"] = k.din("mwu", [8, 2048, 5632]); i["mwd"] = k.din("mwd", [8, 5632, 2048])
    outT = k.dout("outT", [2048, NTOK // 2])
    i["hsel"] = k.din("hsel", [128, 2])
    sc = {"qna": k.dscr("qna", [8, 128, NU], BF16), "kna": k.dscr("kna", [8, 128, NU], BF16), "vna": k.dscr("vna", [NU, 1024], BF16),
          "qg": k.dscr("qg", [8, 128, NU], BF16), "kg": k.dscr("kg", [2, 128, NU], BF16), "vg": k.dscr("vg", [NU, 256], BF16)}
    attnT = k.dscr("attnT", [2048, NU], BF16)
    x1T = k.dscr("x1T", [2048, NU]); p1T = k.dscr("p1T", [8256, NU])
    o_f = k.dscr("o_f", [NTOK, 2048]); o_b = k.dscr("o_b", [NTOK, 2048])
    routed = {"x2T": k.dscr("x2T", [2048, NTOK // 2]), "h2tm": k.dscr("h2tm", [NTOK // 2, 2048], BF16),
              "sgt": k.dscr("sgt", [8 * 1280, NTOK // 2], BF16), "ye": k.dscr("ye", [8 * 1280, 2048], BF16), "rtc": k.din("rtc", [128, 128 + 1280])}
    S = k.S
    k.load_consts(i["ident"], i["perm"])
    mod0 = S.sb("mod0", [128, 96, 2], F32)
    mod1 = S.sb("mod1", [128, 96, 2], F32)
    k.ada(i["cT"], i["ada_w0"], i["ada_b0"], mod0, "ada0")
    k.ada(i["cT"], i["ada_w1"], i["ada_b1"], mod1, "ada1")
    mv0 = k.mod_vectors(mod0, i["nmix0"], i["nffn0"], "ada0")
    mv1 = k.mod_vectors(mod1, i["nmix1"], i["nffn1"], "ada1")
    k.gdn_consts(i["convw"], i["gparam"], i["masks"], i["sel"])
    k.inproj0(i["xcT"], i["w_in0"], mv0, i["qnw"], i["knw"], i["cos"], i["sin"], sc)
    k.attn_gqa(sc["qg"], sc["kg"], sc["vg"], attnT)
    k.attn_na(sc["qna"], sc["kna"], sc["vna"], i["nabias"], attnT)
    k.mlp0(i["xcT"], attnT, i["w_out0"], mv0, i["wg"], i["wu"], i["wd"], mv1, i["w_in1"], x1T, p1T)
    gcache = k.dscr("gcache", [34, 4, 128, 16, 128], BF16)
    k.gdn_pass(p1T, 0, o_f, cache_d=gcache, cache_mode="write")
    k.gdn_pass(p1T, 1, o_b, cache_d=gcache, cache_mode="read")
    k.tail(o_f, o_b, p1T, x1T, i["gnw"], i["w_out1"], mv1, i["rw"], i["selE"], i["mwg"], i["mwu"], i["mwd"], i["fnw"], outT, hsel_d=i["hsel"], routed=routed)
    S.emit()
    return nc


_NC_CACHE = {}


def kernel(x, c, ctx, c_ctx, ada_w0, ada_b0, norm_mix0, norm_ffn0, w_in0, w_out0, na_rpb, q_norm_w, k_norm_w,
           ffn_w_gate, ffn_w_up, ffn_w_down, ada_w1, ada_b1, norm_mix1, norm_ffn1, w_in1, conv_w1,
           a_log_fwd, a_log_bwd, dt_bias_fwd, dt_bias_bwd, gdn_norm_w, w_out1, router_w,
           moe_w_gate, moe_w_up, moe_w_down, final_norm_w):
    f32 = lambda a: np.ascontiguousarray(np.asarray(a, dtype=np.float32))
    x, c, ctx, c_ctx = f32(x), f32(c), f32(ctx), f32(c_ctx)
    hc = _host_consts()
    shared = {
        "ident": hc["ident"], "perm": hc["perm"], "cos": hc["cos"], "sin": hc["sin"], "masks": hc["masks"], "sel": hc["sel"], "selE": hc["selE"], "rtc": hc["rtc"],
        "ada_w0": f32(ada_w0), "ada_b0": _chunked(ada_b0, 96), "ada_w1": f32(ada_w1), "ada_b1": _chunked(ada_b1, 96),
        "nmix0": _chunked(norm_mix0), "nffn0": _chunked(norm_ffn0), "nmix1": _chunked(norm_mix1), "nffn1": _chunked(norm_ffn1), "fnw": _chunked(final_norm_w),
        "w_in0": f32(w_in0), "qnw": f32(q_norm_w).reshape(128, 1), "knw": f32(k_norm_w).reshape(128, 1), "gnw": f32(gdn_norm_w).reshape(128, 1),
        "nabias": _na_bias(f32(na_rpb)),
        "w_out0": f32(w_out0), "wg": f32(ffn_w_gate), "wu": f32(ffn_w_up), "wd": f32(ffn_w_down), "w_in1": f32(w_in1),
        "convw": np.ascontiguousarray(f32(conv_w1).reshape(4, 48, 128).transpose(2, 1, 0)),
        "gparam": np.ascontiguousarray(np.stack([f32(a_log_fwd), f32(a_log_bwd), f32(dt_bias_fwd), f32(dt_bias_bwd)], 0)[None].repeat(128, 0)),
        "w_out1": f32(w_out1), "rw": np.ascontiguousarray(f32(router_w).reshape(16, 128, 8).transpose(1, 0, 2)),
        "mwg": f32(moe_w_gate), "mwu": f32(moe_w_up), "mwd": f32(moe_w_down),
    }
    in_maps = []
    for core in range(8):
        b = core % 4
        m = dict(shared)
        m["cT"] = np.ascontiguousarray(np.stack([c[b], c_ctx], -1).reshape(16, 128, 2).transpose(1, 0, 2))
        m["xcT"] = np.ascontiguousarray(np.concatenate([x[b].T, ctx[b].T], axis=1))
        hs = np.zeros((128, 2), np.float32)
        hs[:, core // 4] = 1.0
        m["hsel"] = hs
        in_maps.append(m)
    if "nc" not in _NC_CACHE:
        _NC_CACHE["nc"] = build_nc()
    res = run_bass_kernel_spmd(_NC_CACHE["nc"], in_maps, core_ids=list(range(8)))
    out = np.stack([np.concatenate([np.asarray(res.results[b]["outT"]).T, np.asarray(res.results[b + 4]["outT"]).T], axis=0) for b in range(4)], 0)
    return np.ascontiguousarray(out.astype(np.float32))
```
